# Optimizing a Trainium2 kernel written in Bass

```python
import jax, jax.numpy as jnp
from jax import lax
import numpy as np

D_MODEL = 1024
BATCH = 4
SEQ = 8192
DEPTH = 2

MIX_WIDTH = D_MODEL
CONV_CH = MIX_WIDTH // 2
CONV_GROUPS = 8
CONV_KERNEL = 31
HEAD_DIM = 64
N_Q_HEADS = (MIX_WIDTH - CONV_CH) // HEAD_DIM
N_KV_HEADS = 2
KV_GROUP = N_Q_HEADS // N_KV_HEADS
ATTN_WIDTH = N_Q_HEADS * HEAD_DIM
KV_WIDTH = N_KV_HEADS * HEAD_DIM
WINDOW = 128
BLOCK = 128
SPLITS = (CONV_CH, 2 * CONV_CH, 2 * CONV_CH + ATTN_WIDTH,
          2 * CONV_CH + ATTN_WIDTH + KV_WIDTH)
IN_COLS = 2 * CONV_CH + ATTN_WIDTH + 2 * KV_WIDTH
D_FF = 2816
N_EXPERTS = 8
TOP_K = 2
D_FF_EXPERT = 3584
N_DENSE = (DEPTH + 1) // 2
N_MOE = DEPTH // 2
EPS = 1e-5

kernel_name = "hymba_conformer_swa_sink_moe_trunk"


def _rmsnorm(x, g):
    xf = x.astype(jnp.float32)
    y = xf * lax.rsqrt(jnp.mean(xf * xf, axis=-1, keepdims=True) + EPS)
    return (y * g.astype(jnp.float32)).astype(x.dtype)


def _layernorm(x, g, b):
    xf = x.astype(jnp.float32)
    mu = jnp.mean(xf, axis=-1, keepdims=True)
    xc = xf - mu
    y = xc * lax.rsqrt(jnp.mean(xc * xc, axis=-1, keepdims=True) + EPS)
    return (y * g.astype(jnp.float32) + b.astype(jnp.float32)).astype(x.dtype)


def _conformer_conv(a, gate, w_dw, b_dw, ln_g, ln_b):
    u = a * jax.nn.sigmoid(gate)
    u = jnp.pad(u, ((0, 0), (CONV_KERNEL - 1, 0), (0, 0)))
    y = lax.conv_general_dilated(
        u, w_dw[:, None, :], window_strides=(1,), padding='VALID',
        dimension_numbers=('NWC', 'WIO', 'NWC'),
        feature_group_count=CONV_CH) + b_dw
    y = _layernorm(y, ln_g, ln_b)
    return jax.nn.silu(y)


def _swa_sink_attention(q, k, v, sinks):
    B, S = q.shape[0], q.shape[1]
    nb = S // BLOCK
    qb = q.reshape(B, nb, BLOCK, N_KV_HEADS, KV_GROUP, HEAD_DIM)
    kb = k.reshape(B, nb, BLOCK, N_KV_HEADS, HEAD_DIM)
    vb = v.reshape(B, nb, BLOCK, N_KV_HEADS, HEAD_DIM)
    pad = ((0, 0), (1, 0), (0, 0), (0, 0), (0, 0))
    k_band = jnp.concatenate([jnp.pad(kb, pad)[:, :-1], kb], axis=2)
    v_band = jnp.concatenate([jnp.pad(vb, pad)[:, :-1], vb], axis=2)
    scale = HEAD_DIM ** -0.5
    s = jnp.einsum('bnqhgd,bnkhd->bnhgqk', qb, k_band,
                   preferred_element_type=jnp.float32) * scale
    q_pos = jnp.arange(BLOCK)[:, None] + BLOCK
    k_pos = jnp.arange(2 * BLOCK)[None, :]
    rel = q_pos - k_pos
    local = (rel >= 0) & (rel < WINDOW)
    has_prev = (jnp.arange(nb)[:, None, None] > 0) | (k_pos >= BLOCK)[None]
    valid = local[None] & has_prev
    s = jnp.where(valid[None, :, None, None], s, -jnp.inf)
    sink = sinks.astype(jnp.float32).reshape(N_KV_HEADS, KV_GROUP)[None, None, :, :, None, None]
    m = jnp.maximum(jnp.max(s, axis=-1, keepdims=True), sink)
    p = jnp.exp(s - m)
    p = p / (jnp.sum(p, axis=-1, keepdims=True) + jnp.exp(sink - m))
    o = jnp.einsum('bnhgqk,bnkhd->bnqhgd', p.astype(v.dtype), v_band)
    return o.reshape(B, S, ATTN_WIDTH)


def _mixer(h, w_in, b_in, conv_w, conv_b, conv_ln_g, conv_ln_b, sinks, w_out, b_out):
    B, S, _ = h.shape
    z = jnp.einsum('bsd,de->bse', h, w_in) + b_in
    a, gate, q, k, v = jnp.split(z, SPLITS, axis=-1)
    conv_out = _conformer_conv(a, gate, conv_w, conv_b, conv_ln_g, conv_ln_b)
    attn_out = _swa_sink_attention(
        q.reshape(B, S, N_Q_HEADS, HEAD_DIM),
        k.reshape(B, S, N_KV_HEADS, HEAD_DIM),
        v.reshape(B, S, N_KV_HEADS, HEAD_DIM), sinks)
    y = jnp.concatenate([conv_out, attn_out], axis=-1)
    return jnp.einsum('bse,ed->bsd', y, w_out) + b_out


def _swiglu(h, w_gate, w_up, w_down):
    return (jax.nn.silu(h @ w_gate) * (h @ w_up)) @ w_down


def _moe(h, w_router, w_gate, w_up, w_down):
    B, S, D = h.shape
    t = h.reshape(B * S, D)
    logits = (t @ w_router).astype(jnp.float32)
    top_vals, top_idx = lax.top_k(logits, TOP_K)
    top_w = jax.nn.softmax(top_vals, axis=-1)
    gates = jnp.sum(jax.nn.one_hot(top_idx, N_EXPERTS, dtype=jnp.float32)
                    * top_w[..., None], axis=1).astype(t.dtype)
    out = jnp.zeros_like(t)
    for e in range(N_EXPERTS):
        out = out + gates[:, e:e + 1] * _swiglu(t, w_gate[e], w_up[e], w_down[e])
    return out.reshape(B, S, D)


def setup_inputs(seed: int = 0) -> dict:
    key = jax.random.key(seed)
    ks = jax.random.split(key, 24)
    nrm = lambda k, shape, s: jax.random.normal(k, shape, jnp.float32) * s
    d = D_MODEL
    return {
        "x": nrm(ks[0], (BATCH, SEQ, d), 1.0),
        "attn_norm": 1.0 + nrm(ks[1], (DEPTH, d), 0.02),
        "ffn_norm": 1.0 + nrm(ks[2], (DEPTH, d), 0.02),
        "w_in": nrm(ks[3], (DEPTH, d, IN_COLS), d ** -0.5),
        "b_in": nrm(ks[4], (DEPTH, IN_COLS), 0.02),
        "conv_w": nrm(ks[5], (DEPTH, CONV_KERNEL, CONV_CH), CONV_KERNEL ** -0.5),
        "conv_b": nrm(ks[6], (DEPTH, CONV_CH), 0.02),
        "conv_ln_g": 1.0 + nrm(ks[7], (DEPTH, CONV_CH), 0.02),
        "conv_ln_b": nrm(ks[8], (DEPTH, CONV_CH), 0.02),
        "sinks": nrm(ks[9], (DEPTH, N_Q_HEADS), 0.5),
        "w_out": nrm(ks[10], (DEPTH, MIX_WIDTH, d), MIX_WIDTH ** -0.5),
        "b_out": nrm(ks[11], (DEPTH, d), 0.02),
        "ffn_w_gate": nrm(ks[12], (N_DENSE, d, D_FF), d ** -0.5),
        "ffn_w_up": nrm(ks[13], (N_DENSE, d, D_FF), d ** -0.5),
        "ffn_w_down": nrm(ks[14], (N_DENSE, D_FF, d), D_FF ** -0.5),
        "moe_router": nrm(ks[15], (N_MOE, d, N_EXPERTS), d ** -0.5),
        "moe_w_gate": nrm(ks[16], (N_MOE, N_EXPERTS, d, D_FF_EXPERT), d ** -0.5),
        "moe_w_up": nrm(ks[17], (N_MOE, N_EXPERTS, d, D_FF_EXPERT), d ** -0.5),
        "moe_w_down": nrm(ks[18], (N_MOE, N_EXPERTS, D_FF_EXPERT, d), D_FF_EXPERT ** -0.5),
        "final_norm": 1.0 + nrm(ks[19], (d,), 0.02),
    }


def reference(x, attn_norm, ffn_norm, w_in, b_in, conv_w, conv_b, conv_ln_g, conv_ln_b,
              sinks, w_out, b_out, ffn_w_gate, ffn_w_up, ffn_w_down, moe_router,
              moe_w_gate, moe_w_up, moe_w_down, final_norm):
    for l in range(DEPTH):
        h = _rmsnorm(x, attn_norm[l])
        x = x + _mixer(h, w_in[l], b_in[l], conv_w[l], conv_b[l], conv_ln_g[l],
                       conv_ln_b[l], sinks[l], w_out[l], b_out[l])
        h = _rmsnorm(x, ffn_norm[l])
        if l % 2 == 0:
            i = l // 2
            x = x + _swiglu(h, ffn_w_gate[i], ffn_w_up[i], ffn_w_down[i])
        else:
            i = l // 2
            x = x + _moe(h, moe_router[i], moe_w_gate[i], moe_w_up[i], moe_w_down[i])
    return _rmsnorm(x, final_norm)
```

```python
import contextlib
import numpy as np
import concourse.bass as bass
import concourse.mybir as mybir
from concourse.bass_utils import run_bass_kernel_spmd

F32 = mybir.dt.float32
BF16 = mybir.dt.bfloat16
AF = mybir.ActivationFunctionType
ALU = mybir.AluOpType
AX = mybir.AxisListType

ENGS = ("pe", "act", "dve", "pool", "sp")
SAME_ENGINE_SYNC = True

D = 1024
NCH = 8
IN_COLS = 1792
CONV_K = 31
D_FF = 2816
D_FFE = 3584
NEXP = 8
EPS = 1e-5
HALO_BLKS = 2
N_CORES = 8


class Instr:
    __slots__ = ("eng", "idx", "fn", "waits", "lane", "lane_ord", "needs_inc", "inc_count", "clock", "region")


class Prog:
    def __init__(self, nc):
        self.nc = nc
        self.streams = {e: [] for e in ENGS}
        self.last_w = {}
        self.readers = {}
        self.clock = {e: {} for e in ENGS}
        self.lane_n = {}
        self.lane_last = {}
        self.stack = contextlib.ExitStack()
        self.cur_region = None
        self.regions = []
        self.markers = {}
        self.nbar = 0

    def barrier(self):
        bid = self.nbar
        self.nbar += 1
        for e in ENGS:
            fn, reads, writes, lane = self.markers[e]
            self.op(e, fn, reads=list(reads), writes=list(writes) + [("bar", bid, e)], lane=lane)
        for e in ENGS:
            self.op(e, None, reads=[("bar", bid, e2) for e2 in ENGS if e2 != e])

    def begin_region(self, cond_ap, thresh):
        self.barrier()
        self._snap = {e: dict(self.clock[e]) for e in ENGS}
        self.regions.append((cond_ap, thresh))
        self.cur_region = len(self.regions) - 1

    def end_region(self):
        self.cur_region = None
        for e in ENGS:
            self.clock[e] = self._snap[e]
        self.barrier()

    def sb(self, name, shape, dtype):
        return self.stack.enter_context(self.nc.sbuf_tensor("sb_" + name, list(shape), dtype))

    def ps(self, name, shape, dtype):
        return self.stack.enter_context(self.nc.psum_tensor("ps_" + name, list(shape), dtype))

    def _dom(self, p):
        return ("L", p.lane) if p.lane is not None else p.eng

    def _ord(self, p):
        return p.lane_ord if p.lane is not None else p.idx + 1

    def op(self, eng, fn, reads=(), writes=(), lane=None):
        ins = Instr()
        ins.eng = eng
        ins.fn = fn
        ins.lane = lane
        ins.needs_inc = False
        ins.inc_count = 0
        ins.region = self.cur_region
        st = self.streams[eng]
        ins.idx = len(st)
        deps = []
        for k in reads:
            w = self.last_w.get(k)
            if w is not None:
                deps.append((w, True))
        for k in writes:
            w = self.last_w.get(k)
            if w is not None:
                deps.append((w, True))
            rd = self.readers.get(k)
            if rd:
                for r in rd.values():
                    deps.append((r, False))
        if lane is not None:
            n = self.lane_n.get(lane, 0) + 1
            self.lane_n[lane] = n
            ins.lane_ord = n
            prev = self.lane_last.get(lane)
            if prev is not None:
                deps.append((prev, True))
            self.lane_last[lane] = ins
        else:
            ins.lane_ord = 0
        clk = self.clock[eng]
        waits = []
        for p, raw in deps:
            if p is ins:
                continue
            d = self._dom(p)
            o = self._ord(p)
            if p.lane is None and p.eng == eng:
                if not (SAME_ENGINE_SYNC and raw) or eng in ("pe", "sp"):
                    continue
            if clk.get(d, 0) >= o:
                continue
            waits.append(p)
            for dd, oo in p.clock.items():
                if clk.get(dd, 0) < oo:
                    clk[dd] = oo
            if p.lane is None:
                p.needs_inc = True
        ins.waits = waits
        c = dict(clk)
        c[self._dom(ins)] = self._ord(ins)
        ins.clock = c
        dom = self._dom(ins)
        for k in reads:
            rd = self.readers.get(k)
            if rd is None:
                rd = self.readers[k] = {}
            rd[dom] = ins
        for k in writes:
            self.last_w[k] = ins
            self.readers[k] = {}
        st.append(ins)
        return ins

    def retire(self, old_keys, new_keys):
        pend = {}

        def add(p):
            d = self._dom(p)
            q = pend.get(d)
            if q is None or self._ord(q) < self._ord(p):
                pend[d] = p
        for k in old_keys:
            w = self.last_w.pop(k, None)
            if w is not None:
                add(w)
            rd = self.readers.pop(k, None)
            if rd:
                for p in rd.values():
                    add(p)
        for k in new_keys:
            self.last_w[k] = None
            self.readers[k] = dict(pend)

    def emit(self, final_lanes=()):
        nc = self.nc
        sems = {e: self.stack.enter_context(nc.semaphore("s_" + e)) for e in ENGS}
        lane_sems = {}
        for i, l in enumerate(self.lane_n):
            lane_sems[l] = self.stack.enter_context(nc.semaphore("l%d" % i))
        for e in ENGS:
            c = 0
            for ins in self.streams[e]:
                if ins.needs_inc:
                    c += 1
                ins.inc_count = c

        def run(e, h):
            stream = self.streams[e]
            reg = [None]
            state = {"cur": None, "guard": None, "first": 0}

            def open_region(R, i0):
                if reg[0] is None:
                    reg[0] = h.alloc_register("creg_" + e)
                cond_ap, thresh = self.regions[R]
                h.reg_load(reg[0], cond_ap)
                g = h.If(h.snap(reg[0]) > thresh)
                g.__enter__()
                state["guard"] = g
                state["first"] = i0

            def close_region(R, i1):
                state["guard"].__exit__(None, None, None)
                body = stream[state["first"]:i1]
                k = sum(1 for x in body if x.needs_inc and x.lane is None)
                pre = stream[state["first"] - 1].inc_count if state["first"] > 0 else 0
                lanes = {}
                for x in body:
                    if x.lane is not None:
                        d = lanes.setdefault(x.lane, [x.lane_ord - 1, 0])
                        d[1] += 1
                if k > 0 or lanes:
                    with h.Else():
                        if k > 0:
                            h.wait_ge(sems[e], pre)
                            h.sem_inc(sems[e], k)
                        for l, (pl, kl) in lanes.items():
                            h.wait_ge(lane_sems[l], 16 * pl)
                            h.sem_inc(lane_sems[l], 16 * kl)

            for i, ins in enumerate(stream):
                if ins.region != state["cur"]:
                    if state["cur"] is not None:
                        close_region(state["cur"], i)
                    if ins.region is not None:
                        open_region(ins.region, i)
                    state["cur"] = ins.region
                for p in ins.waits:
                    if p.lane is not None:
                        h.wait_ge(lane_sems[p.lane], 16 * p.lane_ord)
                    else:
                        h.wait_ge(sems[p.eng], p.inc_count)
                if ins.fn is None:
                    continue
                bi = ins.fn(h)
                if ins.lane is not None:
                    bi.then_inc(lane_sems[ins.lane], 16)
                elif ins.needs_inc:
                    bi.then_inc(sems[e], 1)
            if state["cur"] is not None:
                close_region(state["cur"], len(stream))
            if e == "sp":
                for l in final_lanes:
                    h.wait_ge(lane_sems[l], 16 * self.lane_n[l])

        with nc.Block() as block:
            @block.tensor
            def _(h):
                run("pe", h)

            @block.scalar
            def _(h):
                run("act", h)

            @block.vector
            def _(h):
                run("dve", h)

            @block.gpsimd
            def _(h):
                run("pool", h)

            @block.sync
            def _(h):
                run("sp", h)


def build_program(n_st=4, nb=8, cap=320, sparse=True):
    nc = bass.Bass("TRN2", target_bir_lowering=False)
    ntok = (HALO_BLKS + n_st * nb) * 128
    dr = {}

    def din(name, shape):
        dr[name] = nc.dram_tensor(name, list(shape), F32, kind="ExternalInput").ap()
        return dr[name]

    x_d = din("x", [ntok, D])
    flag_d = din("flag", [128, 1])
    attn_norm = din("attn_norm", [2, D])
    ffn_norm = din("ffn_norm", [2, D])
    w_in = din("w_in", [2, D, IN_COLS])
    b_in = din("b_in", [2, IN_COLS])
    conv_w = din("conv_w", [2, CONV_K, 512])
    conv_b = din("conv_b", [2, 512])
    conv_ln_g = din("conv_ln_g", [2, 512])
    conv_ln_b = din("conv_ln_b", [2, 512])
    sinks = din("sinks", [2, 8])
    w_out = din("w_out", [2, D, D])
    b_out = din("b_out", [2, D])
    ffn_wg = din("ffn_w_gate", [1, D, D_FF])
    ffn_wu = din("ffn_w_up", [1, D, D_FF])
    ffn_wd = din("ffn_w_down", [1, D_FF, D])
    moe_router = din("moe_router", [1, D, NEXP])
    moe_wg = din("moe_w_gate", [1, NEXP, D, D_FFE])
    moe_wu = din("moe_w_up", [1, NEXP, D, D_FFE])
    moe_wd = din("moe_w_down", [1, NEXP, D_FFE, D])
    final_norm = din("final_norm", [D])
    out_d = nc.dram_tensor("out", [n_st * nb * 128, D], F32, kind="ExternalOutput").ap()

    P = Prog(nc)
    op = P.op
    uid = [0]

    def lane_name(s):
        return s

    NBMAX = max(nb, HALO_BLKS)
    x_sb = P.sb("x_sb", [128, NBMAX, D], F32)
    hT = P.sb("hT", [128, NCH, NBMAX * 128], BF16)
    hT32 = P.sb("hT32", [128, NCH, 128], F32)
    hn_t = P.sb("hn", [128, 2, D], F32)
    hn = [hn_t[:, i, :] for i in range(2)]
    sqj = P.sb("sqj", [128, D], BF16)
    stat = P.sb("stat", [128, 8], F32)
    wdbuf = P.sb("wdbuf", [128, 14336], BF16)
    gubuf = P.sb("gubuf", [128, 6 * 2048], BF16)
    arena = P.sb("arena", [128, 14336], BF16)
    arena32 = arena.bitcast(F32)
    sgs = [P.sb("sgs%d" % i, [128, 512], BF16) for i in range(2)]
    SW = 512
    qT = P.sb("qT", [128, 4, SW], BF16)
    kT = P.sb("kT", [128, 128 + SW], BF16)
    vaug = P.sb("vaug", [128, 1 + SW // 128, 2, 65], BF16)
    yT = P.sb("yT", [128, NCH, SW], BF16)
    lnt = [P.sb("lnt%d" % i, [128, SW], F32) for i in range(2)]
    sgm = [P.sb("sgm%d" % i, [128, 512], F32) for i in range(2)]
    eT = [P.sb("eT%d" % i, [128, 512], BF16) for i in range(2)]
    pT = [P.sb("pT%d" % i, [128, 512], BF16) for i in range(8)]
    o_sb = P.sb("o_sb", [128, 512], F32)
    dent = P.sb("dent", [128, 16], F32)
    utail = P.sb("utail", [128, 2, 4, 30], BF16)
    NDG = 12
    dg = P.sb("dg", [128, NDG, 128], BF16)
    kprev = P.sb("kprev", [128, 2, 128], BF16)
    vprev = P.sb("vprev", [128, 2, 2, 65], BF16)
    ident = P.sb("ident", [128, 128], F32)
    ones32 = P.sb("ones32", [128, 128], F32)
    maskp = P.sb("maskp", [128, 128], BF16)
    maskc = P.sb("maskc", [128, 128], BF16)
    gA = P.sb("gA", [128, 2, 8], F32)
    gF = P.sb("gF", [128, 2, 8], F32)
    binT = P.sb("binT", [128, 2, 14], F32)
    bv_bc = P.sb("bv_bc", [128, 2, 128], F32)
    cw = P.sb("cw", [128, 2, 4, 32], F32)
    cb = P.sb("cb", [128, 2, 4], F32)
    lg = P.sb("lg", [128, 2, 4], F32)
    lb = P.sb("lb", [128, 2, 4], F32)
    esink = P.sb("esink", [128, 2, 8], F32)
    bo_bc = P.sb("bo_bc", [128, D], F32)
    wr = P.sb("wr", [128, 8, 8], F32)
    flag = P.sb("flag", [128, 1], F32)
    gates = P.sb("gates", [128, NBMAX, 8], F32)
    rt = P.sb("rt", [128, 64], F32)
    ost_t = P.sb("ost", [128, 2, D], F32)
    ost = [ost_t[:, i, :] for i in range(2)]

    I32 = mybir.dt.int32
    CAP = cap
    NT = (CAP + 127) // 128
    TW = [min(128, CAP - 128 * i) for i in range(NT)]
    ident_bf = P.sb("ident_bf", [128, 128], BF16)
    ustrict = P.sb("ustrict", [128, 128], F32)
    iota_row = P.sb("iota_row", [128, CAP], F32)
    mk = P.sb("mk", [128, 8], F32)
    rmask = P.sb("rmask", [128, 64], F32)
    rtot = P.sb("rtot", [128, 64], F32)
    roff = P.sb("roff", [128, 64], F32)
    rpos = P.sb("rpos", [128, 64], F32)
    rposr = P.sb("rposr", [128, 64], F32)
    ghi = P.sb("ghi", [128, 64], BF16)
    g2 = P.sb("g2", [128, 64, 2], BF16)
    gpos = P.sb("gpos", [128, 4], F32)
    ncnt = P.sb("ncnt", [128, 8], F32)
    cnt_i = P.sb("cnt_i", [128, 1], I32)
    S_v = ost_t.bitcast(BF16)[:, :, :].rearrange("p a n -> p (a n)")[:, 0:8 * CAP].rearrange("p (b n) -> p b n", n=CAP)
    G_v = hn_t.bitcast(BF16)[:, :, :].rearrange("p a n -> p (a n)")[:, 0:NT * 1024].rearrange("p (i n) -> p i n", n=1024)
    pq = [P.ps("pq%d" % i, [128, 1024], F32) for i in range(4)]

    def bank(i):
        return pq[i // 2][:, (i % 2) * 512:(i % 2) * 512 + 512]

    def bk(i):
        return ("pb", i)

    cw_raw = x_sb[0:31, 0, :].rearrange("p (l c) -> p l c", c=512)
    iota_i = x_sb[:, 1, 0:CAP].bitcast(I32)
    actT = arena[:, :].rearrange("p (j n) -> p j n", n=1024)
    UW = 30 + SW
    uT = arena[:, 0:4 * UW].rearrange("p (c n) -> p c n", n=UW)
    Y0 = (2 * UW + 31) // 32 * 32
    yconv = arena32[:, Y0:Y0 + 4 * SW].rearrange("p (c n) -> p c n", n=SW)
    ysq = arena32[:, Y0 + 4 * SW:Y0 + 8 * SW].rearrange("p (c n) -> p c n", n=SW)
    assert Y0 + 8 * SW <= 7168
    actTe = arena[:, 0:14 * CAP].rearrange("p (j n) -> p j n", n=CAP)
    hTe_v = arena[:, 14 * CAP:22 * CAP].rearrange("p (c n) -> p c n", n=CAP)
    ye_v = arena[:, 22 * CAP:22 * CAP + NT * 1024].rearrange("p (i n) -> p i n", n=1024)
    assert 22 * CAP + NT * 1024 <= 14336
    SPARSE_KEYS = [("actTe", j) for j in range(14)] + ["hTe"] + [("ye", i, hf) for i in range(NT) for hf in range(2)]
    hT_flat = hT[:, :, :].rearrange("p c n -> p (c n)")

    def htok(b):
        return hT_flat[:, b * D:(b + 1) * D]
    ACT_KEYS = [("actT", j) for j in range(14)]
    MIX_KEYS = ["uT", "yconv", "ysq"]
    win_v = wdbuf[:, :].rearrange("p (c n) -> p c n", n=IN_COLS)
    wout_v = gubuf[:, 0:8192].rearrange("p (c n) -> p c n", n=D)
    WD_KEYS = [("wd", 0), ("wd", 1)]
    GU_KEYS = [("gu", s) for s in range(6)]

    def wd_v(h):
        return wdbuf[:, h * 7168:(h + 1) * 7168].rearrange("p (j n) -> p j n", n=512)

    def gu_v(s):
        return gubuf[:, s * 2048:(s + 1) * 2048].rearrange("p (c n) -> p c n", n=256)

    def small_dma(eng, out_ap, in_ap, writes, lane):
        def fn(h):
            with nc.allow_non_contiguous_dma(reason="tiny param load"):
                return h.dma_start(out=out_ap, in_=in_ap)
        op(eng, fn, writes=writes, lane=lane)

    op("pool", lambda h: h.memset(ident[:], 0.0), writes=["ident"])
    op("pool", lambda h: h.affine_select(out=ident[:], in_=ident[:], pattern=[[-1, 128]],
                                        compare_op=ALU.not_equal, fill=1.0, base=0, channel_multiplier=1),
       reads=["ident"], writes=["ident"])
    op("pool", lambda h: h.memset(ones32[:], 1.0), writes=["ones32"])
    op("pool", lambda h: h.memset(maskp[:], 1.0), writes=["maskp"])
    op("pool", lambda h: h.memset(maskc[:], 1.0), writes=["maskc"])
    op("pool", lambda h: h.affine_select(out=maskp[:], in_=maskp[:], pattern=[[-1, 128]],
                                        compare_op=ALU.is_gt, fill=0.0, base=0, channel_multiplier=1),
       reads=["maskp"], writes=["maskp"])
    op("pool", lambda h: h.affine_select(out=maskc[:], in_=maskc[:], pattern=[[1, 128]],
                                        compare_op=ALU.is_ge, fill=0.0, base=0, channel_multiplier=-1),
       reads=["maskc"], writes=["maskc"])
    op("pool", lambda h: h.memset(vaug[:], 1.0), writes=["vaug"])
    op("pool", lambda h: h.memset(vprev[:], 0.0), writes=["vprev"])
    op("pool", lambda h: h.memset(vprev[:, :, :, 64:65], 1.0), reads=["vprev"], writes=["vprev"])
    op("pool", lambda h: h.memset(kprev[:], 0.0), writes=["kprev"])
    op("pool", lambda h: h.memset(utail[:], 0.0), writes=["utail"])
    op("pool", lambda h: h.memset(cw[:], 0.0), writes=["cw"])

    op("pool", lambda h: h.tensor_copy(out=ident_bf[:], in_=ident[:]), reads=["ident"], writes=["ident_bf"])
    op("pool", lambda h: h.memset(ustrict[:], 1.0), writes=["ustrict"])
    op("pool", lambda h: h.affine_select(out=ustrict[:], in_=ustrict[:], pattern=[[1, 128]],
                                        compare_op=ALU.is_gt, fill=0.0, base=0, channel_multiplier=-1),
       reads=["ustrict"], writes=["ustrict"])
    op("pool", lambda h: h.iota(iota_i, pattern=[[1, CAP]], base=0, channel_multiplier=0), writes=[("x", 1)])
    op("pool", lambda h: h.tensor_copy(out=iota_row[:], in_=iota_i), reads=[("x", 1)], writes=["iota_row"])
    op("pool", lambda h: h.memset(mk[:], 0.0), writes=["mk0"])
    P.markers = {
        "pe": (lambda h: h.matmul(out=bank(7)[0:1, 0:1], lhsT=ones32[0:1, 0:1], rhs=ones32[0:1, 0:1], start=True, stop=True),
               ["ones32"], [bk(7)], None),
        "act": (lambda h: h.activation(out=mk[:, 0:1], in_=mk[:, 4:5], func=AF.Copy), ["mk0"], [("mk", "act")], None),
        "dve": (lambda h: h.memset(mk[:, 1:2], 0.0), ["mk0"], [("mk", "dve")], None),
        "pool": (lambda h: h.memset(mk[:, 2:3], 0.0), ["mk0"], [("mk", "pool")], None),
        "sp": (lambda h: h.dma_start(out=mk[:, 3:4], in_=mk[:, 5:6]), ["mk0"], [("mk", "sp")], ("bar", "sp")),
    }
    small_dma("sp", flag[:], flag_d, ["flag"], "c_flag")
    small_dma("sp", gA[:], attn_norm.rearrange("l (c p) -> p l c", p=128), ["gA"], "c_gA")
    small_dma("sp", gF[:], ffn_norm.rearrange("l (c p) -> p l c", p=128), ["gF"], "c_gF")
    small_dma("sp", binT[:], b_in.rearrange("l (c p) -> p l c", p=128), ["binT"], "c_binT")
    for l in range(2):
        small_dma("sp", bv_bc[:, l, :], b_in[l, 1664:1792].partition_broadcast(128), ["bv_bc%d" % l], "c_bv%d" % l)
        small_dma("sp", esink[:, l, :], sinks[l, :].partition_broadcast(128), ["esink%d" % l], "c_es%d" % l)
    small_dma("sp", cw_raw, conv_w.rearrange("l k c -> k l c"), [("x", 0)], "c_cwraw")
    small_dma("sp", cb[:], conv_b.rearrange("l (c p) -> p l c", p=128), ["cb"], "c_cb")
    small_dma("sp", lg[:], conv_ln_g.rearrange("l (c p) -> p l c", p=128), ["lg"], "c_lg")
    small_dma("sp", lb[:], conv_ln_b.rearrange("l (c p) -> p l c", p=128), ["lb"], "c_lb")
    small_dma("sp", wr[:], moe_router[0].rearrange("(c p) e -> p c e", p=128), ["wr"], "c_wr")
    op("act", lambda h: h.activation(out=esink[:], in_=esink[:], func=AF.Exp),
       reads=["esink0", "esink1"], writes=["esink"])
    for l in range(2):
        for c in range(4):
            op("pe", lambda h, l=l, c=c: h.transpose(out=bank(c)[:, 0:31], in_=cw_raw[0:31, l, c * 128:(c + 1) * 128],
                                                     identity=ident[0:31, 0:31]),
               reads=[("x", 0), "ident"], writes=[bk(c)])
            op("dve", lambda h, l=l, c=c: h.tensor_copy(out=cw[:, l, c, 0:31], in_=bank(c)[:, 0:31]),
               reads=[bk(c), "cw"], writes=[("cwT", l, c)])
    CW_KEYS = [("cwT", l, c) for l in range(2) for c in range(4)]

    pbrr = [0]
    dgc = [0]

    def next_bank():
        i = pbrr[0] % 8
        pbrr[0] += 1
        return i

    def rms_stage_a(b, gain_bc=None):
        s = b % 2
        ssc = stat[:, 4 * s:4 * s + 1]
        rsc = stat[:, 4 * s + 1:4 * s + 2]
        op("act", lambda h: h.activation(out=sqj[:], in_=x_sb[:, b, :], func=AF.Square, accum_out=ssc),
           reads=[("x", b)], writes=["sqj", ("stat", s)])
        op("act", lambda h: h.activation(out=rsc, in_=ssc, func=AF.Sqrt, bias=EPS, scale=1.0 / D),
           reads=[("stat", s)], writes=[("statr", s)])
        op("dve", lambda h: h.reciprocal(out=rsc, in_=rsc), reads=[("statr", s)], writes=[("statr", s)])
        if gain_bc is None:
            op("dve", lambda h: h.tensor_scalar(out=hn[s], in0=x_sb[:, b, :], scalar1=rsc, scalar2=None, op0=ALU.mult),
               reads=[("x", b), ("statr", s)], writes=[("hn", s)])
        else:
            op("dve", lambda h: h.scalar_tensor_tensor(out=hn[s], in0=x_sb[:, b, :], scalar=rsc, in1=gain_bc, op0=ALU.mult, op1=ALU.mult),
               reads=[("x", b), ("statr", s), "bo_bc"], writes=[("hn", s)])

    def rms_transposes(b):
        s = b % 2
        pt = pq[b % 2]
        for c in range(NCH):
            op("pe", lambda h, c=c: h.transpose(out=pt[:, c * 128:(c + 1) * 128], in_=hn[s][:, c * 128:(c + 1) * 128], identity=ident[:]),
               reads=[("hn", s), "ident"], writes=[bk(2 * (b % 2) + c // 4)])
        return pt[:, :].rearrange("p (c t) -> p c t", t=128)

    def rms_transpose(gT, l, nblk):
        rms_stage_a(0)
        for b in range(nblk):
            if b + 1 < nblk:
                rms_stage_a(b + 1)
            ptv = rms_transposes(b)
            gbc = gT[:, l, :].unsqueeze(2).to_broadcast([128, NCH, 128])
            op("dve", lambda h, b=b, ptv=ptv, gbc=gbc: h.tensor_tensor(out=hT[:, :, b * 128:(b + 1) * 128], in0=ptv, in1=gbc, op=ALU.mult),
               reads=[bk(2 * (b % 2)), bk(2 * (b % 2) + 1), "gA", "gF"], writes=[("hT", b)])

    Lall = P.sb("Lall", [128, NBMAX, 8], F32)
    rtb = P.sb("rtb", [128, 6, NBMAX, 8], F32)
    rts = P.sb("rts", [128, 6, NBMAX], F32)

    def router_gates_all(nblk):
        L = Lall[:, 0:nblk, :]
        mk1, L2, mk2, t1 = (rtb[:, i, 0:nblk, :] for i in range(4))
        m1, m2, dd, w1, w2 = (rts[:, i, 0:nblk] for i in range(5))

        def bc(v):
            return v.unsqueeze(2).to_broadcast([128, nblk, 8])
        K = "rt"
        LK = [("Lall", b) for b in range(nblk)]
        op("dve", lambda h: h.tensor_reduce(out=m1, in_=L, axis=AX.X, op=ALU.max), reads=LK, writes=[K])
        op("dve", lambda h: h.tensor_tensor(out=mk1, in0=L, in1=bc(m1), op=ALU.is_equal), reads=LK + [K], writes=[K])
        op("dve", lambda h: h.scalar_tensor_tensor(out=L2, in0=mk1, scalar=-1e30, in1=L, op0=ALU.mult, op1=ALU.add), reads=LK + [K], writes=[K])
        op("dve", lambda h: h.tensor_reduce(out=m2, in_=L2, axis=AX.X, op=ALU.max), reads=[K], writes=[K])
        op("dve", lambda h: h.tensor_tensor(out=mk2, in0=L2, in1=bc(m2), op=ALU.is_equal), reads=[K], writes=[K])
        op("dve", lambda h: h.tensor_tensor(out=dd, in0=m2, in1=m1, op=ALU.subtract), reads=[K], writes=[K])
        op("act", lambda h: h.activation(out=dd, in_=dd, func=AF.Exp), reads=[K], writes=[K])
        op("dve", lambda h: h.tensor_scalar(out=w1, in0=dd, scalar1=1.0, scalar2=None, op0=ALU.add), reads=[K], writes=[K])
        op("dve", lambda h: h.reciprocal(out=w1, in_=w1), reads=[K], writes=[K])
        op("dve", lambda h: h.tensor_tensor(out=w2, in0=dd, in1=w1, op=ALU.mult), reads=[K], writes=[K])
        op("dve", lambda h: h.tensor_tensor(out=mk1, in0=mk1, in1=bc(w1), op=ALU.mult), reads=[K], writes=[K])
        op("dve", lambda h: h.tensor_tensor(out=t1, in0=mk2, in1=bc(w2), op=ALU.mult), reads=[K], writes=[K])
        op("dve", lambda h: h.tensor_tensor(out=gates[:, 0:nblk, :], in0=mk1, in1=t1, op=ALU.add), reads=[K],
           writes=[("gates", b) for b in range(nblk)])

    def rms_moe(nblk):
        op("sp", lambda h: h.dma_start(out=bo_bc[:], in_=ffn_norm[1, :].partition_broadcast(128)), writes=["bo_bc"], lane="bo_bc")
        rms_stage_a(0, bo_bc[:])
        for b in range(nblk):
            if b + 1 < nblk:
                rms_stage_a(b + 1, bo_bc[:])
            s = b % 2
            op("pool", lambda h, b=b, s=s: h.tensor_copy(out=htok(b), in_=hn[s]), reads=[("hn", s)], writes=[("htok", b)])
            ptv = rms_transposes(b)
            op("act", lambda h, ptv=ptv: h.activation(out=hT32[:], in_=ptv, func=AF.Copy),
               reads=[bk(2 * (b % 2)), bk(2 * (b % 2) + 1)], writes=["hT32"])
            rb = 4 + (b % 2)
            for c in range(NCH):
                op("pe", lambda h, c=c, rb=rb: h.matmul(out=bank(rb)[:, 0:8], lhsT=hT32[:, c, :], rhs=wr[:, c, :],
                                                        start=(c == 0), stop=(c == NCH - 1)),
                   reads=["hT32", "wr"], writes=[bk(rb)])
            op("act", lambda h, b=b, rb=rb: h.activation(out=Lall[:, b, :], in_=bank(rb)[:, 0:8], func=AF.Copy),
               reads=[bk(rb)], writes=[("Lall", b)])
        router_gates_all(nblk)

    def moe_positions(nblk):
        n = nblk * 8
        gk = [("gates", b) for b in range(nblk)]
        gflat = gates[:, 0:nblk, :].rearrange("p b e -> p (b e)")
        op("dve", lambda h: h.tensor_scalar(out=rmask[:, 0:n], in0=gflat, scalar1=0.0, scalar2=None, op0=ALU.is_gt),
           reads=gk, writes=["rmask"])
        pb = 6
        op("pe", lambda h: h.matmul(out=bank(pb)[:, 0:n], lhsT=ustrict[:], rhs=rmask[:, 0:n], start=True, stop=True),
           reads=["rmask", "ustrict"], writes=[bk(pb)])
        op("pe", lambda h: h.matmul(out=bank(pb)[:, 64:64 + n], lhsT=ones32[:], rhs=rmask[:, 0:n], start=True, stop=True),
           reads=["rmask", "ones32"], writes=[bk(pb)])
        op("dve", lambda h: h.tensor_copy(out=rtot[:, 0:n], in_=bank(pb)[:, 64:64 + n]), reads=[bk(pb)], writes=["rtot"])
        op("dve", lambda h: h.memset(roff[:, 0:8], 0.0), writes=["roff"])
        for b in range(1, nblk):
            op("dve", lambda h, b=b: h.tensor_tensor(out=roff[:, 8 * b:8 * b + 8], in0=roff[:, 8 * b - 8:8 * b],
                                                     in1=rtot[:, 8 * b - 8:8 * b], op=ALU.add),
               reads=["roff", "rtot"], writes=["roff"])
        op("dve", lambda h: h.tensor_tensor(out=rpos[:, 0:n], in0=bank(pb)[:, 0:n], in1=roff[:, 0:n], op=ALU.add),
           reads=[bk(pb), "roff"], writes=["rpos"])
        op("dve", lambda h: h.scalar_tensor_tensor(out=rpos[:, 0:n], in0=rpos[:, 0:n], scalar=1.0, in1=rmask[:, 0:n],
                                                   op0=ALU.add, op1=ALU.mult), reads=["rpos", "rmask"], writes=["rpos"])
        op("dve", lambda h: h.tensor_scalar(out=rpos[:, 0:n], in0=rpos[:, 0:n], scalar1=-1.0, scalar2=None, op0=ALU.add),
           reads=["rpos"], writes=["rpos"])
        lb_ = 8 * (nblk - 1)
        op("dve", lambda h: h.tensor_tensor(out=ncnt[:, 0:8], in0=roff[:, lb_:lb_ + 8], in1=rtot[:, lb_:lb_ + 8], op=ALU.add),
           reads=["roff", "rtot"], writes=["ncnt"])
        op("dve", lambda h: h.tensor_reduce(out=rt[:, 48:49], in_=ncnt[:, 0:8], axis=AX.X, op=ALU.max), reads=["ncnt"], writes=["rtmax"])
        op("dve", lambda h: h.tensor_copy(out=cnt_i[:], in_=rt[:, 48:49]), reads=["rtmax"], writes=["cnt_i"])
        op("dve", lambda h: h.tensor_copy(out=ghi[:, 0:n], in_=gflat), reads=gk, writes=["ghi"])
        op("dve", lambda h: h.tensor_copy(out=g2[:, 0:n, 0], in_=ghi[:, 0:n]), reads=["ghi"], writes=["g2"])
        op("dve", lambda h: h.tensor_tensor(out=g2[:, 0:n, 1], in0=gflat, in1=ghi[:, 0:n], op=ALU.subtract),
           reads=gk + ["ghi", "g2"], writes=["g2"])

    def sparse_sbuild(e, nblk):
        for b in range(nblk):
            op("dve", lambda h, b=b: h.tensor_scalar(out=S_v[:, b, :], in0=iota_row[:], scalar1=rposr[:, 8 * b + e:8 * b + e + 1],
                                                     scalar2=None, op0=ALU.is_equal),
               reads=["rposr", "iota_row"], writes=["S"])

    def sparse_pre(e, nblk, gub, build_s=True):
        if build_s:
            sparse_sbuild(e, nblk)
        for c in range(NCH):
            bi = gub[0] % 4
            gub[0] += 1
            for b in range(nblk):
                op("pe", lambda h, bi=bi, b=b, c=c: h.matmul(out=bank(bi)[:, 0:CAP], lhsT=htok(b)[:, c * 128:(c + 1) * 128],
                                                             rhs=S_v[:, b, :], start=(b == 0), stop=(b == nblk - 1)),
                   reads=["S", ("htok", b)], writes=[bk(bi)])
            op("act", lambda h, bi=bi, c=c: h.activation(out=hTe_v[:, c, :], in_=bank(bi)[:, 0:CAP], func=AF.Copy),
               reads=[bk(bi)], writes=["hTe"])
        gub[0] += gub[0] % 2
        for i in range(NT):
            wi = TW[i]
            for b in range(nblk):
                op("pe", lambda h, i=i, b=b, wi=wi: h.matmul(out=bank(6)[0:wi, 2 * i:2 * i + 2], lhsT=S_v[:, b, i * 128:i * 128 + wi],
                                                             rhs=g2[:, 8 * b + e, :], start=(b == 0), stop=(b == nblk - 1)),
                   reads=["S", "g2"], writes=[bk(6)])
        for i in range(NT):
            wi = TW[i]
            g6 = bank(6)[0:wi, 2 * i:2 * i + 2]
            op("dve", lambda h, i=i, wi=wi, g6=g6: h.tensor_reduce(out=gpos[0:wi, i:i + 1], in_=g6, axis=AX.X, op=ALU.add),
               reads=[bk(6)], writes=["gpos"])
        pqb = pq[3].bitcast(BF16)
        for i in range(NT):
            wi = TW[i]
            for b in range(nblk):
                op("pe", lambda h, i=i, b=b, wi=wi: h.transpose(out=pqb[0:wi, 1024 + b * 128:1024 + (b + 1) * 128],
                                                                in_=S_v[:, b, i * 128:i * 128 + wi], identity=ident_bf[:]),
                   reads=["S", "ident_bf"], writes=[bk(7)])
            op("act", lambda h, i=i, wi=wi: h.activation(out=G_v[0:wi, i, 0:nblk * 128], in_=pqb[0:wi, 1024:1024 + nblk * 128], func=AF.Copy),
               reads=[bk(7)], writes=["G"])

    def sparse_scatter(nblk):
        for b in range(nblk):
            for half in range(2):
                bi = 4 + ((2 * b + half) % 2)
                for i in range(NT):
                    wi = TW[i]
                    op("pe", lambda h, bi=bi, i=i, b=b, half=half, wi=wi: h.matmul(
                        out=bank(bi)[:, :], lhsT=G_v[0:wi, i, b * 128:(b + 1) * 128], rhs=ye_v[0:wi, i, half * 512:(half + 1) * 512],
                        start=(i == 0), stop=(i == NT - 1)),
                       reads=["G", ("ye", i, half)], writes=[bk(bi)])
                xs = x_sb[:, b, half * 512:(half + 1) * 512]
                op("dve", lambda h, bi=bi, xs=xs: h.tensor_tensor(out=xs, in0=bank(bi)[:, :], in1=xs, op=ALU.add),
                   reads=[bk(bi), ("x", b)], writes=[("x", b)])

    def load_mixer_weights(l):
        for hh in range(2):
            op("pool", lambda h, hh=hh: h.dma_start(out=win_v[:, 4 * hh:4 * hh + 4, :],
                                                    in_=w_in[l, 512 * hh:512 * hh + 512, :].rearrange("(c p) n -> p c n", p=128)),
               writes=[("wd", hh)], lane=("wd", hh))
        for hh in range(2):
            op("pool", lambda h, hh=hh: h.dma_start(out=wout_v[:, 4 * hh:4 * hh + 4, :],
                                                    in_=w_out[l, 512 * hh:512 * hh + 512, :].rearrange("(c p) n -> p c n", p=128)),
               writes=[("gu", 2 * hh), ("gu", 2 * hh + 1)], lane=("gu", 2 * hh))
        op("sp", lambda h: h.dma_start(out=bo_bc[:], in_=b_out[l, :].partition_broadcast(128)), writes=["bo_bc"], lane="bo_bc")

    def mixer_sub(l, b0, nsb, full=True):
        W = nsb * 128
        c0 = b0 * 128
        WK = WD_KEYS
        op("pool", lambda h: h.tensor_copy(out=uT[:, :, 0:30], in_=utail[:, l, :, :]), reads=[("utail", l), "uT"], writes=["uT"])
        op("pool", lambda h: h.tensor_copy(out=kT[:, 0:128], in_=kprev[:, l, :]), reads=[("kprev", l), "kT"], writes=["kT"])
        op("pool", lambda h: h.tensor_copy(out=vaug[:, 0, :, :], in_=vprev[:, l, :, :]), reads=[("vprev", l), "vaug"], writes=["vaug"])
        if full:
            for b in range(b0, b0 + nsb):
                op("pool", lambda h, b=b: h.tensor_tensor(out=x_sb[:, b, :], in0=x_sb[:, b, :], in1=bo_bc[:], op=ALU.add),
                   reads=[("x", b), "bo_bc"], writes=[("x", b)])
        hkeys = [("hT", b) for b in range(b0, b0 + nsb)]
        for c in range(4):
            ia, ig = next_bank(), next_bank()
            for (m, bi) in ((c, ia), (4 + c, ig)):
                for k in range(NCH):
                    op("pe", lambda h, m=m, bi=bi, k=k: h.matmul(out=bank(bi)[:, 0:W], lhsT=win_v[:, k, m * 128:(m + 1) * 128],
                                                                 rhs=hT[:, k, c0:c0 + W], start=(k == 0), stop=(k == NCH - 1)),
                       reads=WK + hkeys, writes=[bk(bi)])
            s = c % 2
            op("act", lambda h, c=c, ig=ig, s=s: h.activation(out=sgm[s][:, 0:W], in_=bank(ig)[:, 0:W], func=AF.Sigmoid,
                                                             bias=binT[:, l, 4 + c:5 + c]),
               reads=[bk(ig), "binT"], writes=[("sgm", s)])
            op("dve", lambda h, c=c, ia=ia, s=s: h.scalar_tensor_tensor(out=uT[:, c, 30:30 + W], in0=bank(ia)[:, 0:W],
                                                                       scalar=binT[:, l, c:c + 1], in1=sgm[s][:, 0:W],
                                                                       op0=ALU.add, op1=ALU.mult),
               reads=[bk(ia), ("sgm", s), "binT", "uT"], writes=[("uTc", c)])
        UC = [("uTc", c) for c in range(4)]
        for j in range(4):
            bi = next_bank()
            for k in range(NCH):
                op("pe", lambda h, j=j, bi=bi, k=k: h.matmul(out=bank(bi)[:, 0:W], lhsT=win_v[:, k, (8 + j) * 128:(9 + j) * 128],
                                                             rhs=hT[:, k, c0:c0 + W], start=(k == 0), stop=(k == NCH - 1)),
                   reads=WK + hkeys, writes=[bk(bi)])
            op("act", lambda h, j=j, bi=bi: h.activation(out=qT[:, j, 0:W], in_=bank(bi)[:, 0:W], func=AF.Identity,
                                                         bias=binT[:, l, 8 + j:9 + j]),
               reads=[bk(bi), "binT"], writes=[("qT", j)])
        bi = next_bank()
        for k in range(NCH):
            op("pe", lambda h, bi=bi, k=k: h.matmul(out=bank(bi)[:, 0:W], lhsT=win_v[:, k, 1536:1664],
                                                    rhs=hT[:, k, c0:c0 + W], start=(k == 0), stop=(k == NCH - 1)),
               reads=WK + hkeys, writes=[bk(bi)])
        op("act", lambda h, bi=bi: h.activation(out=kT[:, 128:128 + W], in_=bank(bi)[:, 0:W], func=AF.Identity,
                                                bias=binT[:, l, 12:13]),
           reads=[bk(bi), "binT", "kT"], writes=["kTn"])
        for i in range(nsb):
            bi = next_bank()
            for k in range(NCH):
                op("pe", lambda h, bi=bi, k=k, i=i: h.matmul(out=bank(bi)[:, 0:128], lhsT=hT[:, k, c0 + i * 128:c0 + (i + 1) * 128],
                                                             rhs=win_v[:, k, 1664:1792], start=(k == 0), stop=(k == NCH - 1)),
                   reads=WK + hkeys, writes=[bk(bi)])
            op("dve", lambda h, bi=bi, i=i: h.tensor_tensor(out=vaug[:, 1 + i, :, 0:64],
                                                            in0=bank(bi)[:, 0:128].rearrange("p (a d) -> p a d", d=64),
                                                            in1=bv_bc[:, l, :].rearrange("p (a d) -> p a d", d=64), op=ALU.add),
               reads=[bk(bi), "bv_bc%d" % l, "vaug"], writes=[("vaug", 1 + i)])
        VK = [("vaug", 1 + i) for i in range(nsb)]
        op("pool", lambda h: h.tensor_copy(out=utail[:, l, :, :], in_=uT[:, :, W:W + 30]), reads=UC + ["uT"], writes=[("utail", l)])
        op("pool", lambda h: h.tensor_copy(out=kprev[:, l, :], in_=kT[:, W:W + 128]), reads=["kTn", "kT"], writes=[("kprev", l)])
        op("pool", lambda h: h.tensor_copy(out=vprev[:, l, :, :], in_=vaug[:, nsb, :, :]), reads=VK + ["vaug"], writes=[("vprev", l)])
        if not full:
            return
        for c in range(4):
            bi = next_bank()
            for k in range(CONV_K):
                sl = dgc[0] % NDG
                dgc[0] += 1
                de = ("act", "dve")[dgc[0] % 2]
                if de == "act":
                    op("act", lambda h, c=c, k=k, sl=sl: h.activation(out=dg[:, sl, :], in_=ident_bf[:], func=AF.Copy,
                                                                     scale=cw[:, l, c, k:k + 1]),
                       reads=["ident_bf"] + CW_KEYS, writes=[("dg", sl)])
                else:
                    op(de, lambda h, c=c, k=k, sl=sl: h.tensor_scalar(out=dg[:, sl, :], in0=ident_bf[:], scalar1=cw[:, l, c, k:k + 1],
                                                                      scalar2=None, op0=ALU.mult),
                       reads=["ident_bf"] + CW_KEYS, writes=[("dg", sl)])
                op("pe", lambda h, c=c, k=k, sl=sl, bi=bi: h.matmul(out=bank(bi)[:, 0:W], lhsT=dg[:, sl, :], rhs=uT[:, c, k:k + W],
                                                                    start=(k == 0), stop=(k == CONV_K - 1)),
                   reads=[("dg", sl), ("uTc", c), "uT"], writes=[bk(bi)])
            op("act", lambda h, c=c, bi=bi: h.activation(out=yconv[:, c, 0:W], in_=bank(bi)[:, 0:W], func=AF.Identity,
                                                         bias=cb[:, l, c:c + 1]),
               reads=[bk(bi), "cb"], writes=[("yc", c)])
        YC = [("yc", c) for c in range(4)]
        op("act", lambda h: h.activation(out=ysq[:, :, 0:W], in_=yconv[:, :, 0:W], func=AF.Square), reads=YC, writes=["ysq"])
        b1, b2 = next_bank(), next_bank()
        for c in range(4):
            op("pe", lambda h, c=c: h.matmul(out=bank(b1)[:, 0:W], lhsT=ones32[:], rhs=yconv[:, c, 0:W], start=(c == 0), stop=(c == 3)),
               reads=YC + ["ones32"], writes=[bk(b1)])
        for c in range(4):
            op("pe", lambda h, c=c: h.matmul(out=bank(b2)[:, 0:W], lhsT=ones32[:], rhs=ysq[:, c, 0:W], start=(c == 0), stop=(c == 3)),
               reads=["ysq", "ones32"], writes=[bk(b2)])
        mean, var = lnt[0], lnt[1]
        op("act", lambda h: h.activation(out=mean[:, 0:W], in_=bank(b1)[:, 0:W], func=AF.Copy, scale=1.0 / 512), reads=[bk(b1)], writes=["mean"])
        op("dve", lambda h: h.tensor_tensor(out=var[:, 0:W], in0=mean[:, 0:W], in1=mean[:, 0:W], op=ALU.mult), reads=["mean"], writes=["var"])
        op("dve", lambda h: h.scalar_tensor_tensor(out=var[:, 0:W], in0=bank(b2)[:, 0:W], scalar=1.0 / 512, in1=var[:, 0:W],
                                                   op0=ALU.mult, op1=ALU.subtract), reads=[bk(b2), "var"], writes=["var"])
        op("act", lambda h: h.activation(out=var[:, 0:W], in_=var[:, 0:W], func=AF.Sqrt, bias=EPS), reads=["var"], writes=["var"])
        op("dve", lambda h: h.reciprocal(out=var[:, 0:W], in_=var[:, 0:W]), reads=["var"], writes=["var"])
        op("dve", lambda h: h.tensor_tensor(out=yconv[:, :, 0:W], in0=yconv[:, :, 0:W],
                                            in1=mean[:, 0:W].unsqueeze(1).to_broadcast([128, 4, W]), op=ALU.subtract),
           reads=YC + ["mean"], writes=YC)
        op("dve", lambda h: h.tensor_tensor(out=yconv[:, :, 0:W], in0=yconv[:, :, 0:W],
                                            in1=var[:, 0:W].unsqueeze(1).to_broadcast([128, 4, W]), op=ALU.mult),
           reads=YC + ["var"], writes=YC)
        for c in range(4):
            op("act", lambda h, c=c: h.activation(out=yT[:, c, 0:W], in_=yconv[:, c, 0:W], func=AF.Silu,
                                                  bias=lb[:, l, c:c + 1], scale=lg[:, l, c:c + 1]),
               reads=YC + ["lg", "lb"], writes=[("yT", c)])
        def att_scores(i):
            for kv in range(2):
                r0 = 64 * kv
                for kb in range(2):
                    bi = next_bank()
                    while bi >= 6:
                        bi = next_bank()
                    op("pe", lambda h, bi=bi, kb=kb, r0=r0: h.matmul(
                        out=bank(bi)[:, :], lhsT=kT[r0:r0 + 64, (i + kb) * 128:(i + kb + 1) * 128],
                        rhs=qT[r0:r0 + 64, :, i * 128:(i + 1) * 128], start=True, stop=True),
                       reads=["kT", "kTn"] + [("qT", j) for j in range(4)], writes=[bk(bi)])
                    es = kb
                    ps_ = 4 * (i % 2) + 2 * kv + kb
                    op("act", lambda h, bi=bi, es=es: h.activation(out=eT[es][:], in_=bank(bi)[:, :], func=AF.Exp, scale=0.125),
                       reads=[bk(bi)], writes=[("eT", es)])
                    mk_ = maskp if kb == 0 else maskc
                    op("pool", lambda h, es=es, ps_=ps_, mk_=mk_: h.tensor_tensor(
                        out=pT[ps_][:, :].rearrange("p (j q) -> p j q", q=128), in0=eT[es][:, :].rearrange("p (j q) -> p j q", q=128),
                        in1=mk_[:, :].unsqueeze(1).to_broadcast([128, 4, 128]), op=ALU.mult),
                       reads=[("eT", es), "maskp", "maskc"], writes=[("pT", ps_)])

        def att_pv(i):
            po = pq[3]
            for kv in range(2):
                for j in range(4):
                    hh = kv * 4 + j
                    for kb in range(2):
                        ps_ = 4 * (i % 2) + 2 * kv + kb
                        op("pe", lambda h, hh=hh, kv=kv, j=j, kb=kb, ps_=ps_: h.matmul(
                            out=po[:, hh * 128:hh * 128 + 65], lhsT=pT[ps_][:, j * 128:(j + 1) * 128],
                            rhs=vaug[:, i + kb, kv, :], start=(kb == 0), stop=(kb == 1)),
                           reads=[("pT", ps_), "vaug"] + VK, writes=[bk(6 + hh // 4)])
            pov = po[:, :].rearrange("p (h d) -> p h d", d=128)
            op("dve", lambda h: h.tensor_tensor(out=dent[:, 0:8], in0=pov[:, :, 64], in1=esink[:, l, :], op=ALU.add),
               reads=[bk(6), bk(7), "esink"], writes=["dent"])
            op("dve", lambda h: h.reciprocal(out=dent[:, 8:16], in_=dent[:, 0:8]), reads=["dent"], writes=["dent2"])
            op("dve", lambda h: h.tensor_tensor(out=o_sb[:, :].rearrange("p (h d) -> p h d", d=64), in0=pov[:, :, 0:64],
                                                in1=dent[:, 8:16].unsqueeze(2).to_broadcast([128, 8, 64]), op=ALU.mult),
               reads=[bk(6), bk(7), "dent2"], writes=["o_sb"])
            bi = next_bank()
            while bi >= 6:
                bi = next_bank()
            for j in range(4):
                op("pe", lambda h, bi=bi, j=j: h.transpose(out=bank(bi)[:, j * 128:(j + 1) * 128], in_=o_sb[:, j * 128:(j + 1) * 128],
                                                           identity=ident[:]),
                   reads=["o_sb", "ident"], writes=[bk(bi)])
            op("act", lambda h, bi=bi: h.activation(out=yT[:, 4:8, i * 128:(i + 1) * 128],
                                                    in_=bank(bi)[:, :].rearrange("p (j q) -> p j q", q=128), func=AF.Copy),
               reads=[bk(bi)], writes=[("yTa", i)])

        att_scores(0)
        for i in range(nsb):
            if i + 1 < nsb:
                att_scores(i + 1)
            att_pv(i)
        GK = [("gu", s) for s in range(4)]
        for i in range(nsb):
            b = b0 + i
            for half in range(2):
                bi = next_bank()
                for c in range(NCH):
                    op("pe", lambda h, bi=bi, c=c, i=i, half=half: h.matmul(
                        out=bank(bi)[:, :], lhsT=yT[:, c, i * 128:(i + 1) * 128], rhs=wout_v[:, c, half * 512:(half + 1) * 512],
                        start=(c == 0), stop=(c == NCH - 1)),
                       reads=GK + [("yT", c) for c in range(4)] + [("yTa", i)], writes=[bk(bi)])
                op("dve", lambda h, bi=bi, b=b, half=half: h.tensor_tensor(out=x_sb[:, b, half * 512:(half + 1) * 512],
                                                                           in0=bank(bi)[:, :], in1=x_sb[:, b, half * 512:(half + 1) * 512],
                                                                           op=ALU.add),
                   reads=[bk(bi), ("x", b)], writes=[("x", b)])

    def swiglu_phase(units, nblk, sparse_mode=False):
        T = CAP if sparse_mode else nblk * 128
        ncg = (T + 511) // 512
        groups = []
        for ui, (wg, wu, wd, F, gc) in enumerate(units):
            nchunk = F // 128
            g0 = (nchunk + 1) // 2
            groups.append((ui, 0, g0))
            groups.append((ui, g0, nchunk - g0))
        pairs = []
        for gi, (ui, cs, n) in enumerate(groups):
            j = 0
            while j < n:
                m = min(2, n - j)
                pairs.append((gi, cs + j, m))
                j += m
        NSLOT = 3

        def load_pair(pi):
            gi, cs, m = pairs[pi]
            ui = groups[gi][0]
            wg, wu = units[ui][0], units[ui][1]
            s = pi % NSLOT
            for (w, off) in ((wg, 0), (wu, 1)):
                slot = 2 * s + off
                op("pool", lambda h, w=w, slot=slot, cs=cs, m=m: h.dma_start(
                    out=gu_v(slot)[:, :, 0:128 * m], in_=w[:, cs * 128:(cs + m) * 128].rearrange("(c p) n -> p c n", p=128)),
                   writes=[("gu", slot)], lane=("gu", slot))

        def load_wd(gi, half):
            ui, cs, n = groups[gi]
            wd = units[ui][2]
            op("pool", lambda h: h.dma_start(out=wd_v(half)[:, 0:n, :],
                                             in_=wd[cs * 128:(cs + n) * 128, half * 512:(half + 1) * 512].rearrange("(j p) n -> p j n", p=128)),
               writes=[("wd", half)], lane=("wd", half))

        LOOK = 2
        for pi in range(min(LOOK, len(pairs))):
            load_pair(pi)
        load_wd(0, 0)
        load_wd(0, 1)
        hkeys = ["hTe"] if sparse_mode else [("hT", b) for b in range(nblk)]
        akey = "actTe" if sparse_mode else "actT"
        abuf = actTe if sparse_mode else actT
        gub = [0]
        pi = 0
        for gi, (ui, cs, n) in enumerate(groups):
            gc = units[ui][4]
            first_group = (gi % 2 == 0)
            if sparse_mode and first_group:
                sparse_pre(gc, nblk, gub, build_s=(gi == 0))
            while pi < len(pairs) and pairs[pi][0] == gi:
                _, pcs, m = pairs[pi]
                if pi + LOOK < len(pairs):
                    load_pair(pi + LOOK)
                s = pi % NSLOT
                for jj in range(m):
                    jl = pcs + jj - cs
                    for cg in range(ncg):
                        wcols = min(512, T - cg * 512)
                        bg = gub[0] % 4
                        bu = (gub[0] + 1) % 4
                        gub[0] += 2
                        for (slot, bi) in ((2 * s, bg), (2 * s + 1, bu)):
                            for k in range(NCH):
                                op("pe", lambda h, slot=slot, bi=bi, k=k, jj=jj, cg=cg, wcols=wcols: h.matmul(
                                    out=bank(bi)[:, 0:wcols], lhsT=gu_v(slot)[:, k, jj * 128:(jj + 1) * 128],
                                    rhs=(hTe_v[:, k, 0:wcols] if sparse_mode else hT[:, k, cg * 512:cg * 512 + wcols]),
                                    start=(k == 0), stop=(k == NCH - 1)),
                                   reads=[("gu", slot)] + hkeys, writes=[bk(bi)])
                        ss = (gub[0] // 2) % 2
                        op("act", lambda h, bg=bg, ss=ss, wcols=wcols: h.activation(out=sgs[ss][:, 0:wcols], in_=bank(bg)[:, 0:wcols],
                                                                                   func=AF.Silu),
                           reads=[bk(bg)], writes=[("sgs", ss)])
                        op("dve", lambda h, bu=bu, ss=ss, jl=jl, cg=cg, wcols=wcols: h.tensor_tensor(
                            out=abuf[:, jl, cg * 512:cg * 512 + wcols], in0=bank(bu)[:, 0:wcols], in1=sgs[ss][:, 0:wcols], op=ALU.mult),
                           reads=[bk(bu), ("sgs", ss)], writes=[(akey, jl)])
                pi += 1
            if sparse_mode:
                if (not first_group) and gi + 1 < len(groups):
                    sparse_sbuild(units[groups[gi + 1][0]][4], nblk)
                for half in range(2):
                    for i in range(NT):
                        wi = TW[i]
                        bi = 4 + (i % 2)
                        for jl in range(n):
                            op("pe", lambda h, bi=bi, jl=jl, i=i, half=half, n=n, wi=wi: h.matmul(
                                out=bank(bi)[0:wi, :], lhsT=actTe[:, jl, i * 128:i * 128 + wi], rhs=wd_v(half)[:, jl, :],
                                start=(jl == 0), stop=(jl == n - 1)),
                               reads=[("actTe", jl), ("wd", half)], writes=[bk(bi)])
                        yv = ye_v[0:wi, i, half * 512:(half + 1) * 512]
                        if first_group:
                            op("act", lambda h, bi=bi, yv=yv, i=i, wi=wi: h.activation(out=yv, in_=bank(bi)[0:wi, :], func=AF.Copy,
                                                                                      scale=gpos[0:wi, i:i + 1]),
                               reads=[bk(bi), "gpos"], writes=[("ye", i, half)])
                        else:
                            op("dve", lambda h, bi=bi, yv=yv, i=i, wi=wi: h.scalar_tensor_tensor(
                                out=yv, in0=bank(bi)[0:wi, :], scalar=gpos[0:wi, i:i + 1], in1=yv, op0=ALU.mult, op1=ALU.add),
                               reads=[bk(bi), "gpos", ("ye", i, half)], writes=[("ye", i, half)])
                    if gi + 1 < len(groups):
                        load_wd(gi + 1, half)
                if not first_group:
                    sparse_scatter(nblk)
                continue
            for half in range(2):
                for b in range(nblk):
                    bi = 4 + (b % 2)
                    for jl in range(n):
                        op("pe", lambda h, bi=bi, jl=jl, b=b, half=half, n=n: h.matmul(
                            out=bank(bi)[:, :], lhsT=actT[:, jl, b * 128:(b + 1) * 128], rhs=wd_v(half)[:, jl, :],
                            start=(jl == 0), stop=(jl == n - 1)),
                           reads=[("actT", jl), ("wd", half)], writes=[bk(bi)])
                    xs = x_sb[:, b, half * 512:(half + 1) * 512]
                    if gc is None:
                        op("dve", lambda h, bi=bi, xs=xs: h.tensor_tensor(out=xs, in0=bank(bi)[:, :], in1=xs, op=ALU.add),
                           reads=[bk(bi), ("x", b)], writes=[("x", b)])
                    else:
                        op("dve", lambda h, bi=bi, xs=xs, b=b, gc=gc: h.scalar_tensor_tensor(
                            out=xs, in0=bank(bi)[:, :], scalar=gates[:, b, gc:gc + 1], in1=xs, op0=ALU.mult, op1=ALU.add),
                           reads=[bk(bi), ("x", b), ("gates", b)], writes=[("x", b)])
                if gi + 1 < len(groups):
                    load_wd(gi + 1, half)

    def final_store(nblk, row0):
        op("sp", lambda h: h.dma_start(out=bo_bc[:], in_=final_norm.partition_broadcast(128)), writes=["bo_bc"], lane="bo_bc")
        for b in range(nblk):
            s = b % 2
            ssc = stat[:, 4 * s:4 * s + 1]
            rsc = stat[:, 4 * s + 1:4 * s + 2]
            op("act", lambda h, b=b, ssc=ssc: h.activation(out=sqj[:], in_=x_sb[:, b, :], func=AF.Square, accum_out=ssc),
               reads=[("x", b)], writes=["sqj", ("stat", s)])
            op("act", lambda h, ssc=ssc, rsc=rsc: h.activation(out=rsc, in_=ssc, func=AF.Sqrt, bias=EPS, scale=1.0 / D),
               reads=[("stat", s)], writes=[("statr", s)])
            op("dve", lambda h, rsc=rsc: h.reciprocal(out=rsc, in_=rsc), reads=[("statr", s)], writes=[("statr", s)])
            op("dve", lambda h, b=b, s=s, rsc=rsc: h.scalar_tensor_tensor(out=ost[s], in0=x_sb[:, b, :], scalar=rsc, in1=bo_bc[:],
                                                                          op0=ALU.mult, op1=ALU.mult),
               reads=[("x", b), ("statr", s), "bo_bc"], writes=[("ost", s)])
            op("sp", lambda h, b=b, s=s: h.dma_start(out=out_d[row0 + b * 128:row0 + (b + 1) * 128, :], in_=ost[s]),
               reads=[("ost", s)], lane=("ost", s))

    ffn_units = [(ffn_wg[0], ffn_wu[0], ffn_wd[0], D_FF, None)]
    moe_units = [(moe_wg[0, e], moe_wu[0, e], moe_wd[0, e], D_FFE, e) for e in range(NEXP)]

    def load_x(tok0, nblk):
        for b in range(nblk):
            op("sp", lambda h, b=b: h.dma_start(out=x_sb[:, b, :], in_=x_d[tok0 + b * 128:tok0 + (b + 1) * 128, :]),
               writes=[("x", b)], lane=("x", b))

    def mixer_layer(l, nblk, full=True):
        load_mixer_weights(l)
        rms_transpose(gA, l, nblk)
        P.retire(ACT_KEYS, MIX_KEYS + [("uTc", c) for c in range(4)] + [("yc", c) for c in range(4)])
        b0 = 0
        while b0 < nblk:
            nsb = min(SW // 128, nblk - b0)
            mixer_sub(l, b0, nsb, full=full)
            b0 += nsb
        P.retire(MIX_KEYS + [("uTc", c) for c in range(4)] + [("yc", c) for c in range(4)], ACT_KEYS)

    load_x(0, HALO_BLKS)
    mixer_layer(0, HALO_BLKS)
    rms_transpose(gF, 0, HALO_BLKS)
    swiglu_phase(ffn_units, HALO_BLKS)
    mixer_layer(1, HALO_BLKS, full=False)
    for l in range(2):
        op("dve", lambda h, l=l: h.tensor_scalar(out=vprev[:, l, :, :], in0=vprev[:, l, :, :], scalar1=flag[:, 0:1], scalar2=None,
                                                 op0=ALU.mult), reads=[("vprev", l), "flag"], writes=[("vprev", l)])
        op("dve", lambda h, l=l: h.tensor_scalar(out=utail[:, l, :, :], in0=utail[:, l, :, :], scalar1=flag[:, 0:1], scalar2=None,
                                                 op0=ALU.mult), reads=[("utail", l), "flag"], writes=[("utail", l)])
    for st in range(n_st):
        tok0 = (HALO_BLKS + st * nb) * 128
        load_x(tok0, nb)
        mixer_layer(0, nb)
        rms_transpose(gF, 0, nb)
        swiglu_phase(ffn_units, nb)
        mixer_layer(1, nb)
        assert sparse
        if True:
            HTK = [("hT", b) for b in range(NBMAX)]
            TKK = [("htok", b) for b in range(NBMAX)]
            P.retire(HTK, TKK)
            rms_moe(nb)
            moe_positions(nb)
            P.retire(ACT_KEYS, SPARSE_KEYS)
            P.retire([("ost", 0), ("ost", 1)], ["S"])
            P.retire([("hn", 0), ("hn", 1)], ["G"])
            nr = (nb * 128 + CAP - 1) // CAP
            for r in range(nr):
                if r > 0:
                    P.begin_region(cnt_i[0:1, 0:1], CAP * r)
                op("dve", lambda h, r=r: h.tensor_scalar(out=rposr[:, 0:8 * nb], in0=rpos[:, 0:8 * nb], scalar1=float(-CAP * r),
                                                         scalar2=None, op0=ALU.add), reads=["rpos"], writes=["rposr"])
                swiglu_phase(moe_units, nb, sparse_mode=True)
                if r > 0:
                    P.end_region()
            P.retire(SPARSE_KEYS, ACT_KEYS)
            P.retire(["S"], [("ost", 0), ("ost", 1)])
            P.retire(["G"], [("hn", 0), ("hn", 1)])
            P.retire(TKK, HTK)
        final_store(nb, st * nb * 128)

    P.emit(final_lanes=[("ost", 0), ("ost", 1)])
    P.stack.close()
    return nc, P


def _q_perm():
    idx = np.zeros(512, dtype=np.int64)
    for j in range(4):
        for kv in range(2):
            for d in range(64):
                idx[j * 128 + kv * 64 + d] = (kv * 4 + j) * 64 + d
    return idx


def prep_weights(inputs):
    f = lambda a: np.ascontiguousarray(np.asarray(a, dtype=np.float32))
    w = {k: f(v) for k, v in inputs.items() if k != "x"}
    perm = np.arange(IN_COLS)
    perm[1024:1536] = 1024 + _q_perm()
    w["w_in"] = np.ascontiguousarray(w["w_in"][:, :, perm])
    w["b_in"] = np.ascontiguousarray(w["b_in"][:, perm])
    return w


_CACHE = {}


def kernel(**inputs):
    x = np.asarray(inputs["x"], dtype=np.float32)
    B, S, _ = x.shape
    w = prep_weights(inputs)
    n_st, nb = 4, 8
    per = n_st * nb * 128
    halves = S // per
    assert B * halves == N_CORES
    if "prog" not in _CACHE:
        nc, P = build_program(n_st, nb)
        _CACHE["prog"] = (nc, P)
    nc, P = _CACHE["prog"]
    in_maps = []
    for c in range(N_CORES):
        b, hf = c // halves, c % halves
        xs = np.zeros((HALO_BLKS * 128 + per, D), dtype=np.float32)
        xs[HALO_BLKS * 128:] = x[b, hf * per:(hf + 1) * per]
        if hf > 0:
            xs[:HALO_BLKS * 128] = x[b, hf * per - HALO_BLKS * 128:hf * per]
        m = dict(w)
        m["x"] = xs
        m["flag"] = np.full((128, 1), 1.0 if hf > 0 else 0.0, dtype=np.float32)
        in_maps.append(m)
    res = run_bass_kernel_spmd(nc, in_maps, core_ids=list(range(N_CORES)))
    out = np.zeros((B, S, D), dtype=np.float32)
    for c in range(N_CORES):
        b, hf = c // halves, c % halves
        out[b, hf * per:(hf + 1) * per] = res.results[c]["out"]
    return out
```

```python
import contextlib
import numpy as np
import concourse.bass as bass
import concourse.mybir as mybir
from concourse.bass_utils import run_bass_kernel_spmd

F32 = mybir.dt.float32
BF16 = mybir.dt.bfloat16
AF = mybir.ActivationFunctionType
ALU = mybir.AluOpType
AX = mybir.AxisListType

ENGS = ("pe", "act", "dve", "pool", "sp")
SAME_ENGINE_SYNC = True

D = 1024
NCH = 8
IN_COLS = 1792
CONV_K = 31
D_FF = 2816
D_FFE = 3584
NEXP = 8
EPS = 1e-5
HALO_BLKS = 2
N_CORES = 8


class Instr:
    __slots__ = ("eng", "idx", "fn", "waits", "lane", "lane_ord", "needs_inc", "inc_count", "clock", "region")


class Prog:
    def __init__(self, nc):
        self.nc = nc
        self.streams = {e: [] for e in ENGS}
        self.last_w = {}
        self.readers = {}
        self.clock = {e: {} for e in ENGS}
        self.lane_n = {}
        self.lane_last = {}
        self.stack = contextlib.ExitStack()
        self.cur_region = None
        self.regions = []
        self.markers = {}
        self.nbar = 0

    def barrier(self):
        bid = self.nbar
        self.nbar += 1
        for e in ENGS:
            fn, reads, writes, lane = self.markers[e]
            self.op(e, fn, reads=list(reads), writes=list(writes) + [("bar", bid, e)], lane=lane)
        for e in ENGS:
            self.op(e, None, reads=[("bar", bid, e2) for e2 in ENGS if e2 != e], register=False)

    def begin_region(self, cond_ap, thresh, light=False, cond_id=None, cond_key=None):
        if light:
            for e in ENGS:
                if cond_key is not None:
                    self.op(e, None, reads=[cond_key], register=False)
                if self.streams[e]:
                    last = self.streams[e][-1]
                    if last.lane is None and last.fn is not None:
                        last.needs_inc = True
                    else:
                        for x in reversed(self.streams[e]):
                            if x.lane is None and x.fn is not None:
                                x.needs_inc = True
                                break
        else:
            self.barrier()
        self._snap = {e: dict(self.clock[e]) for e in ENGS}
        self.regions.append({"cond": cond_ap, "thresh": thresh, "light": light, "snap": self._snap, "cond_id": cond_id})
        self.cur_region = len(self.regions) - 1

    def end_region(self):
        light = self.regions[self.cur_region]["light"]
        self.cur_region = None
        for e in ENGS:
            self.clock[e] = dict(self._snap[e])
        if not light:
            self.barrier()

    def sb(self, name, shape, dtype):
        return self.stack.enter_context(self.nc.sbuf_tensor("sb_" + name, list(shape), dtype))

    def ps(self, name, shape, dtype):
        return self.stack.enter_context(self.nc.psum_tensor("ps_" + name, list(shape), dtype))

    def _dom(self, p):
        return ("L", p.lane) if p.lane is not None else p.eng

    def _ord(self, p):
        return p.lane_ord if p.lane is not None else p.idx + 1

    def op(self, eng, fn, reads=(), writes=(), lane=None, register=True):
        ins = Instr()
        ins.eng = eng
        ins.fn = fn
        ins.lane = lane
        ins.needs_inc = False
        ins.inc_count = 0
        ins.region = self.cur_region
        st = self.streams[eng]
        ins.idx = len(st)
        deps = []
        for k in reads:
            w = self.last_w.get(k)
            if w is not None:
                deps.append((w, True))
        for k in writes:
            w = self.last_w.get(k)
            if w is not None:
                deps.append((w, True))
            rd = self.readers.get(k)
            if rd:
                for r in rd.values():
                    deps.append((r, False))
        if lane is not None:
            n = self.lane_n.get(lane, 0) + 1
            self.lane_n[lane] = n
            ins.lane_ord = n
            prev = self.lane_last.get(lane)
            if prev is not None:
                deps.append((prev, True))
            self.lane_last[lane] = ins
        else:
            ins.lane_ord = 0
        clk = self.clock[eng]
        waits = []
        for p, raw in deps:
            if p is ins:
                continue
            d = self._dom(p)
            o = self._ord(p)
            if p.lane is None and p.eng == eng:
                if not (SAME_ENGINE_SYNC and raw) or eng in ("pe", "sp"):
                    continue
            if clk.get(d, 0) >= o:
                continue
            waits.append(p)
            if p.region is not None and p.region != self.cur_region:
                src = dict(self.regions[p.region]["snap"][p.eng])
                src[d] = o
            else:
                src = p.clock
            for dd, oo in src.items():
                if clk.get(dd, 0) < oo:
                    clk[dd] = oo
            if p.lane is None:
                p.needs_inc = True
        ins.waits = waits
        c = dict(clk)
        c[self._dom(ins)] = self._ord(ins)
        ins.clock = c
        dom = self._dom(ins)
        for k in (reads if register else ()):
            rd = self.readers.get(k)
            if rd is None:
                rd = self.readers[k] = {}
            rd[dom] = ins
        for k in writes:
            self.last_w[k] = ins
            self.readers[k] = {}
        st.append(ins)
        return ins

    def retire(self, old_keys, new_keys):
        pend = {}

        def add(p):
            d = self._dom(p)
            q = pend.get(d)
            if q is None or self._ord(q) < self._ord(p):
                pend[d] = p
        for k in old_keys:
            w = self.last_w.pop(k, None)
            if w is not None:
                add(w)
            rd = self.readers.pop(k, None)
            if rd:
                for p in rd.values():
                    add(p)
        for k in new_keys:
            self.last_w[k] = None
            self.readers[k] = dict(pend)

    def emit(self, final_lanes=()):
        nc = self.nc
        sems = {e: self.stack.enter_context(nc.semaphore("s_" + e)) for e in ENGS}
        lane_sems = {}
        for i, l in enumerate(self.lane_n):
            lane_sems[l] = self.stack.enter_context(nc.semaphore("l%d" % i))
        for e in ENGS:
            c = 0
            for ins in self.streams[e]:
                if ins.needs_inc:
                    c += 1
                ins.inc_count = c

        def run(e, h):
            stream = self.streams[e]
            reg = [None]
            state = {"cur": None, "guard": None, "first": 0}

            def open_region(R, i0):
                if reg[0] is None:
                    reg[0] = h.alloc_register("creg_" + e)
                rg = self.regions[R]
                cond_ap, thresh = rg["cond"], rg["thresh"]
                h.reg_load(reg[0], cond_ap)
                g = h.If_cmp(reg[0], thresh, "IS_GT")
                g.__enter__()
                state["guard"] = g
                state["first"] = i0

            def close_region(R, i1):
                state["guard"].__exit__(None, None, None)
                body = stream[state["first"]:i1]
                k = sum(1 for x in body if x.needs_inc and x.lane is None)
                pre = stream[state["first"] - 1].inc_count if state["first"] > 0 else 0
                lanes = {}
                for x in body:
                    if x.lane is not None:
                        d = lanes.setdefault(x.lane, [x.lane_ord - 1, 0])
                        d[1] += 1
                if k > 0 or lanes:
                    with h.Else():
                        if k > 0:
                            h.wait_ge(sems[e], pre)
                            h.sem_inc(sems[e], k)
                        for l, (pl, kl) in lanes.items():
                            h.wait_ge(lane_sems[l], 16 * pl)
                            h.sem_inc(lane_sems[l], 16 * kl)

            for i, ins in enumerate(stream):
                if ins.region != state["cur"]:
                    if state["cur"] is not None:
                        close_region(state["cur"], i)
                    if ins.region is not None:
                        open_region(ins.region, i)
                    state["cur"] = ins.region
                for p in ins.waits:
                    if p.lane is not None:
                        h.wait_ge(lane_sems[p.lane], 16 * p.lane_ord)
                    else:
                        h.wait_ge(sems[p.eng], p.inc_count)
                if ins.fn is None:
                    assert not ins.needs_inc
                    continue
                bi = ins.fn(h)
                if ins.lane is not None:
                    bi.then_inc(lane_sems[ins.lane], 16)
                elif ins.needs_inc:
                    bi.then_inc(sems[e], 1)
            if state["cur"] is not None:
                close_region(state["cur"], len(stream))
            if e == "sp":
                for l in final_lanes:
                    h.wait_ge(lane_sems[l], 16 * self.lane_n[l])

        with nc.Block() as block:
            @block.tensor
            def _(h):
                run("pe", h)

            @block.scalar
            def _(h):
                run("act", h)

            @block.vector
            def _(h):
                run("dve", h)

            @block.gpsimd
            def _(h):
                run("pool", h)

            @block.sync
            def _(h):
                run("sp", h)


def build_program(n_st=4, nb=8, cap=384, sparse=True, dbg=False, base=384):
    nc = bass.Bass("TRN2", target_bir_lowering=False)
    ntok = (HALO_BLKS + n_st * nb) * 128
    dr = {}

    def din(name, shape):
        dr[name] = nc.dram_tensor(name, list(shape), F32, kind="ExternalInput").ap()
        return dr[name]

    x_d = din("x", [ntok, D])
    flag_d = din("flag", [128, 1])
    attn_norm = din("attn_norm", [2, D])
    ffn_norm = din("ffn_norm", [2, D])
    w_in = din("w_in", [2, D, IN_COLS])
    b_in = din("b_in", [2, IN_COLS])
    conv_w = din("conv_w", [2, CONV_K, 512])
    conv_b = din("conv_b", [2, 512])
    conv_ln_g = din("conv_ln_g", [2, 512])
    conv_ln_b = din("conv_ln_b", [2, 512])
    sinks = din("sinks", [2, 8])
    w_out = din("w_out", [2, D, D])
    b_out = din("b_out", [2, D])
    ffn_wg = din("ffn_w_gate", [1, D, D_FF])
    ffn_wu = din("ffn_w_up", [1, D, D_FF])
    ffn_wd = din("ffn_w_down", [1, D_FF, D])
    moe_router = din("moe_router", [1, D, NEXP])
    moe_wg = din("moe_w_gate", [1, NEXP, D, D_FFE])
    moe_wu = din("moe_w_up", [1, NEXP, D, D_FFE])
    moe_wd = din("moe_w_down", [1, NEXP, D_FFE, D])
    final_norm = din("final_norm", [D])
    out_d = nc.dram_tensor("out", [n_st * nb * 128, D], F32, kind="ExternalOutput").ap()
    dbg_d = nc.dram_tensor("dbg", [n_st, 8], F32, kind="ExternalOutput").ap() if dbg else None

    P = Prog(nc)
    op = P.op
    uid = [0]

    def lane_name(s):
        return s

    NBMAX = max(nb, HALO_BLKS)
    x_sb = P.sb("x_sb", [128, NBMAX, D], F32)
    hT = P.sb("hT", [128, NCH, NBMAX * 128], BF16)
    hT32 = P.sb("hT32", [128, NCH, 128], F32)
    hn_t = P.sb("hn", [128, 2, D], F32)
    hn = [hn_t[:, i, :] for i in range(2)]
    sqj = P.sb("sqj", [128, D], BF16)
    stat = P.sb("stat", [128, 8], F32)
    wdbuf = P.sb("wdbuf", [128, 14336], BF16)
    gubuf = P.sb("gubuf", [128, 6 * 2048], BF16)
    arena = P.sb("arena", [128, 14336], BF16)
    arena32 = arena.bitcast(F32)
    sgs = [P.sb("sgs%d" % i, [128, 512], BF16) for i in range(2)]
    SW = 512
    qT = P.sb("qT", [128, 4, SW], BF16)
    kT = P.sb("kT", [128, 128 + SW], BF16)
    vaug = P.sb("vaug", [128, 1 + SW // 128, 2, 65], BF16)
    yT = P.sb("yT", [128, NCH, SW], BF16)
    lnt = [P.sb("lnt%d" % i, [128, SW], F32) for i in range(2)]
    sgm = [P.sb("sgm%d" % i, [128, 512], F32) for i in range(2)]
    eT = [P.sb("eT%d" % i, [128, 512], BF16) for i in range(2)]
    pT = [P.sb("pT%d" % i, [128, 512], BF16) for i in range(8)]
    o_sb = P.sb("o_sb", [128, 512], F32)
    dent = P.sb("dent", [128, 16], F32)
    utail = P.sb("utail", [128, 2, 4, 30], BF16)
    NDG = 12
    dg = P.sb("dg", [128, NDG, 128], BF16)
    kprev = P.sb("kprev", [128, 2, 128], BF16)
    vprev = P.sb("vprev", [128, 2, 2, 65], BF16)
    ident = P.sb("ident", [128, 128], F32)
    ones32 = P.sb("ones32", [128, 128], F32)
    maskp = P.sb("maskp", [128, 128], BF16)
    maskc = P.sb("maskc", [128, 128], BF16)
    gA = P.sb("gA", [128, 2, 8], F32)
    gF = P.sb("gF", [128, 2, 8], F32)
    binT = P.sb("binT", [128, 2, 14], F32)
    bv_bc = P.sb("bv_bc", [128, 2, 128], F32)
    cw = P.sb("cw", [128, 2, 4, 32], F32)
    cb = P.sb("cb", [128, 2, 4], F32)
    lg = P.sb("lg", [128, 2, 4], F32)
    lb = P.sb("lb", [128, 2, 4], F32)
    esink = P.sb("esink", [128, 2, 8], F32)
    bo_bc = P.sb("bo_bc", [128, D], F32)
    wr = P.sb("wr", [128, 8, 8], F32)
    flag = P.sb("flag", [128, 1], F32)
    gates = P.sb("gates", [128, NBMAX, 8], F32)
    rt = P.sb("rt", [128, 64], F32)
    ost_t = P.sb("ost", [128, 2, D], F32)
    ost = [ost_t[:, i, :] for i in range(2)]

    I32 = mybir.dt.int32
    CAP = cap
    NT = (CAP + 127) // 128
    TW = [min(128, CAP - 128 * i) for i in range(NT)]
    ident_bf = P.sb("ident_bf", [128, 128], BF16)
    ustrict = P.sb("ustrict", [128, 128], F32)
    iota_row = P.sb("iota_row", [128, CAP], F32)
    mk = P.sb("mk", [128, 8], F32)
    rmask = P.sb("rmask", [128, 64], F32)
    rtot = P.sb("rtot", [128, 64], F32)
    roff = P.sb("roff", [128, 64], F32)
    rpos = P.sb("rpos", [128, 64], F32)
    rposr_t = P.sb("rposr", [128, 4, 64], F32)
    cur_r = [0]
    ghi = P.sb("ghi", [128, 64], BF16)
    g2 = P.sb("g2", [128, 64, 2], BF16)
    gpos = P.sb("gpos", [128, 4], F32)
    ncnt = P.sb("ncnt", [128, 8], F32)
    cnt_i = P.sb("cnt_i", [128, 1], I32)
    cnt8_i = P.sb("cnt8_i", [128, 8], I32)
    BASE = base
    rgn = [0]
    S_v = ost_t.bitcast(BF16)[:, :, :].rearrange("p a n -> p (a n)")[:, 0:8 * CAP].rearrange("p (b n) -> p b n", n=CAP)
    G_v = hn_t.bitcast(BF16)[:, :, :].rearrange("p a n -> p (a n)")[:, 0:NT * 1024].rearrange("p (i n) -> p i n", n=1024)
    pq = [P.ps("pq%d" % i, [128, 1024], F32) for i in range(4)]

    def bank(i):
        return pq[i // 2][:, (i % 2) * 512:(i % 2) * 512 + 512]

    def bk(i):
        return ("pb", i)

    cw_raw = x_sb[0:31, 0, :].rearrange("p (l c) -> p l c", c=512)
    iota_i = x_sb[:, 1, 0:CAP].bitcast(I32)
    actT = arena[:, :].rearrange("p (j n) -> p j n", n=1024)
    UW = 30 + SW
    uT = arena[:, 0:4 * UW].rearrange("p (c n) -> p c n", n=UW)
    Y0 = (2 * UW + 31) // 32 * 32
    yconv = arena32[:, Y0:Y0 + 4 * SW].rearrange("p (c n) -> p c n", n=SW)
    ysq = arena32[:, Y0 + 4 * SW:Y0 + 8 * SW].rearrange("p (c n) -> p c n", n=SW)
    assert Y0 + 8 * SW <= 7168
    actTe = arena[:, 0:14 * CAP].rearrange("p (j n) -> p j n", n=CAP)
    hTe_v = arena[:, 14 * CAP:22 * CAP].rearrange("p (c n) -> p c n", n=CAP)
    ye_v = arena[:, 22 * CAP:22 * CAP + NT * 1024].rearrange("p (i n) -> p i n", n=1024)
    assert 22 * CAP + NT * 1024 <= 14336
    SPARSE_KEYS = ([("actTe", j) for j in range(14)] + [("actTeX", j) for j in range(14)] + ["hTe", "hTeX"]
                   + [("ye", i, hf) for i in range(NT) for hf in range(2)])
    hT_flat = hT[:, :, :].rearrange("p c n -> p (c n)")

    def htok(b):
        return hT_flat[:, b * D:(b + 1) * D]
    ACT_KEYS = [("actT", j) for j in range(14)]
    MIX_KEYS = ["uT", "yconv", "ysq"]
    win_v = wdbuf[:, :].rearrange("p (c n) -> p c n", n=IN_COLS)
    wout_v = gubuf[:, 0:8192].rearrange("p (c n) -> p c n", n=D)
    WD_KEYS = [("wd", 0), ("wd", 1)]
    GU_KEYS = [("gu", s) for s in range(6)]

    def wd_v(h):
        return wdbuf[:, h * 7168:(h + 1) * 7168].rearrange("p (j n) -> p j n", n=512)

    def gu_v(s):
        return gubuf[:, s * 2048:(s + 1) * 2048].rearrange("p (c n) -> p c n", n=256)

    def small_dma(eng, out_ap, in_ap, writes, lane):
        def fn(h):
            with nc.allow_non_contiguous_dma(reason="tiny param load"):
                return h.dma_start(out=out_ap, in_=in_ap)
        op(eng, fn, writes=writes, lane=lane)

    op("pool", lambda h: h.memset(ident[:], 0.0), writes=["ident"])
    op("pool", lambda h: h.affine_select(out=ident[:], in_=ident[:], pattern=[[-1, 128]],
                                        compare_op=ALU.not_equal, fill=1.0, base=0, channel_multiplier=1),
       reads=["ident"], writes=["ident"])
    op("pool", lambda h: h.memset(ones32[:], 1.0), writes=["ones32"])
    op("pool", lambda h: h.memset(maskp[:], 1.0), writes=["maskp"])
    op("pool", lambda h: h.memset(maskc[:], 1.0), writes=["maskc"])
    op("pool", lambda h: h.affine_select(out=maskp[:], in_=maskp[:], pattern=[[-1, 128]],
                                        compare_op=ALU.is_gt, fill=0.0, base=0, channel_multiplier=1),
       reads=["maskp"], writes=["maskp"])
    op("pool", lambda h: h.affine_select(out=maskc[:], in_=maskc[:], pattern=[[1, 128]],
                                        compare_op=ALU.is_ge, fill=0.0, base=0, channel_multiplier=-1),
       reads=["maskc"], writes=["maskc"])
    op("pool", lambda h: h.memset(vaug[:], 1.0), writes=["vaug"])
    op("pool", lambda h: h.memset(vprev[:], 0.0), writes=["vprev"])
    op("pool", lambda h: h.memset(vprev[:, :, :, 64:65], 1.0), reads=["vprev"], writes=["vprev"])
    op("pool", lambda h: h.memset(kprev[:], 0.0), writes=["kprev"])
    op("pool", lambda h: h.memset(utail[:], 0.0), writes=["utail"])
    op("pool", lambda h: h.memset(cw[:], 0.0), writes=["cw"])

    op("pool", lambda h: h.tensor_copy(out=ident_bf[:], in_=ident[:]), reads=["ident"], writes=["ident_bf"])
    op("pool", lambda h: h.memset(ustrict[:], 1.0), writes=["ustrict"])
    op("pool", lambda h: h.affine_select(out=ustrict[:], in_=ustrict[:], pattern=[[1, 128]],
                                        compare_op=ALU.is_gt, fill=0.0, base=0, channel_multiplier=-1),
       reads=["ustrict"], writes=["ustrict"])
    op("pool", lambda h: h.iota(iota_i, pattern=[[1, CAP]], base=0, channel_multiplier=0), writes=[("x", 1)])
    op("pool", lambda h: h.tensor_copy(out=iota_row[:], in_=iota_i), reads=[("x", 1)], writes=["iota_row"])
    op("pool", lambda h: h.memset(mk[:], 0.0), writes=["mk0"])
    P.markers = {
        "pe": (lambda h: h.matmul(out=bank(7)[0:1, 0:1], lhsT=ones32[0:1, 0:1], rhs=ones32[0:1, 0:1], start=True, stop=True),
               ["ones32"], [bk(7)], None),
        "act": (lambda h: h.activation(out=mk[:, 0:1], in_=mk[:, 4:5], func=AF.Copy), ["mk0"], [("mk", "act")], None),
        "dve": (lambda h: h.memset(mk[:, 1:2], 0.0), ["mk0"], [("mk", "dve")], None),
        "pool": (lambda h: h.memset(mk[:, 2:3], 0.0), ["mk0"], [("mk", "pool")], None),
        "sp": (lambda h: h.dma_start(out=mk[:, 3:4], in_=mk[:, 5:6]), ["mk0"], [("mk", "sp")], ("bar", "sp")),
    }
    small_dma("sp", flag[:], flag_d, ["flag"], "c_flag")
    small_dma("sp", gA[:], attn_norm.rearrange("l (c p) -> p l c", p=128), ["gA"], "c_gA")
    small_dma("sp", gF[:], ffn_norm.rearrange("l (c p) -> p l c", p=128), ["gF"], "c_gF")
    small_dma("sp", binT[:], b_in.rearrange("l (c p) -> p l c", p=128), ["binT"], "c_binT")
    for l in range(2):
        small_dma("sp", bv_bc[:, l, :], b_in[l, 1664:1792].partition_broadcast(128), ["bv_bc%d" % l], "c_bv%d" % l)
        small_dma("sp", esink[:, l, :], sinks[l, :].partition_broadcast(128), ["esink%d" % l], "c_es%d" % l)
    small_dma("sp", cw_raw, conv_w.rearrange("l k c -> k l c"), [("x", 0)], "c_cwraw")
    small_dma("sp", cb[:], conv_b.rearrange("l (c p) -> p l c", p=128), ["cb"], "c_cb")
    small_dma("sp", lg[:], conv_ln_g.rearrange("l (c p) -> p l c", p=128), ["lg"], "c_lg")
    small_dma("sp", lb[:], conv_ln_b.rearrange("l (c p) -> p l c", p=128), ["lb"], "c_lb")
    small_dma("sp", wr[:], moe_router[0].rearrange("(c p) e -> p c e", p=128), ["wr"], "c_wr")
    op("act", lambda h: h.activation(out=esink[:], in_=esink[:], func=AF.Exp),
       reads=["esink0", "esink1"], writes=["esink"])
    for l in range(2):
        for c in range(4):
            op("pe", lambda h, l=l, c=c: h.transpose(out=bank(c)[:, 0:31], in_=cw_raw[0:31, l, c * 128:(c + 1) * 128],
                                                     identity=ident[0:31, 0:31]),
               reads=[("x", 0), "ident"], writes=[bk(c)])
            op("dve", lambda h, l=l, c=c: h.tensor_copy(out=cw[:, l, c, 0:31], in_=bank(c)[:, 0:31]),
               reads=[bk(c), "cw"], writes=[("cwT", l, c)])
    CW_KEYS = [("cwT", l, c) for l in range(2) for c in range(4)]

    pbrr = [0]
    dgc = [0]
    cur_st = [0]

    def next_bank():
        i = pbrr[0] % 8
        pbrr[0] += 1
        return i

    def rms_stage_a(b, gain_bc=None):
        s = b % 2
        ssc = stat[:, 4 * s:4 * s + 1]
        rsc = stat[:, 4 * s + 1:4 * s + 2]
        op("act", lambda h: h.activation(out=sqj[:], in_=x_sb[:, b, :], func=AF.Square, accum_out=ssc),
           reads=[("x", b)], writes=["sqj", ("stat", s)])
        op("act", lambda h: h.activation(out=rsc, in_=ssc, func=AF.Sqrt, bias=EPS, scale=1.0 / D),
           reads=[("stat", s)], writes=[("statr", s)])
        op("dve", lambda h: h.reciprocal(out=rsc, in_=rsc), reads=[("statr", s)], writes=[("statr", s)])
        if gain_bc is None:
            op("dve", lambda h: h.tensor_scalar(out=hn[s], in0=x_sb[:, b, :], scalar1=rsc, scalar2=None, op0=ALU.mult),
               reads=[("x", b), ("statr", s)], writes=[("hn", s)])
        else:
            op("dve", lambda h: h.scalar_tensor_tensor(out=hn[s], in0=x_sb[:, b, :], scalar=rsc, in1=gain_bc, op0=ALU.mult, op1=ALU.mult),
               reads=[("x", b), ("statr", s), "bo_bc"], writes=[("hn", s)])

    def rms_transposes(b):
        s = b % 2
        pt = pq[b % 2]
        for c in range(NCH):
            op("pe", lambda h, c=c: h.transpose(out=pt[:, c * 128:(c + 1) * 128], in_=hn[s][:, c * 128:(c + 1) * 128], identity=ident[:]),
               reads=[("hn", s), "ident"], writes=[bk(2 * (b % 2) + c // 4)])
        return pt[:, :].rearrange("p (c t) -> p c t", t=128)

    def rms_transpose(gT, l, nblk):
        rms_stage_a(0)
        for b in range(nblk):
            if b + 1 < nblk:
                rms_stage_a(b + 1)
            ptv = rms_transposes(b)
            gbc = gT[:, l, :].unsqueeze(2).to_broadcast([128, NCH, 128])
            op("dve", lambda h, b=b, ptv=ptv, gbc=gbc: h.tensor_tensor(out=hT[:, :, b * 128:(b + 1) * 128], in0=ptv, in1=gbc, op=ALU.mult),
               reads=[bk(2 * (b % 2)), bk(2 * (b % 2) + 1), "gA", "gF"], writes=[("hT", b)])

    Lall = P.sb("Lall", [128, NBMAX, 8], F32)
    rtb = P.sb("rtb", [128, 6, NBMAX, 8], F32)
    rts = P.sb("rts", [128, 6, NBMAX], F32)

    def router_gates_all(nblk):
        L = Lall[:, 0:nblk, :]
        mk1, L2, mk2, t1 = (rtb[:, i, 0:nblk, :] for i in range(4))
        m1, m2, dd, w1, w2 = (rts[:, i, 0:nblk] for i in range(5))

        def bc(v):
            return v.unsqueeze(2).to_broadcast([128, nblk, 8])
        K = "rt"
        LK = [("Lall", b) for b in range(nblk)]
        op("dve", lambda h: h.tensor_reduce(out=m1, in_=L, axis=AX.X, op=ALU.max), reads=LK, writes=[K])
        op("dve", lambda h: h.tensor_tensor(out=mk1, in0=L, in1=bc(m1), op=ALU.is_equal), reads=LK + [K], writes=[K])
        op("dve", lambda h: h.scalar_tensor_tensor(out=L2, in0=mk1, scalar=-1e30, in1=L, op0=ALU.mult, op1=ALU.add), reads=LK + [K], writes=[K])
        op("dve", lambda h: h.tensor_reduce(out=m2, in_=L2, axis=AX.X, op=ALU.max), reads=[K], writes=[K])
        op("dve", lambda h: h.tensor_tensor(out=mk2, in0=L2, in1=bc(m2), op=ALU.is_equal), reads=[K], writes=[K])
        op("dve", lambda h: h.tensor_tensor(out=dd, in0=m2, in1=m1, op=ALU.subtract), reads=[K], writes=[K])
        op("act", lambda h: h.activation(out=dd, in_=dd, func=AF.Exp), reads=[K], writes=[K])
        op("dve", lambda h: h.tensor_scalar(out=w1, in0=dd, scalar1=1.0, scalar2=None, op0=ALU.add), reads=[K], writes=[K])
        op("dve", lambda h: h.reciprocal(out=w1, in_=w1), reads=[K], writes=[K])
        op("dve", lambda h: h.tensor_tensor(out=w2, in0=dd, in1=w1, op=ALU.mult), reads=[K], writes=[K])
        op("dve", lambda h: h.tensor_tensor(out=mk1, in0=mk1, in1=bc(w1), op=ALU.mult), reads=[K], writes=[K])
        op("dve", lambda h: h.tensor_tensor(out=t1, in0=mk2, in1=bc(w2), op=ALU.mult), reads=[K], writes=[K])
        op("dve", lambda h: h.tensor_tensor(out=gates[:, 0:nblk, :], in0=mk1, in1=t1, op=ALU.add), reads=[K],
           writes=[("gates", b) for b in range(nblk)])

    def rms_moe(nblk):
        op("sp", lambda h: h.dma_start(out=bo_bc[:], in_=ffn_norm[1, :].partition_broadcast(128)), writes=["bo_bc"], lane="bo_bc")
        rms_stage_a(0, bo_bc[:])
        for b in range(nblk):
            if b + 1 < nblk:
                rms_stage_a(b + 1, bo_bc[:])
            s = b % 2
            op("pool", lambda h, b=b, s=s: h.tensor_copy(out=htok(b), in_=hn[s]), reads=[("hn", s)], writes=[("htok", b)])
            ptv = rms_transposes(b)
            op("act", lambda h, ptv=ptv: h.activation(out=hT32[:], in_=ptv, func=AF.Copy),
               reads=[bk(2 * (b % 2)), bk(2 * (b % 2) + 1)], writes=["hT32"])
            rb = 4 + (b % 2)
            for c in range(NCH):
                op("pe", lambda h, c=c, rb=rb: h.matmul(out=bank(rb)[:, 0:8], lhsT=hT32[:, c, :], rhs=wr[:, c, :],
                                                        start=(c == 0), stop=(c == NCH - 1)),
                   reads=["hT32", "wr"], writes=[bk(rb)])
            op("act", lambda h, b=b, rb=rb: h.activation(out=Lall[:, b, :], in_=bank(rb)[:, 0:8], func=AF.Copy),
               reads=[bk(rb)], writes=[("Lall", b)])
        router_gates_all(nblk)

    def moe_positions(nblk):
        n = nblk * 8
        gk = [("gates", b) for b in range(nblk)]
        gflat = gates[:, 0:nblk, :].rearrange("p b e -> p (b e)")
        op("dve", lambda h: h.tensor_scalar(out=rmask[:, 0:n], in0=gflat, scalar1=0.0, scalar2=None, op0=ALU.is_gt),
           reads=gk, writes=["rmask"])
        pb = 6
        op("pe", lambda h: h.matmul(out=bank(pb)[:, 0:n], lhsT=ustrict[:], rhs=rmask[:, 0:n], start=True, stop=True),
           reads=["rmask", "ustrict"], writes=[bk(pb)])
        op("pe", lambda h: h.matmul(out=bank(pb)[:, 64:64 + n], lhsT=ones32[:], rhs=rmask[:, 0:n], start=True, stop=True),
           reads=["rmask", "ones32"], writes=[bk(pb)])
        op("dve", lambda h: h.tensor_copy(out=rtot[:, 0:n], in_=bank(pb)[:, 64:64 + n]), reads=[bk(pb)], writes=["rtot"])
        op("dve", lambda h: h.memset(roff[:, 0:8], 0.0), writes=["roff"])
        for b in range(1, nblk):
            op("dve", lambda h, b=b: h.tensor_tensor(out=roff[:, 8 * b:8 * b + 8], in0=roff[:, 8 * b - 8:8 * b],
                                                     in1=rtot[:, 8 * b - 8:8 * b], op=ALU.add),
               reads=["roff", "rtot"], writes=["roff"])
        op("dve", lambda h: h.tensor_tensor(out=rpos[:, 0:n], in0=bank(pb)[:, 0:n], in1=roff[:, 0:n], op=ALU.add),
           reads=[bk(pb), "roff"], writes=["rpos"])
        op("dve", lambda h: h.scalar_tensor_tensor(out=rpos[:, 0:n], in0=rpos[:, 0:n], scalar=1.0, in1=rmask[:, 0:n],
                                                   op0=ALU.add, op1=ALU.mult), reads=["rpos", "rmask"], writes=["rpos"])
        op("dve", lambda h: h.tensor_scalar(out=rpos[:, 0:n], in0=rpos[:, 0:n], scalar1=-1.0, scalar2=None, op0=ALU.add),
           reads=["rpos"], writes=["rpos"])
        lb_ = 8 * (nblk - 1)
        op("dve", lambda h: h.tensor_tensor(out=ncnt[:, 0:8], in0=roff[:, lb_:lb_ + 8], in1=rtot[:, lb_:lb_ + 8], op=ALU.add),
           reads=["roff", "rtot"], writes=["ncnt"])
        op("dve", lambda h: h.tensor_reduce(out=rt[:, 48:49], in_=ncnt[:, 0:8], axis=AX.X, op=ALU.max), reads=["ncnt"], writes=["rtmax"])
        op("dve", lambda h: h.tensor_copy(out=cnt_i[:], in_=rt[:, 48:49]), reads=["rtmax"], writes=["cnt_i"])
        op("dve", lambda h: h.tensor_copy(out=cnt8_i[:], in_=ncnt[:, 0:8]), reads=["ncnt"], writes=["cnt8"])
        op("dve", lambda h: h.tensor_copy(out=ghi[:, 0:n], in_=gflat), reads=gk, writes=["ghi"])
        op("dve", lambda h: h.tensor_copy(out=g2[:, 0:n, 0], in_=ghi[:, 0:n]), reads=["ghi"], writes=["g2"])
        op("dve", lambda h: h.tensor_tensor(out=g2[:, 0:n, 1], in0=gflat, in1=ghi[:, 0:n], op=ALU.subtract),
           reads=gk + ["ghi", "g2"], writes=["g2"])

    def sparse_sbuild(e, nblk):
        for b in range(nblk):
            op("dve", lambda h, b=b, r_=cur_r[0]: h.tensor_scalar(out=S_v[:, b, :], in0=iota_row[:], scalar1=rposr_t[:, r_, 8 * b + e:8 * b + e + 1],
                                                     scalar2=None, op0=ALU.is_equal),
               reads=[("rposr", cur_r[0]), "iota_row"], writes=["S"])

    def ext_begin(e):
        rgn[0] += 1
        P.begin_region(cnt8_i[0:1, e:e + 1], BASE, light=True, cond_id=("n", rgn[0] // 100000, cur_st[0], e), cond_key="cnt8")

    def col_sets(elastic):
        if elastic and BASE < CAP:
            nbt = BASE // 128
            return [(0, BASE, False), (BASE, CAP - BASE, True)], [(list(range(nbt)), False), (list(range(nbt, NT)), True)]
        return [(0, CAP, False)], [(list(range(NT)), False)]

    def sparse_pre(e, nblk, gub, build_s=True, elastic=False):
        if build_s:
            sparse_sbuild(e, nblk)
        cols, tsets = col_sets(elastic)
        for (c0, w, cond), (tiles, _) in zip(cols, tsets):
            if cond:
                ext_begin(e)
            hk = "hTeX" if cond else "hTe"
            for c in range(NCH):
                bi = gub[0] % 4
                gub[0] += 1
                for b in range(nblk):
                    op("pe", lambda h, bi=bi, b=b, c=c, c0=c0, w=w: h.matmul(out=bank(bi)[:, c0:c0 + w], lhsT=htok(b)[:, c * 128:(c + 1) * 128],
                                                                             rhs=S_v[:, b, c0:c0 + w], start=(b == 0), stop=(b == nblk - 1)),
                       reads=["S", ("htok", b)], writes=[bk(bi)])
                op("act", lambda h, bi=bi, c=c, c0=c0, w=w: h.activation(out=hTe_v[:, c, c0:c0 + w], in_=bank(bi)[:, c0:c0 + w], func=AF.Copy),
                   reads=[bk(bi)], writes=[hk])
            gub[0] += gub[0] % 2
            for i in tiles:
                wi = TW[i]
                for b in range(nblk):
                    op("pe", lambda h, i=i, b=b, wi=wi: h.matmul(out=bank(6)[0:wi, 2 * i:2 * i + 2], lhsT=S_v[:, b, i * 128:i * 128 + wi],
                                                                 rhs=g2[:, 8 * b + e, :], start=(b == 0), stop=(b == nblk - 1)),
                       reads=["S", "g2"], writes=[bk(6)])
            for i in tiles:
                wi = TW[i]
                g6 = bank(6)[0:wi, 2 * i:2 * i + 2]
                op("dve", lambda h, i=i, wi=wi, g6=g6: h.tensor_reduce(out=gpos[0:wi, i:i + 1], in_=g6, axis=AX.X, op=ALU.add),
                   reads=[bk(6)], writes=[("gpos", i)])
            pqb = pq[3].bitcast(BF16)
            for i in tiles:
                wi = TW[i]
                for b in range(nblk):
                    op("pe", lambda h, i=i, b=b, wi=wi: h.transpose(out=pqb[0:wi, 1024 + b * 128:1024 + (b + 1) * 128],
                                                                    in_=S_v[:, b, i * 128:i * 128 + wi], identity=ident_bf[:]),
                       reads=["S", "ident_bf"], writes=[bk(7)])
                op("act", lambda h, i=i, wi=wi: h.activation(out=G_v[0:wi, i, 0:nblk * 128], in_=pqb[0:wi, 1024:1024 + nblk * 128], func=AF.Copy),
                   reads=[bk(7)], writes=[("G", i)])
            if cond:
                P.end_region()

    def sparse_scatter(e, nblk, elastic=False):
        cols, tsets = col_sets(elastic)
        for tiles, cond in tsets:
            if cond:
                ext_begin(e)
            for b in range(nblk):
                for half in range(2):
                    bi = 4 + ((2 * b + half) % 2)
                    for i in tiles:
                        wi = TW[i]
                        op("pe", lambda h, bi=bi, i=i, b=b, half=half, wi=wi, tiles=tiles: h.matmul(
                            out=bank(bi)[:, :], lhsT=G_v[0:wi, i, b * 128:(b + 1) * 128], rhs=ye_v[0:wi, i, half * 512:(half + 1) * 512],
                            start=(i == tiles[0]), stop=(i == tiles[-1])),
                           reads=[("G", i), ("ye", i, half)], writes=[bk(bi)])
                    xs = x_sb[:, b, half * 512:(half + 1) * 512]
                    op("dve", lambda h, bi=bi, xs=xs: h.tensor_tensor(out=xs, in0=bank(bi)[:, :], in1=xs, op=ALU.add),
                       reads=[bk(bi), ("x", b)], writes=[("x", b)])
            if cond:
                P.end_region()

    def load_mixer_weights(l):
        for hh in range(2):
            op("pool", lambda h, hh=hh: h.dma_start(out=win_v[:, 4 * hh:4 * hh + 4, :],
                                                    in_=w_in[l, 512 * hh:512 * hh + 512, :].rearrange("(c p) n -> p c n", p=128)),
               writes=[("wd", hh)], lane=("wd", hh))
        for hh in range(2):
            op("pool", lambda h, hh=hh: h.dma_start(out=wout_v[:, 4 * hh:4 * hh + 4, :],
                                                    in_=w_out[l, 512 * hh:512 * hh + 512, :].rearrange("(c p) n -> p c n", p=128)),
               writes=[("gu", 2 * hh), ("gu", 2 * hh + 1)], lane=("gu", 2 * hh))
        op("sp", lambda h: h.dma_start(out=bo_bc[:], in_=b_out[l, :].partition_broadcast(128)), writes=["bo_bc"], lane="bo_bc")

    def mixer_sub(l, b0, nsb, full=True):
        W = nsb * 128
        c0 = b0 * 128
        WK = WD_KEYS
        op("pool", lambda h: h.tensor_copy(out=uT[:, :, 0:30], in_=utail[:, l, :, :]), reads=[("utail", l), "uT"], writes=["uT"])
        op("pool", lambda h: h.tensor_copy(out=kT[:, 0:128], in_=kprev[:, l, :]), reads=[("kprev", l), "kT"], writes=["kT"])
        op("pool", lambda h: h.tensor_copy(out=vaug[:, 0, :, :], in_=vprev[:, l, :, :]), reads=[("vprev", l), "vaug"], writes=["vaug"])
        if full:
            for b in range(b0, b0 + nsb):
                op("pool", lambda h, b=b: h.tensor_tensor(out=x_sb[:, b, :], in0=x_sb[:, b, :], in1=bo_bc[:], op=ALU.add),
                   reads=[("x", b), "bo_bc"], writes=[("x", b)])
        hkeys = [("hT", b) for b in range(b0, b0 + nsb)]
        for c in range(4):
            ia, ig = next_bank(), next_bank()
            for (m, bi) in ((c, ia), (4 + c, ig)):
                for k in range(NCH):
                    op("pe", lambda h, m=m, bi=bi, k=k: h.matmul(out=bank(bi)[:, 0:W], lhsT=win_v[:, k, m * 128:(m + 1) * 128],
                                                                 rhs=hT[:, k, c0:c0 + W], start=(k == 0), stop=(k == NCH - 1)),
                       reads=WK + hkeys, writes=[bk(bi)])
            s = c % 2
            op("act", lambda h, c=c, ig=ig, s=s: h.activation(out=sgm[s][:, 0:W], in_=bank(ig)[:, 0:W], func=AF.Sigmoid,
                                                             bias=binT[:, l, 4 + c:5 + c]),
               reads=[bk(ig), "binT"], writes=[("sgm", s)])
            op("dve", lambda h, c=c, ia=ia, s=s: h.scalar_tensor_tensor(out=uT[:, c, 30:30 + W], in0=bank(ia)[:, 0:W],
                                                                       scalar=binT[:, l, c:c + 1], in1=sgm[s][:, 0:W],
                                                                       op0=ALU.add, op1=ALU.mult),
               reads=[bk(ia), ("sgm", s), "binT", "uT"], writes=[("uTc", c)])
        UC = [("uTc", c) for c in range(4)]
        for j in range(4):
            bi = next_bank()
            for k in range(NCH):
                op("pe", lambda h, j=j, bi=bi, k=k: h.matmul(out=bank(bi)[:, 0:W], lhsT=win_v[:, k, (8 + j) * 128:(9 + j) * 128],
                                                             rhs=hT[:, k, c0:c0 + W], start=(k == 0), stop=(k == NCH - 1)),
                   reads=WK + hkeys, writes=[bk(bi)])
            op("act", lambda h, j=j, bi=bi: h.activation(out=qT[:, j, 0:W], in_=bank(bi)[:, 0:W], func=AF.Identity,
                                                         bias=binT[:, l, 8 + j:9 + j]),
               reads=[bk(bi), "binT"], writes=[("qT", j)])
        bi = next_bank()
        for k in range(NCH):
            op("pe", lambda h, bi=bi, k=k: h.matmul(out=bank(bi)[:, 0:W], lhsT=win_v[:, k, 1536:1664],
                                                    rhs=hT[:, k, c0:c0 + W], start=(k == 0), stop=(k == NCH - 1)),
               reads=WK + hkeys, writes=[bk(bi)])
        op("act", lambda h, bi=bi: h.activation(out=kT[:, 128:128 + W], in_=bank(bi)[:, 0:W], func=AF.Identity,
                                                bias=binT[:, l, 12:13]),
           reads=[bk(bi), "binT", "kT"], writes=["kTn"])
        for i in range(nsb):
            bi = next_bank()
            for k in range(NCH):
                op("pe", lambda h, bi=bi, k=k, i=i: h.matmul(out=bank(bi)[:, 0:128], lhsT=hT[:, k, c0 + i * 128:c0 + (i + 1) * 128],
                                                             rhs=win_v[:, k, 1664:1792], start=(k == 0), stop=(k == NCH - 1)),
                   reads=WK + hkeys, writes=[bk(bi)])
            op("dve", lambda h, bi=bi, i=i: h.tensor_tensor(out=vaug[:, 1 + i, :, 0:64],
                                                            in0=bank(bi)[:, 0:128].rearrange("p (a d) -> p a d", d=64),
                                                            in1=bv_bc[:, l, :].rearrange("p (a d) -> p a d", d=64), op=ALU.add),
               reads=[bk(bi), "bv_bc%d" % l, "vaug"], writes=[("vaug", 1 + i)])
        VK = [("vaug", 1 + i) for i in range(nsb)]
        op("pool", lambda h: h.tensor_copy(out=utail[:, l, :, :], in_=uT[:, :, W:W + 30]), reads=UC + ["uT"], writes=[("utail", l)])
        op("pool", lambda h: h.tensor_copy(out=kprev[:, l, :], in_=kT[:, W:W + 128]), reads=["kTn", "kT"], writes=[("kprev", l)])
        op("pool", lambda h: h.tensor_copy(out=vprev[:, l, :, :], in_=vaug[:, nsb, :, :]), reads=VK + ["vaug"], writes=[("vprev", l)])
        if not full:
            return
        for c in range(4):
            bi = next_bank()
            for k in range(CONV_K):
                sl = dgc[0] % NDG
                dgc[0] += 1
                de = ("act", "dve")[dgc[0] % 2]
                if de == "act":
                    op("act", lambda h, c=c, k=k, sl=sl: h.activation(out=dg[:, sl, :], in_=ident_bf[:], func=AF.Copy,
                                                                     scale=cw[:, l, c, k:k + 1]),
                       reads=["ident_bf"] + CW_KEYS, writes=[("dg", sl)])
                else:
                    op(de, lambda h, c=c, k=k, sl=sl: h.tensor_scalar(out=dg[:, sl, :], in0=ident_bf[:], scalar1=cw[:, l, c, k:k + 1],
                                                                      scalar2=None, op0=ALU.mult),
                       reads=["ident_bf"] + CW_KEYS, writes=[("dg", sl)])
                op("pe", lambda h, c=c, k=k, sl=sl, bi=bi: h.matmul(out=bank(bi)[:, 0:W], lhsT=dg[:, sl, :], rhs=uT[:, c, k:k + W],
                                                                    start=(k == 0), stop=(k == CONV_K - 1)),
                   reads=[("dg", sl), ("uTc", c), "uT"], writes=[bk(bi)])
            op("act", lambda h, c=c, bi=bi: h.activation(out=yconv[:, c, 0:W], in_=bank(bi)[:, 0:W], func=AF.Identity,
                                                         bias=cb[:, l, c:c + 1]),
               reads=[bk(bi), "cb"], writes=[("yc", c)])
        YC = [("yc", c) for c in range(4)]
        op("act", lambda h: h.activation(out=ysq[:, :, 0:W], in_=yconv[:, :, 0:W], func=AF.Square), reads=YC, writes=["ysq"])
        b1, b2 = next_bank(), next_bank()
        for c in range(4):
            op("pe", lambda h, c=c: h.matmul(out=bank(b1)[:, 0:W], lhsT=ones32[:], rhs=yconv[:, c, 0:W], start=(c == 0), stop=(c == 3)),
               reads=YC + ["ones32"], writes=[bk(b1)])
        for c in range(4):
            op("pe", lambda h, c=c: h.matmul(out=bank(b2)[:, 0:W], lhsT=ones32[:], rhs=ysq[:, c, 0:W], start=(c == 0), stop=(c == 3)),
               reads=["ysq", "ones32"], writes=[bk(b2)])
        mean, var = lnt[0], lnt[1]
        op("act", lambda h: h.activation(out=mean[:, 0:W], in_=bank(b1)[:, 0:W], func=AF.Copy, scale=1.0 / 512), reads=[bk(b1)], writes=["mean"])
        op("dve", lambda h: h.tensor_tensor(out=var[:, 0:W], in0=mean[:, 0:W], in1=mean[:, 0:W], op=ALU.mult), reads=["mean"], writes=["var"])
        op("dve", lambda h: h.scalar_tensor_tensor(out=var[:, 0:W], in0=bank(b2)[:, 0:W], scalar=1.0 / 512, in1=var[:, 0:W],
                                                   op0=ALU.mult, op1=ALU.subtract), reads=[bk(b2), "var"], writes=["var"])
        op("act", lambda h: h.activation(out=var[:, 0:W], in_=var[:, 0:W], func=AF.Sqrt, bias=EPS), reads=["var"], writes=["var"])
        op("dve", lambda h: h.reciprocal(out=var[:, 0:W], in_=var[:, 0:W]), reads=["var"], writes=["var"])
        op("dve", lambda h: h.tensor_tensor(out=yconv[:, :, 0:W], in0=yconv[:, :, 0:W],
                                            in1=mean[:, 0:W].unsqueeze(1).to_broadcast([128, 4, W]), op=ALU.subtract),
           reads=YC + ["mean"], writes=YC)
        op("dve", lambda h: h.tensor_tensor(out=yconv[:, :, 0:W], in0=yconv[:, :, 0:W],
                                            in1=var[:, 0:W].unsqueeze(1).to_broadcast([128, 4, W]), op=ALU.mult),
           reads=YC + ["var"], writes=YC)
        for c in range(4):
            op("act", lambda h, c=c: h.activation(out=yT[:, c, 0:W], in_=yconv[:, c, 0:W], func=AF.Silu,
                                                  bias=lb[:, l, c:c + 1], scale=lg[:, l, c:c + 1]),
               reads=YC + ["lg", "lb"], writes=[("yT", c)])
        def att_scores(i):
            for kv in range(2):
                r0 = 64 * kv
                for kb in range(2):
                    bi = next_bank()
                    while bi >= 6:
                        bi = next_bank()
                    op("pe", lambda h, bi=bi, kb=kb, r0=r0: h.matmul(
                        out=bank(bi)[:, :], lhsT=kT[r0:r0 + 64, (i + kb) * 128:(i + kb + 1) * 128],
                        rhs=qT[r0:r0 + 64, :, i * 128:(i + 1) * 128], start=True, stop=True),
                       reads=["kT", "kTn"] + [("qT", j) for j in range(4)], writes=[bk(bi)])
                    es = kb
                    ps_ = 4 * (i % 2) + 2 * kv + kb
                    op("act", lambda h, bi=bi, es=es: h.activation(out=eT[es][:], in_=bank(bi)[:, :], func=AF.Exp, scale=0.125),
                       reads=[bk(bi)], writes=[("eT", es)])
                    mk_ = maskp if kb == 0 else maskc
                    op("pool", lambda h, es=es, ps_=ps_, mk_=mk_: h.tensor_tensor(
                        out=pT[ps_][:, :].rearrange("p (j q) -> p j q", q=128), in0=eT[es][:, :].rearrange("p (j q) -> p j q", q=128),
                        in1=mk_[:, :].unsqueeze(1).to_broadcast([128, 4, 128]), op=ALU.mult),
                       reads=[("eT", es), "maskp", "maskc"], writes=[("pT", ps_)])

        def att_pv(i):
            po = pq[3]
            for kv in range(2):
                for j in range(4):
                    hh = kv * 4 + j
                    for kb in range(2):
                        ps_ = 4 * (i % 2) + 2 * kv + kb
                        op("pe", lambda h, hh=hh, kv=kv, j=j, kb=kb, ps_=ps_: h.matmul(
                            out=po[:, hh * 128:hh * 128 + 65], lhsT=pT[ps_][:, j * 128:(j + 1) * 128],
                            rhs=vaug[:, i + kb, kv, :], start=(kb == 0), stop=(kb == 1)),
                           reads=[("pT", ps_), "vaug"] + VK, writes=[bk(6 + hh // 4)])
            pov = po[:, :].rearrange("p (h d) -> p h d", d=128)
            op("dve", lambda h: h.tensor_tensor(out=dent[:, 0:8], in0=pov[:, :, 64], in1=esink[:, l, :], op=ALU.add),
               reads=[bk(6), bk(7), "esink"], writes=["dent"])
            op("dve", lambda h: h.reciprocal(out=dent[:, 8:16], in_=dent[:, 0:8]), reads=["dent"], writes=["dent2"])
            op("dve", lambda h: h.tensor_tensor(out=o_sb[:, :].rearrange("p (h d) -> p h d", d=64), in0=pov[:, :, 0:64],
                                                in1=dent[:, 8:16].unsqueeze(2).to_broadcast([128, 8, 64]), op=ALU.mult),
               reads=[bk(6), bk(7), "dent2"], writes=["o_sb"])
            bi = next_bank()
            while bi >= 6:
                bi = next_bank()
            for j in range(4):
                op("pe", lambda h, bi=bi, j=j: h.transpose(out=bank(bi)[:, j * 128:(j + 1) * 128], in_=o_sb[:, j * 128:(j + 1) * 128],
                                                           identity=ident[:]),
                   reads=["o_sb", "ident"], writes=[bk(bi)])
            op("act", lambda h, bi=bi: h.activation(out=yT[:, 4:8, i * 128:(i + 1) * 128],
                                                    in_=bank(bi)[:, :].rearrange("p (j q) -> p j q", q=128), func=AF.Copy),
               reads=[bk(bi)], writes=[("yTa", i)])

        att_scores(0)
        for i in range(nsb):
            if i + 1 < nsb:
                att_scores(i + 1)
            att_pv(i)
        GK = [("gu", s) for s in range(4)]
        for i in range(nsb):
            b = b0 + i
            for half in range(2):
                bi = next_bank()
                for c in range(NCH):
                    op("pe", lambda h, bi=bi, c=c, i=i, half=half: h.matmul(
                        out=bank(bi)[:, :], lhsT=yT[:, c, i * 128:(i + 1) * 128], rhs=wout_v[:, c, half * 512:(half + 1) * 512],
                        start=(c == 0), stop=(c == NCH - 1)),
                       reads=GK + [("yT", c) for c in range(4)] + [("yTa", i)], writes=[bk(bi)])
                op("dve", lambda h, bi=bi, b=b, half=half: h.tensor_tensor(out=x_sb[:, b, half * 512:(half + 1) * 512],
                                                                           in0=bank(bi)[:, :], in1=x_sb[:, b, half * 512:(half + 1) * 512],
                                                                           op=ALU.add),
                   reads=[bk(bi), ("x", b)], writes=[("x", b)])

    def swiglu_phase(units, nblk, sparse_mode=False, elastic=False):
        T = CAP if sparse_mode else nblk * 128
        ncg = (T + 511) // 512
        groups = []
        for ui, (wg, wu, wd, F, gc) in enumerate(units):
            nchunk = F // 128
            g0 = (nchunk + 1) // 2
            groups.append((ui, 0, g0))
            groups.append((ui, g0, nchunk - g0))
        pairs = []
        for gi, (ui, cs, n) in enumerate(groups):
            j = 0
            while j < n:
                m = min(2, n - j)
                pairs.append((gi, cs + j, m))
                j += m
        NSLOT = 3

        def load_pair(pi):
            gi, cs, m = pairs[pi]
            ui = groups[gi][0]
            wg, wu = units[ui][0], units[ui][1]
            s = pi % NSLOT
            for (w, off) in ((wg, 0), (wu, 1)):
                slot = 2 * s + off
                op("pool", lambda h, w=w, slot=slot, cs=cs, m=m: h.dma_start(
                    out=gu_v(slot)[:, :, 0:128 * m], in_=w[:, cs * 128:(cs + m) * 128].rearrange("(c p) n -> p c n", p=128)),
                   writes=[("gu", slot)], lane=("gu", slot))

        def load_wd(gi, half):
            ui, cs, n = groups[gi]
            wd = units[ui][2]
            op("pool", lambda h: h.dma_start(out=wd_v(half)[:, 0:n, :],
                                             in_=wd[cs * 128:(cs + n) * 128, half * 512:(half + 1) * 512].rearrange("(j p) n -> p j n", p=128)),
               writes=[("wd", half)], lane=("wd", half))

        LOOK = 2
        for pi in range(min(LOOK, len(pairs))):
            load_pair(pi)
        load_wd(0, 0)
        load_wd(0, 1)
        hkeys = ["hTe"] if sparse_mode else [("hT", b) for b in range(nblk)]
        cols, tsets = col_sets(elastic) if sparse_mode else (None, None)
        akey = "actTe" if sparse_mode else "actT"
        abuf = actTe if sparse_mode else actT
        gub = [0]
        pi = 0
        for gi, (ui, cs, n) in enumerate(groups):
            gc = units[ui][4]
            first_group = (gi % 2 == 0)
            if sparse_mode and first_group:
                sparse_pre(gc, nblk, gub, build_s=(gi == 0), elastic=elastic)
            while pi < len(pairs) and pairs[pi][0] == gi:
                _, pcs, m = pairs[pi]
                if pi + LOOK < len(pairs):
                    load_pair(pi + LOOK)
                s = pi % NSLOT
                for jj in range(m):
                    jl = pcs + jj - cs
                    if sparse_mode:
                        bg = gub[0] % 4
                        bu = (gub[0] + 1) % 4
                        gub[0] += 2
                        ss = (gub[0] // 2) % 2
                        for (c0, w, cond) in cols:
                            if cond:
                                ext_begin(gc)
                            for (slot, bi) in ((2 * s, bg), (2 * s + 1, bu)):
                                for k in range(NCH):
                                    op("pe", lambda h, slot=slot, bi=bi, k=k, jj=jj, c0=c0, w=w: h.matmul(
                                        out=bank(bi)[:, c0:c0 + w], lhsT=gu_v(slot)[:, k, jj * 128:(jj + 1) * 128],
                                        rhs=hTe_v[:, k, c0:c0 + w], start=(k == 0), stop=(k == NCH - 1)),
                                       reads=[("gu", slot), "hTeX" if cond else "hTe"], writes=[bk(bi)])
                            sk = ("sgsX" if cond else "sgs", ss)
                            op("act", lambda h, bg=bg, ss=ss, c0=c0, w=w: h.activation(out=sgs[ss][:, c0:c0 + w], in_=bank(bg)[:, c0:c0 + w],
                                                                                      func=AF.Silu),
                               reads=[bk(bg)], writes=[sk])
                            op("dve", lambda h, bu=bu, ss=ss, jl=jl, c0=c0, w=w: h.tensor_tensor(
                                out=actTe[:, jl, c0:c0 + w], in0=bank(bu)[:, c0:c0 + w], in1=sgs[ss][:, c0:c0 + w], op=ALU.mult),
                               reads=[bk(bu), sk], writes=[("actTeX" if cond else "actTe", jl)])
                            if cond:
                                P.end_region()
                        continue
                    for cg in range(ncg):
                        wcols = min(512, T - cg * 512)
                        bg = gub[0] % 4
                        bu = (gub[0] + 1) % 4
                        gub[0] += 2
                        for (slot, bi) in ((2 * s, bg), (2 * s + 1, bu)):
                            for k in range(NCH):
                                op("pe", lambda h, slot=slot, bi=bi, k=k, jj=jj, cg=cg, wcols=wcols: h.matmul(
                                    out=bank(bi)[:, 0:wcols], lhsT=gu_v(slot)[:, k, jj * 128:(jj + 1) * 128],
                                    rhs=hT[:, k, cg * 512:cg * 512 + wcols], start=(k == 0), stop=(k == NCH - 1)),
                                   reads=[("gu", slot)] + hkeys, writes=[bk(bi)])
                        ss = (gub[0] // 2) % 2
                        op("act", lambda h, bg=bg, ss=ss, wcols=wcols: h.activation(out=sgs[ss][:, 0:wcols], in_=bank(bg)[:, 0:wcols],
                                                                                   func=AF.Silu),
                           reads=[bk(bg)], writes=[("sgs", ss)])
                        op("dve", lambda h, bu=bu, ss=ss, jl=jl, cg=cg, wcols=wcols: h.tensor_tensor(
                            out=actT[:, jl, cg * 512:cg * 512 + wcols], in0=bank(bu)[:, 0:wcols], in1=sgs[ss][:, 0:wcols], op=ALU.mult),
                           reads=[bk(bu), ("sgs", ss)], writes=[("actT", jl)])
                pi += 1
            if sparse_mode:
                if (not first_group) and gi + 1 < len(groups):
                    sparse_sbuild(units[groups[gi + 1][0]][4], nblk)
                for half in range(2):
                    for tiles, cond in tsets:
                        if cond:
                            ext_begin(gc)
                        for i in tiles:
                            wi = TW[i]
                            bi = 4 + (i % 2)
                            for jl in range(n):
                                op("pe", lambda h, bi=bi, jl=jl, i=i, half=half, n=n, wi=wi: h.matmul(
                                    out=bank(bi)[0:wi, :], lhsT=actTe[:, jl, i * 128:i * 128 + wi], rhs=wd_v(half)[:, jl, :],
                                    start=(jl == 0), stop=(jl == n - 1)),
                                   reads=[("actTeX" if cond else "actTe", jl), ("wd", half)], writes=[bk(bi)])
                            yv = ye_v[0:wi, i, half * 512:(half + 1) * 512]
                            if first_group:
                                op("act", lambda h, bi=bi, yv=yv, i=i, wi=wi: h.activation(out=yv, in_=bank(bi)[0:wi, :], func=AF.Copy,
                                                                                          scale=gpos[0:wi, i:i + 1]),
                                   reads=[bk(bi), ("gpos", i)], writes=[("ye", i, half)])
                            else:
                                op("dve", lambda h, bi=bi, yv=yv, i=i, wi=wi: h.scalar_tensor_tensor(
                                    out=yv, in0=bank(bi)[0:wi, :], scalar=gpos[0:wi, i:i + 1], in1=yv, op0=ALU.mult, op1=ALU.add),
                                   reads=[bk(bi), ("gpos", i), ("ye", i, half)], writes=[("ye", i, half)])
                        if cond:
                            P.end_region()
                    if gi + 1 < len(groups):
                        load_wd(gi + 1, half)
                if not first_group:
                    sparse_scatter(gc, nblk, elastic=elastic)
                continue
            for half in range(2):
                for b in range(nblk):
                    bi = 4 + (b % 2)
                    for jl in range(n):
                        op("pe", lambda h, bi=bi, jl=jl, b=b, half=half, n=n: h.matmul(
                            out=bank(bi)[:, :], lhsT=actT[:, jl, b * 128:(b + 1) * 128], rhs=wd_v(half)[:, jl, :],
                            start=(jl == 0), stop=(jl == n - 1)),
                           reads=[("actT", jl), ("wd", half)], writes=[bk(bi)])
                    xs = x_sb[:, b, half * 512:(half + 1) * 512]
                    if gc is None:
                        op("dve", lambda h, bi=bi, xs=xs: h.tensor_tensor(out=xs, in0=bank(bi)[:, :], in1=xs, op=ALU.add),
                           reads=[bk(bi), ("x", b)], writes=[("x", b)])
                    else:
                        op("dve", lambda h, bi=bi, xs=xs, b=b, gc=gc: h.scalar_tensor_tensor(
                            out=xs, in0=bank(bi)[:, :], scalar=gates[:, b, gc:gc + 1], in1=xs, op0=ALU.mult, op1=ALU.add),
                           reads=[bk(bi), ("x", b), ("gates", b)], writes=[("x", b)])
                if gi + 1 < len(groups):
                    load_wd(gi + 1, half)

    def final_store(nblk, row0):
        op("sp", lambda h: h.dma_start(out=bo_bc[:], in_=final_norm.partition_broadcast(128)), writes=["bo_bc"], lane="bo_bc")
        for b in range(nblk):
            s = b % 2
            ssc = stat[:, 4 * s:4 * s + 1]
            rsc = stat[:, 4 * s + 1:4 * s + 2]
            op("act", lambda h, b=b, ssc=ssc: h.activation(out=sqj[:], in_=x_sb[:, b, :], func=AF.Square, accum_out=ssc),
               reads=[("x", b)], writes=["sqj", ("stat", s)])
            op("act", lambda h, ssc=ssc, rsc=rsc: h.activation(out=rsc, in_=ssc, func=AF.Sqrt, bias=EPS, scale=1.0 / D),
               reads=[("stat", s)], writes=[("statr", s)])
            op("dve", lambda h, rsc=rsc: h.reciprocal(out=rsc, in_=rsc), reads=[("statr", s)], writes=[("statr", s)])
            op("dve", lambda h, b=b, s=s, rsc=rsc: h.scalar_tensor_tensor(out=ost[s], in0=x_sb[:, b, :], scalar=rsc, in1=bo_bc[:],
                                                                          op0=ALU.mult, op1=ALU.mult),
               reads=[("x", b), ("statr", s), "bo_bc"], writes=[("ost", s)])
            op("sp", lambda h, b=b, s=s: h.dma_start(out=out_d[row0 + b * 128:row0 + (b + 1) * 128, :], in_=ost[s]),
               reads=[("ost", s)], lane=("ost", s))

    ffn_units = [(ffn_wg[0], ffn_wu[0], ffn_wd[0], D_FF, None)]
    moe_units = [(moe_wg[0, e], moe_wu[0, e], moe_wd[0, e], D_FFE, e) for e in range(NEXP)]

    def load_x(tok0, nblk):
        for b in range(nblk):
            op("sp", lambda h, b=b: h.dma_start(out=x_sb[:, b, :], in_=x_d[tok0 + b * 128:tok0 + (b + 1) * 128, :]),
               writes=[("x", b)], lane=("x", b))

    def mixer_layer(l, nblk, full=True):
        load_mixer_weights(l)
        rms_transpose(gA, l, nblk)
        P.retire(ACT_KEYS, MIX_KEYS + [("uTc", c) for c in range(4)] + [("yc", c) for c in range(4)])
        b0 = 0
        while b0 < nblk:
            nsb = min(SW // 128, nblk - b0)
            mixer_sub(l, b0, nsb, full=full)
            b0 += nsb
        P.retire(MIX_KEYS + [("uTc", c) for c in range(4)] + [("yc", c) for c in range(4)], ACT_KEYS)

    load_x(0, HALO_BLKS)
    mixer_layer(0, HALO_BLKS)
    rms_transpose(gF, 0, HALO_BLKS)
    swiglu_phase(ffn_units, HALO_BLKS)
    mixer_layer(1, HALO_BLKS, full=False)
    for l in range(2):
        op("dve", lambda h, l=l: h.tensor_scalar(out=vprev[:, l, :, :], in0=vprev[:, l, :, :], scalar1=flag[:, 0:1], scalar2=None,
                                                 op0=ALU.mult), reads=[("vprev", l), "flag"], writes=[("vprev", l)])
        op("dve", lambda h, l=l: h.tensor_scalar(out=utail[:, l, :, :], in0=utail[:, l, :, :], scalar1=flag[:, 0:1], scalar2=None,
                                                 op0=ALU.mult), reads=[("utail", l), "flag"], writes=[("utail", l)])
    for st in range(n_st):
        tok0 = (HALO_BLKS + st * nb) * 128
        load_x(tok0, nb)
        mixer_layer(0, nb)
        rms_transpose(gF, 0, nb)
        swiglu_phase(ffn_units, nb)
        mixer_layer(1, nb)
        assert sparse
        if True:
            HTK = [("hT", b) for b in range(NBMAX)]
            TKK = [("htok", b) for b in range(NBMAX)]
            P.retire(HTK, TKK)
            rms_moe(nb)
            moe_positions(nb)
            if dbg:
                op("sp", lambda h, st=st: h.dma_start(out=dbg_d[st:st + 1, :], in_=ncnt[0:1, 0:8]), reads=["ncnt"], lane="dbg")
            P.retire(ACT_KEYS, SPARSE_KEYS)
            P.retire([("ost", 0), ("ost", 1)], ["S"])
            P.retire([("hn", 0), ("hn", 1)], [("G", i) for i in range(NT)])
            nr = (nb * 128 + CAP - 1) // CAP
            assert nr <= 4
            cur_st[0] = st
            for r in range(nr):
                op("dve", lambda h, r=r: h.tensor_scalar(out=rposr_t[:, r, 0:8 * nb], in0=rpos[:, 0:8 * nb], scalar1=float(-CAP * r),
                                                         scalar2=None, op0=ALU.add), reads=["rpos"], writes=[("rposr", r)])
            cur_r[0] = 0
            swiglu_phase(moe_units, nb, sparse_mode=True, elastic=(BASE < CAP))
            for r in range(1, nr):
                cur_r[0] = r
                for e in range(NEXP):
                    rgn[0] += 1
                    P.begin_region(cnt8_i[0:1, e:e + 1], CAP * r, light=True, cond_id=("ov", st, e), cond_key="cnt8")
                    swiglu_phase([moe_units[e]], nb, sparse_mode=True, elastic=False)
                    P.end_region()
            cur_r[0] = 0
            P.retire(SPARSE_KEYS, ACT_KEYS)
            P.retire(["S"], [("ost", 0), ("ost", 1)])
            P.retire([("G", i) for i in range(NT)], [("hn", 0), ("hn", 1)])
            P.retire(TKK, HTK)
        final_store(nb, st * nb * 128)

    P.emit(final_lanes=[("ost", 0), ("ost", 1)] + (["dbg"] if dbg else []))
    P.stack.close()
    return nc, P


def _q_perm():
    idx = np.zeros(512, dtype=np.int64)
    for j in range(4):
        for kv in range(2):
            for d in range(64):
                idx[j * 128 + kv * 64 + d] = (kv * 4 + j) * 64 + d
    return idx


def prep_weights(inputs):
    f = lambda a: np.ascontiguousarray(np.asarray(a, dtype=np.float32))
    w = {k: f(v) for k, v in inputs.items() if k != "x"}
    perm = np.arange(IN_COLS)
    perm[1024:1536] = 1024 + _q_perm()
    w["w_in"] = np.ascontiguousarray(w["w_in"][:, :, perm])
    w["b_in"] = np.ascontiguousarray(w["b_in"][:, perm])
    return w


_CACHE = {}


def kernel(**inputs):
    x = np.asarray(inputs["x"], dtype=np.float32)
    B, S, _ = x.shape
    w = prep_weights(inputs)
    n_st, nb = 4, 8
    per = n_st * nb * 128
    halves = S // per
    assert B * halves == N_CORES
    if "prog" not in _CACHE:
        nc, P = build_program(n_st, nb)
        _CACHE["prog"] = (nc, P)
    nc, P = _CACHE["prog"]
    in_maps = []
    for c in range(N_CORES):
        b, hf = c // halves, c % halves
        xs = np.zeros((HALO_BLKS * 128 + per, D), dtype=np.float32)
        xs[HALO_BLKS * 128:] = x[b, hf * per:(hf + 1) * per]
        if hf > 0:
            xs[:HALO_BLKS * 128] = x[b, hf * per - HALO_BLKS * 128:hf * per]
        m = dict(w)
        m["x"] = xs
        m["flag"] = np.full((128, 1), 1.0 if hf > 0 else 0.0, dtype=np.float32)
        in_maps.append(m)
    res = run_bass_kernel_spmd(nc, in_maps, core_ids=list(range(N_CORES)))
    out = np.zeros((B, S, D), dtype=np.float32)
    for c in range(N_CORES):
        b, hf = c // halves, c % halves
        out[b, hf * per:(hf + 1) * per] = res.results[c]["out"]
    return out
```

```python
import contextlib
import numpy as np
import concourse.bass as bass
import concourse.mybir as mybir
from concourse.bass_utils import run_bass_kernel_spmd

F32 = mybir.dt.float32
BF16 = mybir.dt.bfloat16
AF = mybir.ActivationFunctionType
ALU = mybir.AluOpType
AX = mybir.AxisListType

ENGS = ("pe", "act", "dve", "pool", "sp")
SAME_ENGINE_SYNC = True

D = 1024
NCH = 8
IN_COLS = 1792
CONV_K = 31
D_FF = 2816
D_FFE = 3584
NEXP = 8
EPS = 1e-5
HALO_BLKS = 2
N_CORES = 8


class Instr:
    __slots__ = ("eng", "idx", "fn", "waits", "lane", "lane_ord", "needs_inc", "inc_count", "clock", "region")


class Prog:
    def __init__(self, nc):
        self.nc = nc
        self.streams = {e: [] for e in ENGS}
        self.last_w = {}
        self.readers = {}
        self.clock = {e: {} for e in ENGS}
        self.lane_n = {}
        self.lane_last = {}
        self.stack = contextlib.ExitStack()
        self.cur_region = None
        self.regions = []
        self.markers = {}
        self.nbar = 0

    def barrier(self):
        bid = self.nbar
        self.nbar += 1
        for e in ENGS:
            fn, reads, writes, lane = self.markers[e]
            self.op(e, fn, reads=list(reads), writes=list(writes) + [("bar", bid, e)], lane=lane)
        for e in ENGS:
            self.op(e, None, reads=[("bar", bid, e2) for e2 in ENGS if e2 != e], register=False)

    def begin_region(self, cond_ap, thresh, light=False, cond_id=None, cond_key=None):
        if light:
            for e in ENGS:
                if cond_key is not None:
                    self.op(e, None, reads=[cond_key], register=False)
                if self.streams[e]:
                    last = self.streams[e][-1]
                    if last.lane is None and last.fn is not None:
                        last.needs_inc = True
                    else:
                        for x in reversed(self.streams[e]):
                            if x.lane is None and x.fn is not None:
                                x.needs_inc = True
                                break
        else:
            self.barrier()
        self._snap = {e: dict(self.clock[e]) for e in ENGS}
        self.regions.append({"cond": cond_ap, "thresh": thresh, "light": light, "snap": self._snap, "cond_id": cond_id})
        self.cur_region = len(self.regions) - 1

    def end_region(self):
        light = self.regions[self.cur_region]["light"]
        self.cur_region = None
        for e in ENGS:
            self.clock[e] = dict(self._snap[e])
        if not light:
            self.barrier()

    def sb(self, name, shape, dtype):
        return self.stack.enter_context(self.nc.sbuf_tensor("sb_" + name, list(shape), dtype))

    def ps(self, name, shape, dtype):
        return self.stack.enter_context(self.nc.psum_tensor("ps_" + name, list(shape), dtype))

    def _dom(self, p):
        return ("L", p.lane) if p.lane is not None else p.eng

    def _ord(self, p):
        return p.lane_ord if p.lane is not None else p.idx + 1

    def op(self, eng, fn, reads=(), writes=(), lane=None, register=True):
        ins = Instr()
        ins.eng = eng
        ins.fn = fn
        ins.lane = lane
        ins.needs_inc = False
        ins.inc_count = 0
        ins.region = self.cur_region
        st = self.streams[eng]
        ins.idx = len(st)
        deps = []
        for k in reads:
            w = self.last_w.get(k)
            if w is not None:
                deps.append((w, True))
        for k in writes:
            w = self.last_w.get(k)
            if w is not None:
                deps.append((w, True))
            rd = self.readers.get(k)
            if rd:
                for r in rd.values():
                    deps.append((r, False))
        if lane is not None:
            n = self.lane_n.get(lane, 0) + 1
            self.lane_n[lane] = n
            ins.lane_ord = n
            prev = self.lane_last.get(lane)
            if prev is not None:
                deps.append((prev, True))
            self.lane_last[lane] = ins
        else:
            ins.lane_ord = 0
        clk = self.clock[eng]
        waits = []
        for p, raw in deps:
            if p is ins:
                continue
            d = self._dom(p)
            o = self._ord(p)
            if p.lane is None and p.eng == eng:
                if not (SAME_ENGINE_SYNC and raw) or eng in ("pe", "sp"):
                    continue
            if clk.get(d, 0) >= o:
                continue
            waits.append(p)
            if p.region is not None and p.region != self.cur_region:
                src = dict(self.regions[p.region]["snap"][p.eng])
                src[d] = o
            else:
                src = p.clock
            for dd, oo in src.items():
                if clk.get(dd, 0) < oo:
                    clk[dd] = oo
            if p.lane is None:
                p.needs_inc = True
        ins.waits = waits
        c = dict(clk)
        c[self._dom(ins)] = self._ord(ins)
        ins.clock = c
        dom = self._dom(ins)
        for k in (reads if register else ()):
            rd = self.readers.get(k)
            if rd is None:
                rd = self.readers[k] = {}
            rd[dom] = ins
        for k in writes:
            self.last_w[k] = ins
            self.readers[k] = {}
        st.append(ins)
        return ins

    def retire(self, old_keys, new_keys):
        pend = {}

        def add(p):
            d = self._dom(p)
            q = pend.get(d)
            if q is None or self._ord(q) < self._ord(p):
                pend[d] = p
        for k in old_keys:
            w = self.last_w.pop(k, None)
            if w is not None:
                add(w)
            rd = self.readers.pop(k, None)
            if rd:
                for p in rd.values():
                    add(p)
        for k in new_keys:
            self.last_w[k] = None
            self.readers[k] = dict(pend)

    def emit(self, final_lanes=()):
        nc = self.nc
        sems = {e: self.stack.enter_context(nc.semaphore("s_" + e)) for e in ENGS}
        lane_sems = {}
        for i, l in enumerate(self.lane_n):
            lane_sems[l] = self.stack.enter_context(nc.semaphore("l%d" % i))
        for e in ENGS:
            c = 0
            for ins in self.streams[e]:
                if ins.needs_inc:
                    c += 1
                ins.inc_count = c

        def run(e, h):
            stream = self.streams[e]
            reg = [None]
            state = {"cur": None, "guard": None, "first": 0}

            def open_region(R, i0):
                if reg[0] is None:
                    reg[0] = h.alloc_register("creg_" + e)
                rg = self.regions[R]
                cond_ap, thresh = rg["cond"], rg["thresh"]
                h.reg_load(reg[0], cond_ap)
                g = h.If_cmp(reg[0], thresh, "IS_GT")
                g.__enter__()
                state["guard"] = g
                state["first"] = i0

            def close_region(R, i1):
                state["guard"].__exit__(None, None, None)
                body = stream[state["first"]:i1]
                k = sum(1 for x in body if x.needs_inc and x.lane is None)
                pre = stream[state["first"] - 1].inc_count if state["first"] > 0 else 0
                lanes = {}
                for x in body:
                    if x.lane is not None:
                        d = lanes.setdefault(x.lane, [x.lane_ord - 1, 0])
                        d[1] += 1
                if k > 0 or lanes:
                    with h.Else():
                        if k > 0:
                            h.wait_ge(sems[e], pre)
                            h.sem_inc(sems[e], k)
                        for l, (pl, kl) in lanes.items():
                            h.wait_ge(lane_sems[l], 16 * pl)
                            h.sem_inc(lane_sems[l], 16 * kl)

            for i, ins in enumerate(stream):
                if ins.region != state["cur"]:
                    if state["cur"] is not None:
                        close_region(state["cur"], i)
                    if ins.region is not None:
                        open_region(ins.region, i)
                    state["cur"] = ins.region
                for p in ins.waits:
                    if p.lane is not None:
                        h.wait_ge(lane_sems[p.lane], 16 * p.lane_ord)
                    else:
                        h.wait_ge(sems[p.eng], p.inc_count)
                if ins.fn is None:
                    assert not ins.needs_inc
                    continue
                bi = ins.fn(h)
                if ins.lane is not None:
                    bi.then_inc(lane_sems[ins.lane], 16)
                elif ins.needs_inc:
                    bi.then_inc(sems[e], 1)
            if state["cur"] is not None:
                close_region(state["cur"], len(stream))
            if e == "sp":
                for l in final_lanes:
                    h.wait_ge(lane_sems[l], 16 * self.lane_n[l])

        with nc.Block() as block:
            @block.tensor
            def _(h):
                run("pe", h)

            @block.scalar
            def _(h):
                run("act", h)

            @block.vector
            def _(h):
                run("dve", h)

            @block.gpsimd
            def _(h):
                run("pool", h)

            @block.sync
            def _(h):
                run("sp", h)


def build_program(n_st=4, nb=8, cap=384, sparse=True, dbg=False, base=384):
    nc = bass.Bass("TRN2", target_bir_lowering=False)
    ntok = (HALO_BLKS + n_st * nb) * 128
    dr = {}

    def din(name, shape):
        dr[name] = nc.dram_tensor(name, list(shape), F32, kind="ExternalInput").ap()
        return dr[name]

    x_d = din("x", [ntok, D])
    flag_d = din("flag", [128, 1])
    attn_norm = din("attn_norm", [2, D])
    ffn_norm = din("ffn_norm", [2, D])
    w_in = din("w_in", [2, D, IN_COLS])
    b_in = din("b_in", [2, IN_COLS])
    conv_w = din("conv_w", [2, CONV_K, 512])
    conv_b = din("conv_b", [2, 512])
    conv_ln_g = din("conv_ln_g", [2, 512])
    conv_ln_b = din("conv_ln_b", [2, 512])
    sinks = din("sinks", [2, 8])
    w_out = din("w_out", [2, D, D])
    b_out = din("b_out", [2, D])
    ffn_wg = din("ffn_w_gate", [1, D, D_FF])
    ffn_wu = din("ffn_w_up", [1, D, D_FF])
    ffn_wd = din("ffn_w_down", [1, D_FF, D])
    moe_router = din("moe_router", [1, D, NEXP])
    moe_wg = din("moe_w_gate", [1, NEXP, D, D_FFE])
    moe_wu = din("moe_w_up", [1, NEXP, D, D_FFE])
    moe_wd = din("moe_w_down", [1, NEXP, D_FFE, D])
    final_norm = din("final_norm", [D])
    out_d = nc.dram_tensor("out", [n_st * nb * 128, D], F32, kind="ExternalOutput").ap()
    dbg_d = nc.dram_tensor("dbg", [n_st, 8], F32, kind="ExternalOutput").ap() if dbg else None

    scr_gu = nc.dram_tensor("scr_gu", [NEXP, 14, 2, 128, 2048], BF16, kind="Internal").ap()
    scr_wd = nc.dram_tensor("scr_wd", [NEXP, 2, 2, 128, 7168], BF16, kind="Internal").ap()

    P = Prog(nc)
    op = P.op
    uid = [0]

    def lane_name(s):
        return s

    NBMAX = max(nb, HALO_BLKS)
    x_sb = P.sb("x_sb", [128, NBMAX, D], F32)
    hT = P.sb("hT", [128, NCH, NBMAX * 128], BF16)
    hT32 = P.sb("hT32", [128, NCH, 128], F32)
    hn_t = P.sb("hn", [128, 2, D], F32)
    hn = [hn_t[:, i, :] for i in range(2)]
    sqj = P.sb("sqj", [128, D], BF16)
    stat = P.sb("stat", [128, 8], F32)
    wdbuf = P.sb("wdbuf", [128, 14336], BF16)
    gubuf = P.sb("gubuf", [128, 6 * 2048], BF16)
    arena = P.sb("arena", [128, 14336], BF16)
    arena32 = arena.bitcast(F32)
    sgs = [P.sb("sgs%d" % i, [128, 512], BF16) for i in range(2)]
    SW = 512
    qT = P.sb("qT", [128, 4, SW], BF16)
    kT = P.sb("kT", [128, 128 + SW], BF16)
    vaug = P.sb("vaug", [128, 1 + SW // 128, 2, 65], BF16)
    yT = P.sb("yT", [128, NCH, SW], BF16)
    lnt = [P.sb("lnt%d" % i, [128, SW], F32) for i in range(2)]
    sgm = [P.sb("sgm%d" % i, [128, 512], F32) for i in range(2)]
    eT = [P.sb("eT%d" % i, [128, 512], BF16) for i in range(2)]
    pT = [P.sb("pT%d" % i, [128, 512], BF16) for i in range(8)]
    o_sb = P.sb("o_sb", [128, 512], F32)
    dent = P.sb("dent", [128, 16], F32)
    utail = P.sb("utail", [128, 2, 4, 30], BF16)
    NDG = 12
    dg = P.sb("dg", [128, NDG, 128], BF16)
    kprev = P.sb("kprev", [128, 2, 128], BF16)
    vprev = P.sb("vprev", [128, 2, 2, 65], BF16)
    ident = P.sb("ident", [128, 128], F32)
    ones32 = P.sb("ones32", [128, 128], F32)
    maskp = P.sb("maskp", [128, 128], BF16)
    maskc = P.sb("maskc", [128, 128], BF16)
    gA = P.sb("gA", [128, 2, 8], F32)
    gF = P.sb("gF", [128, 2, 8], F32)
    binT = P.sb("binT", [128, 2, 14], F32)
    bv_bc = P.sb("bv_bc", [128, 2, 128], F32)
    cw = P.sb("cw", [128, 2, 4, 32], F32)
    cb = P.sb("cb", [128, 2, 4], F32)
    lg = P.sb("lg", [128, 2, 4], F32)
    lb = P.sb("lb", [128, 2, 4], F32)
    esink = P.sb("esink", [128, 2, 8], F32)
    bo_bc = P.sb("bo_bc", [128, D], F32)
    wr = P.sb("wr", [128, 8, 8], F32)
    flag = P.sb("flag", [128, 1], F32)
    gates = P.sb("gates", [128, NBMAX, 8], F32)
    rt = P.sb("rt", [128, 64], F32)
    ost_t = P.sb("ost", [128, 2, D], F32)
    ost = [ost_t[:, i, :] for i in range(2)]

    I32 = mybir.dt.int32
    CAP = cap
    NT = (CAP + 127) // 128
    TW = [min(128, CAP - 128 * i) for i in range(NT)]
    ident_bf = P.sb("ident_bf", [128, 128], BF16)
    ustrict = P.sb("ustrict", [128, 128], F32)
    iota_row = P.sb("iota_row", [128, CAP], F32)
    mk = P.sb("mk", [128, 8], F32)
    rmask = P.sb("rmask", [128, 64], F32)
    rtot = P.sb("rtot", [128, 64], F32)
    roff = P.sb("roff", [128, 64], F32)
    rpos = P.sb("rpos", [128, 64], F32)
    rposr_t = P.sb("rposr", [128, 4, 64], F32)
    cur_r = [0]
    ghi = P.sb("ghi", [128, 64], BF16)
    g2 = P.sb("g2", [128, 64, 2], BF16)
    gpos = P.sb("gpos", [128, 4], F32)
    ncnt = P.sb("ncnt", [128, 8], F32)
    cnt_i = P.sb("cnt_i", [128, 1], I32)
    cnt8_i = P.sb("cnt8_i", [128, 8], I32)
    BASE = base
    rgn = [0]
    S_v = ost_t.bitcast(BF16)[:, :, :].rearrange("p a n -> p (a n)")[:, 0:8 * CAP].rearrange("p (b n) -> p b n", n=CAP)
    G_v = hn_t.bitcast(BF16)[:, :, :].rearrange("p a n -> p (a n)")[:, 0:NT * 1024].rearrange("p (i n) -> p i n", n=1024)
    pq = [P.ps("pq%d" % i, [128, 1024], F32) for i in range(4)]

    def bank(i):
        return pq[i // 2][:, (i % 2) * 512:(i % 2) * 512 + 512]

    def bk(i):
        return ("pb", i)

    cw_raw = x_sb[0:31, 0, :].rearrange("p (l c) -> p l c", c=512)
    iota_i = x_sb[:, 1, 0:CAP].bitcast(I32)
    actT = arena[:, :].rearrange("p (j n) -> p j n", n=1024)
    UW = 30 + SW
    uT = arena[:, 0:4 * UW].rearrange("p (c n) -> p c n", n=UW)
    Y0 = (2 * UW + 31) // 32 * 32
    yconv = arena32[:, Y0:Y0 + 4 * SW].rearrange("p (c n) -> p c n", n=SW)
    ysq = arena32[:, Y0 + 4 * SW:Y0 + 8 * SW].rearrange("p (c n) -> p c n", n=SW)
    assert Y0 + 8 * SW <= 7168
    actTe = arena[:, 0:14 * CAP].rearrange("p (j n) -> p j n", n=CAP)
    hTe_v = arena[:, 14 * CAP:22 * CAP].rearrange("p (c n) -> p c n", n=CAP)
    ye_v = arena[:, 22 * CAP:22 * CAP + NT * 1024].rearrange("p (i n) -> p i n", n=1024)
    assert 22 * CAP + NT * 1024 <= 14336
    SPARSE_KEYS = ([("actTe", j) for j in range(14)] + [("actTeX", j) for j in range(14)] + ["hTe", "hTeX"]
                   + [("ye", i, hf) for i in range(NT) for hf in range(2)])
    hT_flat = hT[:, :, :].rearrange("p c n -> p (c n)")

    def htok(b):
        return hT_flat[:, b * D:(b + 1) * D]
    ACT_KEYS = [("actT", j) for j in range(14)]
    MIX_KEYS = ["uT", "yconv", "ysq"]
    win_v = wdbuf[:, :].rearrange("p (c n) -> p c n", n=IN_COLS)
    wout_v = gubuf[:, 0:8192].rearrange("p (c n) -> p c n", n=D)
    WD_KEYS = [("wd", 0), ("wd", 1)]
    GU_KEYS = [("gu", s) for s in range(6)]

    def wd_v(h):
        return wdbuf[:, h * 7168:(h + 1) * 7168].rearrange("p (j n) -> p j n", n=512)

    def gu_v(s):
        return gubuf[:, s * 2048:(s + 1) * 2048].rearrange("p (c n) -> p c n", n=256)

    def small_dma(eng, out_ap, in_ap, writes, lane):
        def fn(h):
            with nc.allow_non_contiguous_dma(reason="tiny param load"):
                return h.dma_start(out=out_ap, in_=in_ap)
        op(eng, fn, writes=writes, lane=lane)

    op("pool", lambda h: h.memset(ident[:], 0.0), writes=["ident"])
    op("pool", lambda h: h.affine_select(out=ident[:], in_=ident[:], pattern=[[-1, 128]],
                                        compare_op=ALU.not_equal, fill=1.0, base=0, channel_multiplier=1),
       reads=["ident"], writes=["ident"])
    op("pool", lambda h: h.memset(ones32[:], 1.0), writes=["ones32"])
    op("pool", lambda h: h.memset(maskp[:], 1.0), writes=["maskp"])
    op("pool", lambda h: h.memset(maskc[:], 1.0), writes=["maskc"])
    op("pool", lambda h: h.affine_select(out=maskp[:], in_=maskp[:], pattern=[[-1, 128]],
                                        compare_op=ALU.is_gt, fill=0.0, base=0, channel_multiplier=1),
       reads=["maskp"], writes=["maskp"])
    op("pool", lambda h: h.affine_select(out=maskc[:], in_=maskc[:], pattern=[[1, 128]],
                                        compare_op=ALU.is_ge, fill=0.0, base=0, channel_multiplier=-1),
       reads=["maskc"], writes=["maskc"])
    op("pool", lambda h: h.memset(vaug[:], 1.0), writes=["vaug"])
    op("pool", lambda h: h.memset(vprev[:], 0.0), writes=["vprev"])
    op("pool", lambda h: h.memset(vprev[:, :, :, 64:65], 1.0), reads=["vprev"], writes=["vprev"])
    op("pool", lambda h: h.memset(kprev[:], 0.0), writes=["kprev"])
    op("pool", lambda h: h.memset(utail[:], 0.0), writes=["utail"])
    op("pool", lambda h: h.memset(cw[:], 0.0), writes=["cw"])

    op("pool", lambda h: h.tensor_copy(out=ident_bf[:], in_=ident[:]), reads=["ident"], writes=["ident_bf"])
    op("pool", lambda h: h.memset(ustrict[:], 1.0), writes=["ustrict"])
    op("pool", lambda h: h.affine_select(out=ustrict[:], in_=ustrict[:], pattern=[[1, 128]],
                                        compare_op=ALU.is_gt, fill=0.0, base=0, channel_multiplier=-1),
       reads=["ustrict"], writes=["ustrict"])
    op("pool", lambda h: h.iota(iota_i, pattern=[[1, CAP]], base=0, channel_multiplier=0), writes=[("x", 1)])
    op("pool", lambda h: h.tensor_copy(out=iota_row[:], in_=iota_i), reads=[("x", 1)], writes=["iota_row"])
    op("pool", lambda h: h.memset(mk[:], 0.0), writes=["mk0"])
    P.markers = {
        "pe": (lambda h: h.matmul(out=bank(7)[0:1, 0:1], lhsT=ones32[0:1, 0:1], rhs=ones32[0:1, 0:1], start=True, stop=True),
               ["ones32"], [bk(7)], None),
        "act": (lambda h: h.activation(out=mk[:, 0:1], in_=mk[:, 4:5], func=AF.Copy), ["mk0"], [("mk", "act")], None),
        "dve": (lambda h: h.memset(mk[:, 1:2], 0.0), ["mk0"], [("mk", "dve")], None),
        "pool": (lambda h: h.memset(mk[:, 2:3], 0.0), ["mk0"], [("mk", "pool")], None),
        "sp": (lambda h: h.dma_start(out=mk[:, 3:4], in_=mk[:, 5:6]), ["mk0"], [("mk", "sp")], ("bar", "sp")),
    }
    small_dma("sp", flag[:], flag_d, ["flag"], "c_flag")
    small_dma("sp", gA[:], attn_norm.rearrange("l (c p) -> p l c", p=128), ["gA"], "c_gA")
    small_dma("sp", gF[:], ffn_norm.rearrange("l (c p) -> p l c", p=128), ["gF"], "c_gF")
    small_dma("sp", binT[:], b_in.rearrange("l (c p) -> p l c", p=128), ["binT"], "c_binT")
    for l in range(2):
        small_dma("sp", bv_bc[:, l, :], b_in[l, 1664:1792].partition_broadcast(128), ["bv_bc%d" % l], "c_bv%d" % l)
        small_dma("sp", esink[:, l, :], sinks[l, :].partition_broadcast(128), ["esink%d" % l], "c_es%d" % l)
    small_dma("sp", cw_raw, conv_w.rearrange("l k c -> k l c"), [("x", 0)], "c_cwraw")
    small_dma("sp", cb[:], conv_b.rearrange("l (c p) -> p l c", p=128), ["cb"], "c_cb")
    small_dma("sp", lg[:], conv_ln_g.rearrange("l (c p) -> p l c", p=128), ["lg"], "c_lg")
    small_dma("sp", lb[:], conv_ln_b.rearrange("l (c p) -> p l c", p=128), ["lb"], "c_lb")
    small_dma("sp", wr[:], moe_router[0].rearrange("(c p) e -> p c e", p=128), ["wr"], "c_wr")
    op("act", lambda h: h.activation(out=esink[:], in_=esink[:], func=AF.Exp),
       reads=["esink0", "esink1"], writes=["esink"])
    for l in range(2):
        for c in range(4):
            op("pe", lambda h, l=l, c=c: h.transpose(out=bank(c)[:, 0:31], in_=cw_raw[0:31, l, c * 128:(c + 1) * 128],
                                                     identity=ident[0:31, 0:31]),
               reads=[("x", 0), "ident"], writes=[bk(c)])
            op("dve", lambda h, l=l, c=c: h.tensor_copy(out=cw[:, l, c, 0:31], in_=bank(c)[:, 0:31]),
               reads=[bk(c), "cw"], writes=[("cwT", l, c)])
    CW_KEYS = [("cwT", l, c) for l in range(2) for c in range(4)]

    pbrr = [0]
    dgc = [0]
    cur_st = [0]

    def next_bank():
        i = pbrr[0] % 8
        pbrr[0] += 1
        return i

    def rms_stage_a(b, gain_bc=None):
        s = b % 2
        ssc = stat[:, 4 * s:4 * s + 1]
        rsc = stat[:, 4 * s + 1:4 * s + 2]
        op("act", lambda h: h.activation(out=sqj[:], in_=x_sb[:, b, :], func=AF.Square, accum_out=ssc),
           reads=[("x", b)], writes=["sqj", ("stat", s)])
        op("act", lambda h: h.activation(out=rsc, in_=ssc, func=AF.Sqrt, bias=EPS, scale=1.0 / D),
           reads=[("stat", s)], writes=[("statr", s)])
        op("dve", lambda h: h.reciprocal(out=rsc, in_=rsc), reads=[("statr", s)], writes=[("statr", s)])
        if gain_bc is None:
            op("dve", lambda h: h.tensor_scalar(out=hn[s], in0=x_sb[:, b, :], scalar1=rsc, scalar2=None, op0=ALU.mult),
               reads=[("x", b), ("statr", s)], writes=[("hn", s)])
        else:
            op("dve", lambda h: h.scalar_tensor_tensor(out=hn[s], in0=x_sb[:, b, :], scalar=rsc, in1=gain_bc, op0=ALU.mult, op1=ALU.mult),
               reads=[("x", b), ("statr", s), "bo_bc"], writes=[("hn", s)])

    def rms_transposes(b):
        s = b % 2
        pt = pq[b % 2]
        for c in range(NCH):
            op("pe", lambda h, c=c: h.transpose(out=pt[:, c * 128:(c + 1) * 128], in_=hn[s][:, c * 128:(c + 1) * 128], identity=ident[:]),
               reads=[("hn", s), "ident"], writes=[bk(2 * (b % 2) + c // 4)])
        return pt[:, :].rearrange("p (c t) -> p c t", t=128)

    def rms_transpose(gT, l, nblk):
        rms_stage_a(0)
        for b in range(nblk):
            if b + 1 < nblk:
                rms_stage_a(b + 1)
            ptv = rms_transposes(b)
            gbc = gT[:, l, :].unsqueeze(2).to_broadcast([128, NCH, 128])
            op("dve", lambda h, b=b, ptv=ptv, gbc=gbc: h.tensor_tensor(out=hT[:, :, b * 128:(b + 1) * 128], in0=ptv, in1=gbc, op=ALU.mult),
               reads=[bk(2 * (b % 2)), bk(2 * (b % 2) + 1), "gA", "gF"], writes=[("hT", b)])

    Lall = P.sb("Lall", [128, NBMAX, 8], F32)
    rtb = P.sb("rtb", [128, 6, NBMAX, 8], F32)
    rts = P.sb("rts", [128, 6, NBMAX], F32)

    def router_gates_all(nblk):
        L = Lall[:, 0:nblk, :]
        mk1, L2, mk2, t1 = (rtb[:, i, 0:nblk, :] for i in range(4))
        m1, m2, dd, w1, w2 = (rts[:, i, 0:nblk] for i in range(5))

        def bc(v):
            return v.unsqueeze(2).to_broadcast([128, nblk, 8])
        K = "rt"
        LK = [("Lall", b) for b in range(nblk)]
        op("dve", lambda h: h.tensor_reduce(out=m1, in_=L, axis=AX.X, op=ALU.max), reads=LK, writes=[K])
        op("dve", lambda h: h.tensor_tensor(out=mk1, in0=L, in1=bc(m1), op=ALU.is_equal), reads=LK + [K], writes=[K])
        op("dve", lambda h: h.scalar_tensor_tensor(out=L2, in0=mk1, scalar=-1e30, in1=L, op0=ALU.mult, op1=ALU.add), reads=LK + [K], writes=[K])
        op("dve", lambda h: h.tensor_reduce(out=m2, in_=L2, axis=AX.X, op=ALU.max), reads=[K], writes=[K])
        op("dve", lambda h: h.tensor_tensor(out=mk2, in0=L2, in1=bc(m2), op=ALU.is_equal), reads=[K], writes=[K])
        op("dve", lambda h: h.tensor_tensor(out=dd, in0=m2, in1=m1, op=ALU.subtract), reads=[K], writes=[K])
        op("act", lambda h: h.activation(out=dd, in_=dd, func=AF.Exp), reads=[K], writes=[K])
        op("dve", lambda h: h.tensor_scalar(out=w1, in0=dd, scalar1=1.0, scalar2=None, op0=ALU.add), reads=[K], writes=[K])
        op("dve", lambda h: h.reciprocal(out=w1, in_=w1), reads=[K], writes=[K])
        op("dve", lambda h: h.tensor_tensor(out=w2, in0=dd, in1=w1, op=ALU.mult), reads=[K], writes=[K])
        op("dve", lambda h: h.tensor_tensor(out=mk1, in0=mk1, in1=bc(w1), op=ALU.mult), reads=[K], writes=[K])
        op("dve", lambda h: h.tensor_tensor(out=t1, in0=mk2, in1=bc(w2), op=ALU.mult), reads=[K], writes=[K])
        op("dve", lambda h: h.tensor_tensor(out=gates[:, 0:nblk, :], in0=mk1, in1=t1, op=ALU.add), reads=[K],
           writes=[("gates", b) for b in range(nblk)])

    def rms_moe(nblk):
        op("sp", lambda h: h.dma_start(out=bo_bc[:], in_=ffn_norm[1, :].partition_broadcast(128)), writes=["bo_bc"], lane="bo_bc")
        rms_stage_a(0, bo_bc[:])
        for b in range(nblk):
            if b + 1 < nblk:
                rms_stage_a(b + 1, bo_bc[:])
            s = b % 2
            op("pool", lambda h, b=b, s=s: h.tensor_copy(out=htok(b), in_=hn[s]), reads=[("hn", s)], writes=[("htok", b)])
            ptv = rms_transposes(b)
            op("act", lambda h, ptv=ptv: h.activation(out=hT32[:], in_=ptv, func=AF.Copy),
               reads=[bk(2 * (b % 2)), bk(2 * (b % 2) + 1)], writes=["hT32"])
            rb = 4 + (b % 2)
            for c in range(NCH):
                op("pe", lambda h, c=c, rb=rb: h.matmul(out=bank(rb)[:, 0:8], lhsT=hT32[:, c, :], rhs=wr[:, c, :],
                                                        start=(c == 0), stop=(c == NCH - 1)),
                   reads=["hT32", "wr"], writes=[bk(rb)])
            op("act", lambda h, b=b, rb=rb: h.activation(out=Lall[:, b, :], in_=bank(rb)[:, 0:8], func=AF.Copy),
               reads=[bk(rb)], writes=[("Lall", b)])
        router_gates_all(nblk)

    def moe_positions(nblk):
        n = nblk * 8
        gk = [("gates", b) for b in range(nblk)]
        gflat = gates[:, 0:nblk, :].rearrange("p b e -> p (b e)")
        op("dve", lambda h: h.tensor_scalar(out=rmask[:, 0:n], in0=gflat, scalar1=0.0, scalar2=None, op0=ALU.is_gt),
           reads=gk, writes=["rmask"])
        pb = 6
        op("pe", lambda h: h.matmul(out=bank(pb)[:, 0:n], lhsT=ustrict[:], rhs=rmask[:, 0:n], start=True, stop=True),
           reads=["rmask", "ustrict"], writes=[bk(pb)])
        op("pe", lambda h: h.matmul(out=bank(pb)[:, 64:64 + n], lhsT=ones32[:], rhs=rmask[:, 0:n], start=True, stop=True),
           reads=["rmask", "ones32"], writes=[bk(pb)])
        op("dve", lambda h: h.tensor_copy(out=rtot[:, 0:n], in_=bank(pb)[:, 64:64 + n]), reads=[bk(pb)], writes=["rtot"])
        op("dve", lambda h: h.memset(roff[:, 0:8], 0.0), writes=["roff"])
        for b in range(1, nblk):
            op("dve", lambda h, b=b: h.tensor_tensor(out=roff[:, 8 * b:8 * b + 8], in0=roff[:, 8 * b - 8:8 * b],
                                                     in1=rtot[:, 8 * b - 8:8 * b], op=ALU.add),
               reads=["roff", "rtot"], writes=["roff"])
        op("dve", lambda h: h.tensor_tensor(out=rpos[:, 0:n], in0=bank(pb)[:, 0:n], in1=roff[:, 0:n], op=ALU.add),
           reads=[bk(pb), "roff"], writes=["rpos"])
        op("dve", lambda h: h.scalar_tensor_tensor(out=rpos[:, 0:n], in0=rpos[:, 0:n], scalar=1.0, in1=rmask[:, 0:n],
                                                   op0=ALU.add, op1=ALU.mult), reads=["rpos", "rmask"], writes=["rpos"])
        op("dve", lambda h: h.tensor_scalar(out=rpos[:, 0:n], in0=rpos[:, 0:n], scalar1=-1.0, scalar2=None, op0=ALU.add),
           reads=["rpos"], writes=["rpos"])
        lb_ = 8 * (nblk - 1)
        op("dve", lambda h: h.tensor_tensor(out=ncnt[:, 0:8], in0=roff[:, lb_:lb_ + 8], in1=rtot[:, lb_:lb_ + 8], op=ALU.add),
           reads=["roff", "rtot"], writes=["ncnt"])
        op("dve", lambda h: h.tensor_reduce(out=rt[:, 48:49], in_=ncnt[:, 0:8], axis=AX.X, op=ALU.max), reads=["ncnt"], writes=["rtmax"])
        op("dve", lambda h: h.tensor_copy(out=cnt_i[:], in_=rt[:, 48:49]), reads=["rtmax"], writes=["cnt_i"])
        op("dve", lambda h: h.tensor_copy(out=cnt8_i[:], in_=ncnt[:, 0:8]), reads=["ncnt"], writes=["cnt8"])
        op("dve", lambda h: h.tensor_copy(out=ghi[:, 0:n], in_=gflat), reads=gk, writes=["ghi"])
        op("dve", lambda h: h.tensor_copy(out=g2[:, 0:n, 0], in_=ghi[:, 0:n]), reads=["ghi"], writes=["g2"])
        op("dve", lambda h: h.tensor_tensor(out=g2[:, 0:n, 1], in0=gflat, in1=ghi[:, 0:n], op=ALU.subtract),
           reads=gk + ["ghi", "g2"], writes=["g2"])

    def sparse_sbuild(e, nblk):
        for b in range(nblk):
            op("dve", lambda h, b=b, r_=cur_r[0]: h.tensor_scalar(out=S_v[:, b, :], in0=iota_row[:], scalar1=rposr_t[:, r_, 8 * b + e:8 * b + e + 1],
                                                     scalar2=None, op0=ALU.is_equal),
               reads=[("rposr", cur_r[0]), "iota_row"], writes=["S"])

    def ext_begin(e):
        rgn[0] += 1
        P.begin_region(cnt8_i[0:1, e:e + 1], BASE, light=True, cond_id=("n", rgn[0] // 100000, cur_st[0], e), cond_key="cnt8")

    def col_sets(elastic):
        if elastic and BASE < CAP:
            nbt = BASE // 128
            return [(0, BASE, False), (BASE, CAP - BASE, True)], [(list(range(nbt)), False), (list(range(nbt, NT)), True)]
        return [(0, CAP, False)], [(list(range(NT)), False)]

    def sparse_pre(e, nblk, gub, build_s=True, elastic=False):
        if build_s:
            sparse_sbuild(e, nblk)
        cols, tsets = col_sets(elastic)
        for (c0, w, cond), (tiles, _) in zip(cols, tsets):
            if cond:
                ext_begin(e)
            hk = "hTeX" if cond else "hTe"
            for c in range(NCH):
                bi = gub[0] % 4
                gub[0] += 1
                for b in range(nblk):
                    op("pe", lambda h, bi=bi, b=b, c=c, c0=c0, w=w: h.matmul(out=bank(bi)[:, c0:c0 + w], lhsT=htok(b)[:, c * 128:(c + 1) * 128],
                                                                             rhs=S_v[:, b, c0:c0 + w], start=(b == 0), stop=(b == nblk - 1)),
                       reads=["S", ("htok", b)], writes=[bk(bi)])
                op("act", lambda h, bi=bi, c=c, c0=c0, w=w: h.activation(out=hTe_v[:, c, c0:c0 + w], in_=bank(bi)[:, c0:c0 + w], func=AF.Copy),
                   reads=[bk(bi)], writes=[hk])
            gub[0] += gub[0] % 2
            for i in tiles:
                wi = TW[i]
                for b in range(nblk):
                    op("pe", lambda h, i=i, b=b, wi=wi: h.matmul(out=bank(6)[0:wi, 2 * i:2 * i + 2], lhsT=S_v[:, b, i * 128:i * 128 + wi],
                                                                 rhs=g2[:, 8 * b + e, :], start=(b == 0), stop=(b == nblk - 1)),
                       reads=["S", "g2"], writes=[bk(6)])
            for i in tiles:
                wi = TW[i]
                g6 = bank(6)[0:wi, 2 * i:2 * i + 2]
                op("dve", lambda h, i=i, wi=wi, g6=g6: h.tensor_reduce(out=gpos[0:wi, i:i + 1], in_=g6, axis=AX.X, op=ALU.add),
                   reads=[bk(6)], writes=[("gpos", i)])
            pqb = pq[3].bitcast(BF16)
            for i in tiles:
                wi = TW[i]
                for b in range(nblk):
                    op("pe", lambda h, i=i, b=b, wi=wi: h.transpose(out=pqb[0:wi, 1024 + b * 128:1024 + (b + 1) * 128],
                                                                    in_=S_v[:, b, i * 128:i * 128 + wi], identity=ident_bf[:]),
                       reads=["S", "ident_bf"], writes=[bk(7)])
                op("act", lambda h, i=i, wi=wi: h.activation(out=G_v[0:wi, i, 0:nblk * 128], in_=pqb[0:wi, 1024:1024 + nblk * 128], func=AF.Copy),
                   reads=[bk(7)], writes=[("G", i)])
            if cond:
                P.end_region()

    def sparse_scatter(e, nblk, elastic=False):
        cols, tsets = col_sets(elastic)
        for tiles, cond in tsets:
            if cond:
                ext_begin(e)
            for b in range(nblk):
                for half in range(2):
                    bi = 4 + ((2 * b + half) % 2)
                    for i in tiles:
                        wi = TW[i]
                        op("pe", lambda h, bi=bi, i=i, b=b, half=half, wi=wi, tiles=tiles: h.matmul(
                            out=bank(bi)[:, :], lhsT=G_v[0:wi, i, b * 128:(b + 1) * 128], rhs=ye_v[0:wi, i, half * 512:(half + 1) * 512],
                            start=(i == tiles[0]), stop=(i == tiles[-1])),
                           reads=[("G", i), ("ye", i, half)], writes=[bk(bi)])
                    xs = x_sb[:, b, half * 512:(half + 1) * 512]
                    op("dve", lambda h, bi=bi, xs=xs: h.tensor_tensor(out=xs, in0=bank(bi)[:, :], in1=xs, op=ALU.add),
                       reads=[bk(bi), ("x", b)], writes=[("x", b)])
            if cond:
                P.end_region()

    def load_mixer_weights(l):
        for hh in range(2):
            op("pool", lambda h, hh=hh: h.dma_start(out=win_v[:, 4 * hh:4 * hh + 4, :],
                                                    in_=w_in[l, 512 * hh:512 * hh + 512, :].rearrange("(c p) n -> p c n", p=128)),
               writes=[("wd", hh)], lane=("wd", hh))
        for hh in range(2):
            op("pool", lambda h, hh=hh: h.dma_start(out=wout_v[:, 4 * hh:4 * hh + 4, :],
                                                    in_=w_out[l, 512 * hh:512 * hh + 512, :].rearrange("(c p) n -> p c n", p=128)),
               writes=[("gu", 2 * hh), ("gu", 2 * hh + 1)], lane=("gu", 2 * hh))
        op("sp", lambda h: h.dma_start(out=bo_bc[:], in_=b_out[l, :].partition_broadcast(128)), writes=["bo_bc"], lane="bo_bc")

    def mixer_sub(l, b0, nsb, full=True):
        W = nsb * 128
        c0 = b0 * 128
        WK = WD_KEYS
        op("pool", lambda h: h.tensor_copy(out=uT[:, :, 0:30], in_=utail[:, l, :, :]), reads=[("utail", l), "uT"], writes=["uT"])
        op("pool", lambda h: h.tensor_copy(out=kT[:, 0:128], in_=kprev[:, l, :]), reads=[("kprev", l), "kT"], writes=["kT"])
        op("pool", lambda h: h.tensor_copy(out=vaug[:, 0, :, :], in_=vprev[:, l, :, :]), reads=[("vprev", l), "vaug"], writes=["vaug"])
        if full:
            for b in range(b0, b0 + nsb):
                op("pool", lambda h, b=b: h.tensor_tensor(out=x_sb[:, b, :], in0=x_sb[:, b, :], in1=bo_bc[:], op=ALU.add),
                   reads=[("x", b), "bo_bc"], writes=[("x", b)])
        hkeys = [("hT", b) for b in range(b0, b0 + nsb)]
        for c in range(4):
            ia, ig = next_bank(), next_bank()
            for (m, bi) in ((c, ia), (4 + c, ig)):
                for k in range(NCH):
                    op("pe", lambda h, m=m, bi=bi, k=k: h.matmul(out=bank(bi)[:, 0:W], lhsT=win_v[:, k, m * 128:(m + 1) * 128],
                                                                 rhs=hT[:, k, c0:c0 + W], start=(k == 0), stop=(k == NCH - 1)),
                       reads=WK + hkeys, writes=[bk(bi)])
            s = c % 2
            op("act", lambda h, c=c, ig=ig, s=s: h.activation(out=sgm[s][:, 0:W], in_=bank(ig)[:, 0:W], func=AF.Sigmoid,
                                                             bias=binT[:, l, 4 + c:5 + c]),
               reads=[bk(ig), "binT"], writes=[("sgm", s)])
            op("dve", lambda h, c=c, ia=ia, s=s: h.scalar_tensor_tensor(out=uT[:, c, 30:30 + W], in0=bank(ia)[:, 0:W],
                                                                       scalar=binT[:, l, c:c + 1], in1=sgm[s][:, 0:W],
                                                                       op0=ALU.add, op1=ALU.mult),
               reads=[bk(ia), ("sgm", s), "binT", "uT"], writes=[("uTc", c)])
        UC = [("uTc", c) for c in range(4)]
        for j in range(4):
            bi = next_bank()
            for k in range(NCH):
                op("pe", lambda h, j=j, bi=bi, k=k: h.matmul(out=bank(bi)[:, 0:W], lhsT=win_v[:, k, (8 + j) * 128:(9 + j) * 128],
                                                             rhs=hT[:, k, c0:c0 + W], start=(k == 0), stop=(k == NCH - 1)),
                   reads=WK + hkeys, writes=[bk(bi)])
            op("act", lambda h, j=j, bi=bi: h.activation(out=qT[:, j, 0:W], in_=bank(bi)[:, 0:W], func=AF.Identity,
                                                         bias=binT[:, l, 8 + j:9 + j]),
               reads=[bk(bi), "binT"], writes=[("qT", j)])
        bi = next_bank()
        for k in range(NCH):
            op("pe", lambda h, bi=bi, k=k: h.matmul(out=bank(bi)[:, 0:W], lhsT=win_v[:, k, 1536:1664],
                                                    rhs=hT[:, k, c0:c0 + W], start=(k == 0), stop=(k == NCH - 1)),
               reads=WK + hkeys, writes=[bk(bi)])
        op("act", lambda h, bi=bi: h.activation(out=kT[:, 128:128 + W], in_=bank(bi)[:, 0:W], func=AF.Identity,
                                                bias=binT[:, l, 12:13]),
           reads=[bk(bi), "binT", "kT"], writes=["kTn"])
        for i in range(nsb):
            bi = next_bank()
            for k in range(NCH):
                op("pe", lambda h, bi=bi, k=k, i=i: h.matmul(out=bank(bi)[:, 0:128], lhsT=hT[:, k, c0 + i * 128:c0 + (i + 1) * 128],
                                                             rhs=win_v[:, k, 1664:1792], start=(k == 0), stop=(k == NCH - 1)),
                   reads=WK + hkeys, writes=[bk(bi)])
            op("dve", lambda h, bi=bi, i=i: h.tensor_tensor(out=vaug[:, 1 + i, :, 0:64],
                                                            in0=bank(bi)[:, 0:128].rearrange("p (a d) -> p a d", d=64),
                                                            in1=bv_bc[:, l, :].rearrange("p (a d) -> p a d", d=64), op=ALU.add),
               reads=[bk(bi), "bv_bc%d" % l, "vaug"], writes=[("vaug", 1 + i)])
        VK = [("vaug", 1 + i) for i in range(nsb)]
        op("pool", lambda h: h.tensor_copy(out=utail[:, l, :, :], in_=uT[:, :, W:W + 30]), reads=UC + ["uT"], writes=[("utail", l)])
        op("pool", lambda h: h.tensor_copy(out=kprev[:, l, :], in_=kT[:, W:W + 128]), reads=["kTn", "kT"], writes=[("kprev", l)])
        op("pool", lambda h: h.tensor_copy(out=vprev[:, l, :, :], in_=vaug[:, nsb, :, :]), reads=VK + ["vaug"], writes=[("vprev", l)])
        if not full:
            return
        for c in range(4):
            bi = next_bank()
            for k in range(CONV_K):
                sl = dgc[0] % NDG
                dgc[0] += 1
                de = ("act", "dve")[dgc[0] % 2]
                if de == "act":
                    op("act", lambda h, c=c, k=k, sl=sl: h.activation(out=dg[:, sl, :], in_=ident_bf[:], func=AF.Copy,
                                                                     scale=cw[:, l, c, k:k + 1]),
                       reads=["ident_bf"] + CW_KEYS, writes=[("dg", sl)])
                else:
                    op(de, lambda h, c=c, k=k, sl=sl: h.tensor_scalar(out=dg[:, sl, :], in0=ident_bf[:], scalar1=cw[:, l, c, k:k + 1],
                                                                      scalar2=None, op0=ALU.mult),
                       reads=["ident_bf"] + CW_KEYS, writes=[("dg", sl)])
                op("pe", lambda h, c=c, k=k, sl=sl, bi=bi: h.matmul(out=bank(bi)[:, 0:W], lhsT=dg[:, sl, :], rhs=uT[:, c, k:k + W],
                                                                    start=(k == 0), stop=(k == CONV_K - 1)),
                   reads=[("dg", sl), ("uTc", c), "uT"], writes=[bk(bi)])
            op("act", lambda h, c=c, bi=bi: h.activation(out=yconv[:, c, 0:W], in_=bank(bi)[:, 0:W], func=AF.Identity,
                                                         bias=cb[:, l, c:c + 1]),
               reads=[bk(bi), "cb"], writes=[("yc", c)])
        YC = [("yc", c) for c in range(4)]
        op("act", lambda h: h.activation(out=ysq[:, :, 0:W], in_=yconv[:, :, 0:W], func=AF.Square), reads=YC, writes=["ysq"])
        b1, b2 = next_bank(), next_bank()
        for c in range(4):
            op("pe", lambda h, c=c: h.matmul(out=bank(b1)[:, 0:W], lhsT=ones32[:], rhs=yconv[:, c, 0:W], start=(c == 0), stop=(c == 3)),
               reads=YC + ["ones32"], writes=[bk(b1)])
        for c in range(4):
            op("pe", lambda h, c=c: h.matmul(out=bank(b2)[:, 0:W], lhsT=ones32[:], rhs=ysq[:, c, 0:W], start=(c == 0), stop=(c == 3)),
               reads=["ysq", "ones32"], writes=[bk(b2)])
        mean, var = lnt[0], lnt[1]
        op("act", lambda h: h.activation(out=mean[:, 0:W], in_=bank(b1)[:, 0:W], func=AF.Copy, scale=1.0 / 512), reads=[bk(b1)], writes=["mean"])
        op("dve", lambda h: h.tensor_tensor(out=var[:, 0:W], in0=mean[:, 0:W], in1=mean[:, 0:W], op=ALU.mult), reads=["mean"], writes=["var"])
        op("dve", lambda h: h.scalar_tensor_tensor(out=var[:, 0:W], in0=bank(b2)[:, 0:W], scalar=1.0 / 512, in1=var[:, 0:W],
                                                   op0=ALU.mult, op1=ALU.subtract), reads=[bk(b2), "var"], writes=["var"])
        op("act", lambda h: h.activation(out=var[:, 0:W], in_=var[:, 0:W], func=AF.Sqrt, bias=EPS), reads=["var"], writes=["var"])
        op("dve", lambda h: h.reciprocal(out=var[:, 0:W], in_=var[:, 0:W]), reads=["var"], writes=["var"])
        op("dve", lambda h: h.tensor_tensor(out=yconv[:, :, 0:W], in0=yconv[:, :, 0:W],
                                            in1=mean[:, 0:W].unsqueeze(1).to_broadcast([128, 4, W]), op=ALU.subtract),
           reads=YC + ["mean"], writes=YC)
        op("dve", lambda h: h.tensor_tensor(out=yconv[:, :, 0:W], in0=yconv[:, :, 0:W],
                                            in1=var[:, 0:W].unsqueeze(1).to_broadcast([128, 4, W]), op=ALU.mult),
           reads=YC + ["var"], writes=YC)
        for c in range(4):
            op("act", lambda h, c=c: h.activation(out=yT[:, c, 0:W], in_=yconv[:, c, 0:W], func=AF.Silu,
                                                  bias=lb[:, l, c:c + 1], scale=lg[:, l, c:c + 1]),
               reads=YC + ["lg", "lb"], writes=[("yT", c)])
        def att_scores(i):
            for kv in range(2):
                r0 = 64 * kv
                for kb in range(2):
                    bi = next_bank()
                    while bi >= 6:
                        bi = next_bank()
                    op("pe", lambda h, bi=bi, kb=kb, r0=r0: h.matmul(
                        out=bank(bi)[:, :], lhsT=kT[r0:r0 + 64, (i + kb) * 128:(i + kb + 1) * 128],
                        rhs=qT[r0:r0 + 64, :, i * 128:(i + 1) * 128], start=True, stop=True),
                       reads=["kT", "kTn"] + [("qT", j) for j in range(4)], writes=[bk(bi)])
                    es = kb
                    ps_ = 4 * (i % 2) + 2 * kv + kb
                    op("act", lambda h, bi=bi, es=es: h.activation(out=eT[es][:], in_=bank(bi)[:, :], func=AF.Exp, scale=0.125),
                       reads=[bk(bi)], writes=[("eT", es)])
                    mk_ = maskp if kb == 0 else maskc
                    op("pool", lambda h, es=es, ps_=ps_, mk_=mk_: h.tensor_tensor(
                        out=pT[ps_][:, :].rearrange("p (j q) -> p j q", q=128), in0=eT[es][:, :].rearrange("p (j q) -> p j q", q=128),
                        in1=mk_[:, :].unsqueeze(1).to_broadcast([128, 4, 128]), op=ALU.mult),
                       reads=[("eT", es), "maskp", "maskc"], writes=[("pT", ps_)])

        def att_pv(i):
            po = pq[3]
            for kv in range(2):
                for j in range(4):
                    hh = kv * 4 + j
                    for kb in range(2):
                        ps_ = 4 * (i % 2) + 2 * kv + kb
                        op("pe", lambda h, hh=hh, kv=kv, j=j, kb=kb, ps_=ps_: h.matmul(
                            out=po[:, hh * 128:hh * 128 + 65], lhsT=pT[ps_][:, j * 128:(j + 1) * 128],
                            rhs=vaug[:, i + kb, kv, :], start=(kb == 0), stop=(kb == 1)),
                           reads=[("pT", ps_), "vaug"] + VK, writes=[bk(6 + hh // 4)])
            pov = po[:, :].rearrange("p (h d) -> p h d", d=128)
            op("dve", lambda h: h.tensor_tensor(out=dent[:, 0:8], in0=pov[:, :, 64], in1=esink[:, l, :], op=ALU.add),
               reads=[bk(6), bk(7), "esink"], writes=["dent"])
            op("dve", lambda h: h.reciprocal(out=dent[:, 8:16], in_=dent[:, 0:8]), reads=["dent"], writes=["dent2"])
            op("dve", lambda h: h.tensor_tensor(out=o_sb[:, :].rearrange("p (h d) -> p h d", d=64), in0=pov[:, :, 0:64],
                                                in1=dent[:, 8:16].unsqueeze(2).to_broadcast([128, 8, 64]), op=ALU.mult),
               reads=[bk(6), bk(7), "dent2"], writes=["o_sb"])
            bi = next_bank()
            while bi >= 6:
                bi = next_bank()
            for j in range(4):
                op("pe", lambda h, bi=bi, j=j: h.transpose(out=bank(bi)[:, j * 128:(j + 1) * 128], in_=o_sb[:, j * 128:(j + 1) * 128],
                                                           identity=ident[:]),
                   reads=["o_sb", "ident"], writes=[bk(bi)])
            op("act", lambda h, bi=bi: h.activation(out=yT[:, 4:8, i * 128:(i + 1) * 128],
                                                    in_=bank(bi)[:, :].rearrange("p (j q) -> p j q", q=128), func=AF.Copy),
               reads=[bk(bi)], writes=[("yTa", i)])

        att_scores(0)
        for i in range(nsb):
            if i + 1 < nsb:
                att_scores(i + 1)
            att_pv(i)
        GK = [("gu", s) for s in range(4)]
        for i in range(nsb):
            b = b0 + i
            for half in range(2):
                bi = next_bank()
                for c in range(NCH):
                    op("pe", lambda h, bi=bi, c=c, i=i, half=half: h.matmul(
                        out=bank(bi)[:, :], lhsT=yT[:, c, i * 128:(i + 1) * 128], rhs=wout_v[:, c, half * 512:(half + 1) * 512],
                        start=(c == 0), stop=(c == NCH - 1)),
                       reads=GK + [("yT", c) for c in range(4)] + [("yTa", i)], writes=[bk(bi)])
                op("dve", lambda h, bi=bi, b=b, half=half: h.tensor_tensor(out=x_sb[:, b, half * 512:(half + 1) * 512],
                                                                           in0=bank(bi)[:, :], in1=x_sb[:, b, half * 512:(half + 1) * 512],
                                                                           op=ALU.add),
                   reads=[bk(bi), ("x", b)], writes=[("x", b)])

    def swiglu_phase(units, nblk, sparse_mode=False, elastic=False, wmode="hbm"):
        T = CAP if sparse_mode else nblk * 128
        ncg = (T + 511) // 512
        groups = []
        for ui, (wg, wu, wd, F, gc) in enumerate(units):
            nchunk = F // 128
            g0 = (nchunk + 1) // 2
            groups.append((ui, 0, g0))
            groups.append((ui, g0, nchunk - g0))
        pairs = []
        for gi, (ui, cs, n) in enumerate(groups):
            j = 0
            while j < n:
                m = min(2, n - j)
                pairs.append((gi, cs + j, m))
                j += m
        NSLOT = 3

        def load_pair(pi):
            gi, cs, m = pairs[pi]
            ui = groups[gi][0]
            wg, wu = units[ui][0], units[ui][1]
            e_ = units[ui][4]
            pidx = cs // 2
            s = pi % NSLOT
            for (w, off) in ((wg, 0), (wu, 1)):
                slot = 2 * s + off
                flat = gubuf[:, slot * 2048:(slot + 1) * 2048]
                if wmode == "scr":
                    op("sp", lambda h, slot=slot, flat=flat, off=off: h.dma_start(out=flat, in_=scr_gu[e_, pidx, off]),
                       reads=[("scrgu", e_, pidx, off)], writes=[("gu", slot)], lane=("gu", slot))
                    continue
                op("pool", lambda h, w=w, slot=slot, cs=cs, m=m: h.dma_start(
                    out=gu_v(slot)[:, :, 0:128 * m], in_=w[:, cs * 128:(cs + m) * 128].rearrange("(c p) n -> p c n", p=128)),
                   writes=[("gu", slot)], lane=("gu", slot))
                if wmode == "store":
                    op("sp", lambda h, flat=flat, off=off: h.dma_start(out=scr_gu[e_, pidx, off], in_=flat),
                       reads=[("gu", slot)], writes=[("scrgu", e_, pidx, off)], lane=("sts", slot))

        def load_wd(gi, half):
            ui, cs, n = groups[gi]
            wd = units[ui][2]
            e_ = units[ui][4]
            g_ = gi % 2
            flat = wdbuf[:, half * 7168:(half + 1) * 7168]
            if wmode == "scr":
                op("sp", lambda h: h.dma_start(out=flat, in_=scr_wd[e_, g_, half]),
                   reads=[("scrwd", e_, g_, half)], writes=[("wd", half)], lane=("wd", half))
                return
            op("pool", lambda h: h.dma_start(out=wd_v(half)[:, 0:n, :],
                                             in_=wd[cs * 128:(cs + n) * 128, half * 512:(half + 1) * 512].rearrange("(j p) n -> p j n", p=128)),
               writes=[("wd", half)], lane=("wd", half))
            if wmode == "store":
                op("sp", lambda h: h.dma_start(out=scr_wd[e_, g_, half], in_=flat),
                   reads=[("wd", half)], writes=[("scrwd", e_, g_, half)], lane=("stw", half))

        LOOK = 2
        for pi in range(min(LOOK, len(pairs))):
            load_pair(pi)
        load_wd(0, 0)
        load_wd(0, 1)
        hkeys = ["hTe"] if sparse_mode else [("hT", b) for b in range(nblk)]
        cols, tsets = col_sets(elastic) if sparse_mode else (None, None)
        akey = "actTe" if sparse_mode else "actT"
        abuf = actTe if sparse_mode else actT
        gub = [0]
        pi = 0
        for gi, (ui, cs, n) in enumerate(groups):
            gc = units[ui][4]
            first_group = (gi % 2 == 0)
            if sparse_mode and first_group:
                sparse_pre(gc, nblk, gub, build_s=(gi == 0), elastic=elastic)
            pig = 0
            while pi < len(pairs) and pairs[pi][0] == gi:
                _, pcs, m = pairs[pi]
                if pi + LOOK < len(pairs):
                    load_pair(pi + LOOK)
                pig += 1
                s = pi % NSLOT
                for jj in range(m):
                    jl = pcs + jj - cs
                    if sparse_mode:
                        bg = gub[0] % 4
                        bu = (gub[0] + 1) % 4
                        gub[0] += 2
                        ss = (gub[0] // 2) % 2
                        for (c0, w, cond) in cols:
                            if cond:
                                ext_begin(gc)
                            for (slot, bi) in ((2 * s, bg), (2 * s + 1, bu)):
                                for k in range(NCH):
                                    op("pe", lambda h, slot=slot, bi=bi, k=k, jj=jj, c0=c0, w=w: h.matmul(
                                        out=bank(bi)[:, c0:c0 + w], lhsT=gu_v(slot)[:, k, jj * 128:(jj + 1) * 128],
                                        rhs=hTe_v[:, k, c0:c0 + w], start=(k == 0), stop=(k == NCH - 1)),
                                       reads=[("gu", slot), "hTeX" if cond else "hTe"], writes=[bk(bi)])
                            sk = ("sgsX" if cond else "sgs", ss)
                            op("act", lambda h, bg=bg, ss=ss, c0=c0, w=w: h.activation(out=sgs[ss][:, c0:c0 + w], in_=bank(bg)[:, c0:c0 + w],
                                                                                      func=AF.Silu),
                               reads=[bk(bg)], writes=[sk])
                            op("dve", lambda h, bu=bu, ss=ss, jl=jl, c0=c0, w=w: h.tensor_tensor(
                                out=actTe[:, jl, c0:c0 + w], in0=bank(bu)[:, c0:c0 + w], in1=sgs[ss][:, c0:c0 + w], op=ALU.mult),
                               reads=[bk(bu), sk], writes=[("actTeX" if cond else "actTe", jl)])
                            if cond:
                                P.end_region()
                        continue
                    for cg in range(ncg):
                        wcols = min(512, T - cg * 512)
                        bg = gub[0] % 4
                        bu = (gub[0] + 1) % 4
                        gub[0] += 2
                        for (slot, bi) in ((2 * s, bg), (2 * s + 1, bu)):
                            for k in range(NCH):
                                op("pe", lambda h, slot=slot, bi=bi, k=k, jj=jj, cg=cg, wcols=wcols: h.matmul(
                                    out=bank(bi)[:, 0:wcols], lhsT=gu_v(slot)[:, k, jj * 128:(jj + 1) * 128],
                                    rhs=hT[:, k, cg * 512:cg * 512 + wcols], start=(k == 0), stop=(k == NCH - 1)),
                                   reads=[("gu", slot)] + hkeys, writes=[bk(bi)])
                        ss = (gub[0] // 2) % 2
                        op("act", lambda h, bg=bg, ss=ss, wcols=wcols: h.activation(out=sgs[ss][:, 0:wcols], in_=bank(bg)[:, 0:wcols],
                                                                                   func=AF.Silu),
                           reads=[bk(bg)], writes=[("sgs", ss)])
                        op("dve", lambda h, bu=bu, ss=ss, jl=jl, cg=cg, wcols=wcols: h.tensor_tensor(
                            out=actT[:, jl, cg * 512:cg * 512 + wcols], in0=bank(bu)[:, 0:wcols], in1=sgs[ss][:, 0:wcols], op=ALU.mult),
                           reads=[bk(bu), ("sgs", ss)], writes=[("actT", jl)])
                pi += 1
            if sparse_mode:
                if (not first_group) and gi + 1 < len(groups):
                    sparse_sbuild(units[groups[gi + 1][0]][4], nblk)
                for half in range(2):
                    for tiles, cond in tsets:
                        if cond:
                            ext_begin(gc)
                        for i in tiles:
                            wi = TW[i]
                            bi = 4 + (i % 2)
                            for jl in range(n):
                                op("pe", lambda h, bi=bi, jl=jl, i=i, half=half, n=n, wi=wi: h.matmul(
                                    out=bank(bi)[0:wi, :], lhsT=actTe[:, jl, i * 128:i * 128 + wi], rhs=wd_v(half)[:, jl, :],
                                    start=(jl == 0), stop=(jl == n - 1)),
                                   reads=[("actTeX" if cond else "actTe", jl), ("wd", half)], writes=[bk(bi)])
                            yv = ye_v[0:wi, i, half * 512:(half + 1) * 512]
                            if first_group:
                                op("act", lambda h, bi=bi, yv=yv, i=i, wi=wi: h.activation(out=yv, in_=bank(bi)[0:wi, :], func=AF.Copy,
                                                                                          scale=gpos[0:wi, i:i + 1]),
                                   reads=[bk(bi), ("gpos", i)], writes=[("ye", i, half)])
                            else:
                                op("dve", lambda h, bi=bi, yv=yv, i=i, wi=wi: h.scalar_tensor_tensor(
                                    out=yv, in0=bank(bi)[0:wi, :], scalar=gpos[0:wi, i:i + 1], in1=yv, op0=ALU.mult, op1=ALU.add),
                                   reads=[bk(bi), ("gpos", i), ("ye", i, half)], writes=[("ye", i, half)])
                        if cond:
                            P.end_region()
                    if gi + 1 < len(groups):
                        load_wd(gi + 1, half)
                if not first_group:
                    sparse_scatter(gc, nblk, elastic=elastic)
                continue
            for half in range(2):
                for b in range(nblk):
                    bi = 4 + (b % 2)
                    for jl in range(n):
                        op("pe", lambda h, bi=bi, jl=jl, b=b, half=half, n=n: h.matmul(
                            out=bank(bi)[:, :], lhsT=actT[:, jl, b * 128:(b + 1) * 128], rhs=wd_v(half)[:, jl, :],
                            start=(jl == 0), stop=(jl == n - 1)),
                           reads=[("actT", jl), ("wd", half)], writes=[bk(bi)])
                    xs = x_sb[:, b, half * 512:(half + 1) * 512]
                    if gc is None:
                        op("dve", lambda h, bi=bi, xs=xs: h.tensor_tensor(out=xs, in0=bank(bi)[:, :], in1=xs, op=ALU.add),
                           reads=[bk(bi), ("x", b)], writes=[("x", b)])
                    else:
                        op("dve", lambda h, bi=bi, xs=xs, b=b, gc=gc: h.scalar_tensor_tensor(
                            out=xs, in0=bank(bi)[:, :], scalar=gates[:, b, gc:gc + 1], in1=xs, op0=ALU.mult, op1=ALU.add),
                           reads=[bk(bi), ("x", b), ("gates", b)], writes=[("x", b)])
                if gi + 1 < len(groups):
                    load_wd(gi + 1, half)

    def final_store(nblk, row0):
        op("sp", lambda h: h.dma_start(out=bo_bc[:], in_=final_norm.partition_broadcast(128)), writes=["bo_bc"], lane="bo_bc")
        for b in range(nblk):
            s = b % 2
            ssc = stat[:, 4 * s:4 * s + 1]
            rsc = stat[:, 4 * s + 1:4 * s + 2]
            op("act", lambda h, b=b, ssc=ssc: h.activation(out=sqj[:], in_=x_sb[:, b, :], func=AF.Square, accum_out=ssc),
               reads=[("x", b)], writes=["sqj", ("stat", s)])
            op("act", lambda h, ssc=ssc, rsc=rsc: h.activation(out=rsc, in_=ssc, func=AF.Sqrt, bias=EPS, scale=1.0 / D),
               reads=[("stat", s)], writes=[("statr", s)])
            op("dve", lambda h, rsc=rsc: h.reciprocal(out=rsc, in_=rsc), reads=[("statr", s)], writes=[("statr", s)])
            op("dve", lambda h, b=b, s=s, rsc=rsc: h.scalar_tensor_tensor(out=ost[s], in0=x_sb[:, b, :], scalar=rsc, in1=bo_bc[:],
                                                                          op0=ALU.mult, op1=ALU.mult),
               reads=[("x", b), ("statr", s), "bo_bc"], writes=[("ost", s)])
            op("sp", lambda h, b=b, s=s: h.dma_start(out=out_d[row0 + b * 128:row0 + (b + 1) * 128, :], in_=ost[s]),
               reads=[("ost", s)], lane=("ost", s))

    ffn_units = [(ffn_wg[0], ffn_wu[0], ffn_wd[0], D_FF, None)]
    moe_units = [(moe_wg[0, e], moe_wu[0, e], moe_wd[0, e], D_FFE, e) for e in range(NEXP)]

    def load_x(tok0, nblk):
        for b in range(nblk):
            op("sp", lambda h, b=b: h.dma_start(out=x_sb[:, b, :], in_=x_d[tok0 + b * 128:tok0 + (b + 1) * 128, :]),
               writes=[("x", b)], lane=("x", b))

    def mixer_layer(l, nblk, full=True):
        load_mixer_weights(l)
        rms_transpose(gA, l, nblk)
        P.retire(ACT_KEYS, MIX_KEYS + [("uTc", c) for c in range(4)] + [("yc", c) for c in range(4)])
        b0 = 0
        while b0 < nblk:
            nsb = min(SW // 128, nblk - b0)
            mixer_sub(l, b0, nsb, full=full)
            b0 += nsb
        P.retire(MIX_KEYS + [("uTc", c) for c in range(4)] + [("yc", c) for c in range(4)], ACT_KEYS)

    load_x(0, HALO_BLKS)
    mixer_layer(0, HALO_BLKS)
    rms_transpose(gF, 0, HALO_BLKS)
    swiglu_phase(ffn_units, HALO_BLKS)
    mixer_layer(1, HALO_BLKS, full=False)
    for l in range(2):
        op("dve", lambda h, l=l: h.tensor_scalar(out=vprev[:, l, :, :], in0=vprev[:, l, :, :], scalar1=flag[:, 0:1], scalar2=None,
                                                 op0=ALU.mult), reads=[("vprev", l), "flag"], writes=[("vprev", l)])
        op("dve", lambda h, l=l: h.tensor_scalar(out=utail[:, l, :, :], in0=utail[:, l, :, :], scalar1=flag[:, 0:1], scalar2=None,
                                                 op0=ALU.mult), reads=[("utail", l), "flag"], writes=[("utail", l)])
    for st in range(n_st):
        tok0 = (HALO_BLKS + st * nb) * 128
        load_x(tok0, nb)
        mixer_layer(0, nb)
        rms_transpose(gF, 0, nb)
        swiglu_phase(ffn_units, nb)
        mixer_layer(1, nb)
        assert sparse
        if True:
            HTK = [("hT", b) for b in range(NBMAX)]
            TKK = [("htok", b) for b in range(NBMAX)]
            P.retire(HTK, TKK)
            rms_moe(nb)
            moe_positions(nb)
            if dbg:
                op("sp", lambda h, st=st: h.dma_start(out=dbg_d[st:st + 1, :], in_=ncnt[0:1, 0:8]), reads=["ncnt"], lane="dbg")
            P.retire(ACT_KEYS, SPARSE_KEYS)
            P.retire([("ost", 0), ("ost", 1)], ["S"])
            P.retire([("hn", 0), ("hn", 1)], [("G", i) for i in range(NT)])
            nr = (nb * 128 + CAP - 1) // CAP
            assert nr <= 4
            cur_st[0] = st
            for r in range(nr):
                op("dve", lambda h, r=r: h.tensor_scalar(out=rposr_t[:, r, 0:8 * nb], in0=rpos[:, 0:8 * nb], scalar1=float(-CAP * r),
                                                         scalar2=None, op0=ALU.add), reads=["rpos"], writes=[("rposr", r)])
            cur_r[0] = 0
            swiglu_phase(moe_units, nb, sparse_mode=True, elastic=(BASE < CAP), wmode=("store" if st == 0 else "scr"))
            for r in range(1, nr):
                cur_r[0] = r
                for e in range(NEXP):
                    rgn[0] += 1
                    P.begin_region(cnt8_i[0:1, e:e + 1], CAP * r, light=True, cond_id=("ov", st, e), cond_key="cnt8")
                    swiglu_phase([moe_units[e]], nb, sparse_mode=True, elastic=False, wmode=("hbm" if st == 0 else "scr"))
                    P.end_region()
            cur_r[0] = 0
            P.retire(SPARSE_KEYS, ACT_KEYS)
            P.retire(["S"], [("ost", 0), ("ost", 1)])
            P.retire([("G", i) for i in range(NT)], [("hn", 0), ("hn", 1)])
            P.retire(TKK, HTK)
        final_store(nb, st * nb * 128)

    P.emit(final_lanes=[("ost", 0), ("ost", 1)] + (["dbg"] if dbg else []))
    P.stack.close()
    return nc, P


def _q_perm():
    idx = np.zeros(512, dtype=np.int64)
    for j in range(4):
        for kv in range(2):
            for d in range(64):
                idx[j * 128 + kv * 64 + d] = (kv * 4 + j) * 64 + d
    return idx


def prep_weights(inputs):
    f = lambda a: np.ascontiguousarray(np.asarray(a, dtype=np.float32))
    w = {k: f(v) for k, v in inputs.items() if k != "x"}
    perm = np.arange(IN_COLS)
    perm[1024:1536] = 1024 + _q_perm()
    w["w_in"] = np.ascontiguousarray(w["w_in"][:, :, perm])
    w["b_in"] = np.ascontiguousarray(w["b_in"][:, perm])
    return w


_CACHE = {}


def kernel(**inputs):
    x = np.asarray(inputs["x"], dtype=np.float32)
    B, S, _ = x.shape
    w = prep_weights(inputs)
    n_st, nb = 4, 8
    per = n_st * nb * 128
    halves = S // per
    assert B * halves == N_CORES
    if "prog" not in _CACHE:
        nc, P = build_program(n_st, nb)
        _CACHE["prog"] = (nc, P)
    nc, P = _CACHE["prog"]
    in_maps = []
    for c in range(N_CORES):
        b, hf = c // halves, c % halves
        xs = np.zeros((HALO_BLKS * 128 + per, D), dtype=np.float32)
        xs[HALO_BLKS * 128:] = x[b, hf * per:(hf + 1) * per]
        if hf > 0:
            xs[:HALO_BLKS * 128] = x[b, hf * per - HALO_BLKS * 128:hf * per]
        m = dict(w)
        m["x"] = xs
        m["flag"] = np.full((128, 1), 1.0 if hf > 0 else 0.0, dtype=np.float32)
        in_maps.append(m)
    res = run_bass_kernel_spmd(nc, in_maps, core_ids=list(range(N_CORES)))
    out = np.zeros((B, S, D), dtype=np.float32)
    for c in range(N_CORES):
        b, hf = c // halves, c % halves
        out[b, hf * per:(hf + 1) * per] = res.results[c]["out"]
    return out
```

```python
import contextlib
import numpy as np
import concourse.bass as bass
import concourse.mybir as mybir
from concourse.bass_utils import run_bass_kernel_spmd

F32 = mybir.dt.float32
BF16 = mybir.dt.bfloat16
AF = mybir.ActivationFunctionType
ALU = mybir.AluOpType
AX = mybir.AxisListType

ENGS = ("pe", "act", "dve", "pool", "sp")
SAME_ENGINE_SYNC = True

D = 1024
NCH = 8
IN_COLS = 1792
CONV_K = 31
D_FF = 2816
D_FFE = 3584
NEXP = 8
EPS = 1e-5
HALO_BLKS = 2
N_CORES = 8


class Instr:
    __slots__ = ("eng", "idx", "fn", "waits", "lane", "lane_ord", "needs_inc", "inc_count", "clock", "region")


class Prog:
    def __init__(self, nc):
        self.nc = nc
        self.streams = {e: [] for e in ENGS}
        self.last_w = {}
        self.readers = {}
        self.clock = {e: {} for e in ENGS}
        self.lane_n = {}
        self.lane_last = {}
        self.stack = contextlib.ExitStack()
        self.cur_region = None
        self.regions = []
        self.markers = {}
        self.nbar = 0

    def barrier(self):
        bid = self.nbar
        self.nbar += 1
        for e in ENGS:
            fn, reads, writes, lane = self.markers[e]
            self.op(e, fn, reads=list(reads), writes=list(writes) + [("bar", bid, e)], lane=lane)
        for e in ENGS:
            self.op(e, None, reads=[("bar", bid, e2) for e2 in ENGS if e2 != e], register=False)

    def begin_region(self, cond_ap, thresh, light=False, cond_id=None, cond_key=None, cmp="IS_GT"):
        if light:
            for e in ENGS:
                if cond_key is not None:
                    self.op(e, None, reads=[cond_key], register=False)
                if self.streams[e]:
                    last = self.streams[e][-1]
                    if last.lane is None and last.fn is not None:
                        last.needs_inc = True
                    else:
                        for x in reversed(self.streams[e]):
                            if x.lane is None and x.fn is not None:
                                x.needs_inc = True
                                break
        else:
            self.barrier()
        self._snap = {e: dict(self.clock[e]) for e in ENGS}
        self.regions.append({"cond": cond_ap, "thresh": thresh, "light": light, "snap": self._snap, "cond_id": cond_id, "cmp": cmp})
        self.cur_region = len(self.regions) - 1

    def end_region(self):
        light = self.regions[self.cur_region]["light"]
        self.cur_region = None
        for e in ENGS:
            self.clock[e] = dict(self._snap[e])
        if not light:
            self.barrier()

    def sb(self, name, shape, dtype):
        return self.stack.enter_context(self.nc.sbuf_tensor("sb_" + name, list(shape), dtype))

    def ps(self, name, shape, dtype):
        return self.stack.enter_context(self.nc.psum_tensor("ps_" + name, list(shape), dtype))

    def _dom(self, p):
        return ("L", p.lane) if p.lane is not None else p.eng

    def _ord(self, p):
        return p.lane_ord if p.lane is not None else p.idx + 1

    def op(self, eng, fn, reads=(), writes=(), lane=None, register=True):
        ins = Instr()
        ins.eng = eng
        ins.fn = fn
        ins.lane = lane
        ins.needs_inc = False
        ins.inc_count = 0
        ins.region = self.cur_region
        st = self.streams[eng]
        ins.idx = len(st)
        deps = []
        for k in reads:
            w = self.last_w.get(k)
            if w is not None:
                deps.append((w, True))
        for k in writes:
            w = self.last_w.get(k)
            if w is not None:
                deps.append((w, True))
            rd = self.readers.get(k)
            if rd:
                for r in rd.values():
                    deps.append((r, False))
        if lane is not None:
            n = self.lane_n.get(lane, 0) + 1
            self.lane_n[lane] = n
            ins.lane_ord = n
            prev = self.lane_last.get(lane)
            if prev is not None:
                deps.append((prev, True))
            self.lane_last[lane] = ins
        else:
            ins.lane_ord = 0
        clk = self.clock[eng]
        waits = []
        for p, raw in deps:
            if p is ins:
                continue
            d = self._dom(p)
            o = self._ord(p)
            if p.lane is None and p.eng == eng:
                if not (SAME_ENGINE_SYNC and raw) or eng in ("pe", "sp"):
                    continue
            if clk.get(d, 0) >= o:
                continue
            waits.append(p)
            if p.region is not None and p.region != self.cur_region:
                src = dict(self.regions[p.region]["snap"][p.eng])
                src[d] = o
            else:
                src = p.clock
            for dd, oo in src.items():
                if clk.get(dd, 0) < oo:
                    clk[dd] = oo
            if p.lane is None:
                p.needs_inc = True
        ins.waits = waits
        c = dict(clk)
        c[self._dom(ins)] = self._ord(ins)
        ins.clock = c
        dom = self._dom(ins)
        for k in (reads if register else ()):
            rd = self.readers.get(k)
            if rd is None:
                rd = self.readers[k] = {}
            rd[dom] = ins
        for k in writes:
            self.last_w[k] = ins
            self.readers[k] = {}
        st.append(ins)
        return ins

    def retire(self, old_keys, new_keys):
        pend = {}

        def add(p):
            d = self._dom(p)
            q = pend.get(d)
            if q is None or self._ord(q) < self._ord(p):
                pend[d] = p
        for k in old_keys:
            w = self.last_w.pop(k, None)
            if w is not None:
                add(w)
            rd = self.readers.pop(k, None)
            if rd:
                for p in rd.values():
                    add(p)
        for k in new_keys:
            self.last_w[k] = None
            self.readers[k] = dict(pend)

    def emit(self, final_lanes=()):
        nc = self.nc
        sems = {e: self.stack.enter_context(nc.semaphore("s_" + e)) for e in ENGS}
        lane_sems = {}
        for i, l in enumerate(self.lane_n):
            lane_sems[l] = self.stack.enter_context(nc.semaphore("l%d" % i))
        for e in ENGS:
            c = 0
            for ins in self.streams[e]:
                if ins.needs_inc:
                    c += 1
                ins.inc_count = c

        def run(e, h):
            stream = self.streams[e]
            reg = [None]
            state = {"cur": None, "guard": None, "first": 0}

            def open_region(R, i0):
                if reg[0] is None:
                    reg[0] = h.alloc_register("creg_" + e)
                rg = self.regions[R]
                cond_ap, thresh = rg["cond"], rg["thresh"]
                h.reg_load(reg[0], cond_ap)
                g = h.If_cmp(reg[0], thresh, rg["cmp"])
                g.__enter__()
                state["guard"] = g
                state["first"] = i0

            def close_region(R, i1):
                state["guard"].__exit__(None, None, None)
                body = stream[state["first"]:i1]
                k = sum(1 for x in body if x.needs_inc and x.lane is None)
                pre = stream[state["first"] - 1].inc_count if state["first"] > 0 else 0
                lanes = {}
                for x in body:
                    if x.lane is not None:
                        d = lanes.setdefault(x.lane, [x.lane_ord - 1, 0])
                        d[1] += 1
                if k > 0 or lanes:
                    with h.Else():
                        if k > 0:
                            h.wait_ge(sems[e], pre)
                            h.sem_inc(sems[e], k)
                        for l, (pl, kl) in lanes.items():
                            h.wait_ge(lane_sems[l], 16 * pl)
                            h.sem_inc(lane_sems[l], 16 * kl)

            for i, ins in enumerate(stream):
                if ins.region != state["cur"]:
                    if state["cur"] is not None:
                        close_region(state["cur"], i)
                    if ins.region is not None:
                        open_region(ins.region, i)
                    state["cur"] = ins.region
                for p in ins.waits:
                    if p.lane is not None:
                        h.wait_ge(lane_sems[p.lane], 16 * p.lane_ord)
                    else:
                        h.wait_ge(sems[p.eng], p.inc_count)
                if ins.fn is None:
                    assert not ins.needs_inc
                    continue
                bi = ins.fn(h)
                if ins.lane is not None:
                    bi.then_inc(lane_sems[ins.lane], 16)
                elif ins.needs_inc:
                    bi.then_inc(sems[e], 1)
            if state["cur"] is not None:
                close_region(state["cur"], len(stream))
            if e == "sp":
                for l in final_lanes:
                    h.wait_ge(lane_sems[l], 16 * self.lane_n[l])

        with nc.Block() as block:
            @block.tensor
            def _(h):
                run("pe", h)

            @block.scalar
            def _(h):
                run("act", h)

            @block.vector
            def _(h):
                run("dve", h)

            @block.gpsimd
            def _(h):
                run("pool", h)

            @block.sync
            def _(h):
                run("sp", h)


def build_program(n_st=4, nb=8, cap=384, sparse=True, dbg=False, base=384):
    nc = bass.Bass("TRN2", target_bir_lowering=False)
    ntok = (HALO_BLKS + n_st * nb) * 128
    dr = {}

    def din(name, shape):
        dr[name] = nc.dram_tensor(name, list(shape), F32, kind="ExternalInput").ap()
        return dr[name]

    x_d = din("x", [ntok, D])
    flag_d = din("flag", [128, 1])
    attn_norm = din("attn_norm", [2, D])
    ffn_norm = din("ffn_norm", [2, D])
    w_in = din("w_in", [2, D, IN_COLS])
    b_in = din("b_in", [2, IN_COLS])
    conv_w = din("conv_w", [2, CONV_K, 512])
    conv_b = din("conv_b", [2, 512])
    conv_ln_g = din("conv_ln_g", [2, 512])
    conv_ln_b = din("conv_ln_b", [2, 512])
    sinks = din("sinks", [2, 8])
    w_out = din("w_out", [2, D, D])
    b_out = din("b_out", [2, D])
    ffn_wg = din("ffn_w_gate", [1, D, D_FF])
    ffn_wu = din("ffn_w_up", [1, D, D_FF])
    ffn_wd = din("ffn_w_down", [1, D_FF, D])
    moe_router = din("moe_router", [1, D, NEXP])
    moe_wg = din("moe_w_gate", [1, NEXP, D, D_FFE])
    moe_wu = din("moe_w_up", [1, NEXP, D, D_FFE])
    moe_wd = din("moe_w_down", [1, NEXP, D_FFE, D])
    final_norm = din("final_norm", [D])
    out_d = nc.dram_tensor("out", [n_st * nb * 128, D], F32, kind="ExternalOutput").ap()
    dbg_d = nc.dram_tensor("dbg", [n_st, 8], F32, kind="ExternalOutput").ap() if dbg else None

    scr_gu = nc.dram_tensor("scr_gu", [NEXP, 14, 2, 128, 2048], BF16, kind="Internal").ap()
    scr_wd = nc.dram_tensor("scr_wd", [NEXP, 2, 2, 128, 7168], BF16, kind="Internal").ap()

    P = Prog(nc)
    op = P.op
    uid = [0]

    def lane_name(s):
        return s

    NBMAX = max(nb, HALO_BLKS)
    x_sb = P.sb("x_sb", [128, NBMAX, D], F32)
    hT = P.sb("hT", [128, NCH, NBMAX * 128], BF16)
    hT32 = P.sb("hT32", [128, NCH, 128], F32)
    hn_t = P.sb("hn", [128, 2, D], F32)
    hn = [hn_t[:, i, :] for i in range(2)]
    sqj = P.sb("sqj", [128, D], BF16)
    stat = P.sb("stat", [128, 8], F32)
    wdbuf = P.sb("wdbuf", [128, 14336], BF16)
    gubuf = P.sb("gubuf", [128, 6 * 2048], BF16)
    arena = P.sb("arena", [128, 14336], BF16)
    arena32 = arena.bitcast(F32)
    sgs = [P.sb("sgs%d" % i, [128, 512], BF16) for i in range(2)]
    SW = 512
    qT = P.sb("qT", [128, 4, SW], BF16)
    kT = P.sb("kT", [128, 128 + SW], BF16)
    vaug = P.sb("vaug", [128, 1 + SW // 128, 2, 65], BF16)
    yT = P.sb("yT", [128, NCH, SW], BF16)
    lnt = [P.sb("lnt%d" % i, [128, SW], F32) for i in range(2)]
    sgm = [P.sb("sgm%d" % i, [128, 512], F32) for i in range(2)]
    eT = [P.sb("eT%d" % i, [128, 512], BF16) for i in range(2)]
    pT = [P.sb("pT%d" % i, [128, 512], BF16) for i in range(8)]
    o_sb = P.sb("o_sb", [128, 512], F32)
    dent = P.sb("dent", [128, 16], F32)
    utail = P.sb("utail", [128, 2, 4, 30], BF16)
    NDG = 12
    dg = P.sb("dg", [128, NDG, 128], BF16)
    kprev = P.sb("kprev", [128, 2, 128], BF16)
    vprev = P.sb("vprev", [128, 2, 2, 65], BF16)
    ident = P.sb("ident", [128, 128], F32)
    ones32 = P.sb("ones32", [128, 128], F32)
    maskp = P.sb("maskp", [128, 128], BF16)
    maskc = P.sb("maskc", [128, 128], BF16)
    gA = P.sb("gA", [128, 2, 8], F32)
    gF = P.sb("gF", [128, 2, 8], F32)
    binT = P.sb("binT", [128, 2, 14], F32)
    bv_bc = P.sb("bv_bc", [128, 2, 128], F32)
    cw = P.sb("cw", [128, 2, 4, 32], F32)
    cb = P.sb("cb", [128, 2, 4], F32)
    lg = P.sb("lg", [128, 2, 4], F32)
    lb = P.sb("lb", [128, 2, 4], F32)
    esink = P.sb("esink", [128, 2, 8], F32)
    bo_bc = P.sb("bo_bc", [128, D], F32)
    wr = P.sb("wr", [128, 8, 8], F32)
    flag = P.sb("flag", [128, 1], F32)
    gates = P.sb("gates", [128, NBMAX, 8], F32)
    rt = P.sb("rt", [128, 64], F32)
    ost_t = P.sb("ost", [128, 2, D], F32)
    ost = [ost_t[:, i, :] for i in range(2)]

    I32 = mybir.dt.int32
    CAP = cap
    NT = (CAP + 127) // 128
    TW = [min(128, CAP - 128 * i) for i in range(NT)]
    ident_bf = P.sb("ident_bf", [128, 128], BF16)
    ustrict = P.sb("ustrict", [128, 128], F32)
    iota_row = P.sb("iota_row", [128, CAP], F32)
    mk = P.sb("mk", [128, 8], F32)
    rmask = P.sb("rmask", [128, 64], F32)
    rtot = P.sb("rtot", [128, 64], F32)
    roff = P.sb("roff", [128, 64], F32)
    rpos = P.sb("rpos", [128, 64], F32)
    rposr_t = P.sb("rposr", [128, 4, 64], F32)
    cur_r = [0]
    ghi = P.sb("ghi", [128, 64], BF16)
    g2 = P.sb("g2", [128, 64, 2], BF16)
    gpos = P.sb("gpos", [128, 4], F32)
    ncnt = P.sb("ncnt", [128, 8], F32)
    cnt_i = P.sb("cnt_i", [128, 1], I32)
    cnt8_i = P.sb("cnt8_i", [128, 8], I32)
    BASE = base
    rgn = [0]
    S_v = ost_t.bitcast(BF16)[:, :, :].rearrange("p a n -> p (a n)")[:, 0:8 * CAP].rearrange("p (b n) -> p b n", n=CAP)
    G_v = hn_t.bitcast(BF16)[:, :, :].rearrange("p a n -> p (a n)")[:, 0:NT * 1024].rearrange("p (i n) -> p i n", n=1024)
    pq = [P.ps("pq%d" % i, [128, 1024], F32) for i in range(4)]

    def bank(i):
        return pq[i // 2][:, (i % 2) * 512:(i % 2) * 512 + 512]

    def bk(i):
        return ("pb", i)

    cw_raw = x_sb[0:31, 0, :].rearrange("p (l c) -> p l c", c=512)
    iota_i = x_sb[:, 1, 0:CAP].bitcast(I32)
    actT = arena[:, :].rearrange("p (j n) -> p j n", n=1024)
    UW = 30 + SW
    uT = arena[:, 0:4 * UW].rearrange("p (c n) -> p c n", n=UW)
    Y0 = (2 * UW + 31) // 32 * 32
    yconv = arena32[:, Y0:Y0 + 4 * SW].rearrange("p (c n) -> p c n", n=SW)
    ysq = arena32[:, Y0 + 4 * SW:Y0 + 8 * SW].rearrange("p (c n) -> p c n", n=SW)
    assert Y0 + 8 * SW <= 7168
    actTe = arena[:, 0:14 * CAP].rearrange("p (j n) -> p j n", n=CAP)
    hTe_v = arena[:, 14 * CAP:22 * CAP].rearrange("p (c n) -> p c n", n=CAP)
    ye_v = arena[:, 22 * CAP:22 * CAP + NT * 1024].rearrange("p (i n) -> p i n", n=1024)
    assert 22 * CAP + NT * 1024 <= 14336
    SPARSE_KEYS = ([("actTe", j) for j in range(14)] + [("actTeX", j) for j in range(14)] + ["hTe", "hTeX"]
                   + [("ye", i, hf) for i in range(NT) for hf in range(2)])
    hT_flat = hT[:, :, :].rearrange("p c n -> p (c n)")

    def htok(b):
        return hT_flat[:, b * D:(b + 1) * D]
    ACT_KEYS = [("actT", j) for j in range(14)]
    MIX_KEYS = ["uT", "yconv", "ysq"]
    win_v = wdbuf[:, :].rearrange("p (c n) -> p c n", n=IN_COLS)
    wout_v = gubuf[:, 0:8192].rearrange("p (c n) -> p c n", n=D)
    WD_KEYS = [("wd", 0), ("wd", 1)]
    GU_KEYS = [("gu", s) for s in range(6)]

    def wd_v(h):
        return wdbuf[:, h * 7168:(h + 1) * 7168].rearrange("p (j n) -> p j n", n=512)

    def gu_v(s):
        return gubuf[:, s * 2048:(s + 1) * 2048].rearrange("p (c n) -> p c n", n=256)

    def small_dma(eng, out_ap, in_ap, writes, lane):
        def fn(h):
            with nc.allow_non_contiguous_dma(reason="tiny param load"):
                return h.dma_start(out=out_ap, in_=in_ap)
        op(eng, fn, writes=writes, lane=lane)

    op("pool", lambda h: h.memset(ident[:], 0.0), writes=["ident"])
    op("pool", lambda h: h.affine_select(out=ident[:], in_=ident[:], pattern=[[-1, 128]],
                                        compare_op=ALU.not_equal, fill=1.0, base=0, channel_multiplier=1),
       reads=["ident"], writes=["ident"])
    op("pool", lambda h: h.memset(ones32[:], 1.0), writes=["ones32"])
    op("pool", lambda h: h.memset(maskp[:], 1.0), writes=["maskp"])
    op("pool", lambda h: h.memset(maskc[:], 1.0), writes=["maskc"])
    op("pool", lambda h: h.affine_select(out=maskp[:], in_=maskp[:], pattern=[[-1, 128]],
                                        compare_op=ALU.is_gt, fill=0.0, base=0, channel_multiplier=1),
       reads=["maskp"], writes=["maskp"])
    op("pool", lambda h: h.affine_select(out=maskc[:], in_=maskc[:], pattern=[[1, 128]],
                                        compare_op=ALU.is_ge, fill=0.0, base=0, channel_multiplier=-1),
       reads=["maskc"], writes=["maskc"])
    op("pool", lambda h: h.memset(vaug[:], 1.0), writes=["vaug"])
    op("pool", lambda h: h.memset(vprev[:], 0.0), writes=["vprev"])
    op("pool", lambda h: h.memset(vprev[:, :, :, 64:65], 1.0), reads=["vprev"], writes=["vprev"])
    op("pool", lambda h: h.memset(kprev[:], 0.0), writes=["kprev"])
    op("pool", lambda h: h.memset(utail[:], 0.0), writes=["utail"])
    op("pool", lambda h: h.memset(cw[:], 0.0), writes=["cw"])

    op("pool", lambda h: h.tensor_copy(out=ident_bf[:], in_=ident[:]), reads=["ident"], writes=["ident_bf"])
    op("pool", lambda h: h.memset(ustrict[:], 1.0), writes=["ustrict"])
    op("pool", lambda h: h.affine_select(out=ustrict[:], in_=ustrict[:], pattern=[[1, 128]],
                                        compare_op=ALU.is_gt, fill=0.0, base=0, channel_multiplier=-1),
       reads=["ustrict"], writes=["ustrict"])
    op("pool", lambda h: h.iota(iota_i, pattern=[[1, CAP]], base=0, channel_multiplier=0), writes=[("x", 1)])
    op("pool", lambda h: h.tensor_copy(out=iota_row[:], in_=iota_i), reads=[("x", 1)], writes=["iota_row"])
    op("pool", lambda h: h.memset(mk[:], 0.0), writes=["mk0"])
    P.markers = {
        "pe": (lambda h: h.matmul(out=bank(7)[0:1, 0:1], lhsT=ones32[0:1, 0:1], rhs=ones32[0:1, 0:1], start=True, stop=True),
               ["ones32"], [bk(7)], None),
        "act": (lambda h: h.activation(out=mk[:, 0:1], in_=mk[:, 4:5], func=AF.Copy), ["mk0"], [("mk", "act")], None),
        "dve": (lambda h: h.memset(mk[:, 1:2], 0.0), ["mk0"], [("mk", "dve")], None),
        "pool": (lambda h: h.memset(mk[:, 2:3], 0.0), ["mk0"], [("mk", "pool")], None),
        "sp": (lambda h: h.dma_start(out=mk[:, 3:4], in_=mk[:, 5:6]), ["mk0"], [("mk", "sp")], ("bar", "sp")),
    }
    small_dma("sp", flag[:], flag_d, ["flag"], "c_flag")
    small_dma("sp", gA[:], attn_norm.rearrange("l (c p) -> p l c", p=128), ["gA"], "c_gA")
    small_dma("sp", gF[:], ffn_norm.rearrange("l (c p) -> p l c", p=128), ["gF"], "c_gF")
    small_dma("sp", binT[:], b_in.rearrange("l (c p) -> p l c", p=128), ["binT"], "c_binT")
    for l in range(2):
        small_dma("sp", bv_bc[:, l, :], b_in[l, 1664:1792].partition_broadcast(128), ["bv_bc%d" % l], "c_bv%d" % l)
        small_dma("sp", esink[:, l, :], sinks[l, :].partition_broadcast(128), ["esink%d" % l], "c_es%d" % l)
    small_dma("sp", cw_raw, conv_w.rearrange("l k c -> k l c"), [("x", 0)], "c_cwraw")
    small_dma("sp", cb[:], conv_b.rearrange("l (c p) -> p l c", p=128), ["cb"], "c_cb")
    small_dma("sp", lg[:], conv_ln_g.rearrange("l (c p) -> p l c", p=128), ["lg"], "c_lg")
    small_dma("sp", lb[:], conv_ln_b.rearrange("l (c p) -> p l c", p=128), ["lb"], "c_lb")
    small_dma("sp", wr[:], moe_router[0].rearrange("(c p) e -> p c e", p=128), ["wr"], "c_wr")
    op("act", lambda h: h.activation(out=esink[:], in_=esink[:], func=AF.Exp),
       reads=["esink0", "esink1"], writes=["esink"])
    for l in range(2):
        for c in range(4):
            op("pe", lambda h, l=l, c=c: h.transpose(out=bank(c)[:, 0:31], in_=cw_raw[0:31, l, c * 128:(c + 1) * 128],
                                                     identity=ident[0:31, 0:31]),
               reads=[("x", 0), "ident"], writes=[bk(c)])
            op("dve", lambda h, l=l, c=c: h.tensor_copy(out=cw[:, l, c, 0:31], in_=bank(c)[:, 0:31]),
               reads=[bk(c), "cw"], writes=[("cwT", l, c)])
    CW_KEYS = [("cwT", l, c) for l in range(2) for c in range(4)]

    pbrr = [0]
    dgc = [0]
    cur_st = [0]
    cur_cap = [CAP]
    CAP_LO = min(CAP, 256)

    def next_bank():
        i = pbrr[0] % 8
        pbrr[0] += 1
        return i

    def rms_stage_a(b, gain_bc=None):
        s = b % 2
        ssc = stat[:, 4 * s:4 * s + 1]
        rsc = stat[:, 4 * s + 1:4 * s + 2]
        op("act", lambda h: h.activation(out=sqj[:], in_=x_sb[:, b, :], func=AF.Square, accum_out=ssc),
           reads=[("x", b)], writes=["sqj", ("stat", s)])
        op("act", lambda h: h.activation(out=rsc, in_=ssc, func=AF.Sqrt, bias=EPS, scale=1.0 / D),
           reads=[("stat", s)], writes=[("statr", s)])
        op("dve", lambda h: h.reciprocal(out=rsc, in_=rsc), reads=[("statr", s)], writes=[("statr", s)])
        if gain_bc is None:
            op("dve", lambda h: h.tensor_scalar(out=hn[s], in0=x_sb[:, b, :], scalar1=rsc, scalar2=None, op0=ALU.mult),
               reads=[("x", b), ("statr", s)], writes=[("hn", s)])
        else:
            op("dve", lambda h: h.scalar_tensor_tensor(out=hn[s], in0=x_sb[:, b, :], scalar=rsc, in1=gain_bc, op0=ALU.mult, op1=ALU.mult),
               reads=[("x", b), ("statr", s), "bo_bc"], writes=[("hn", s)])

    def rms_transposes(b):
        s = b % 2
        pt = pq[b % 2]
        for c in range(NCH):
            op("pe", lambda h, c=c: h.transpose(out=pt[:, c * 128:(c + 1) * 128], in_=hn[s][:, c * 128:(c + 1) * 128], identity=ident[:]),
               reads=[("hn", s), "ident"], writes=[bk(2 * (b % 2) + c // 4)])
        return pt[:, :].rearrange("p (c t) -> p c t", t=128)

    def rms_transpose(gT, l, nblk):
        rms_stage_a(0)
        for b in range(nblk):
            if b + 1 < nblk:
                rms_stage_a(b + 1)
            ptv = rms_transposes(b)
            gbc = gT[:, l, :].unsqueeze(2).to_broadcast([128, NCH, 128])
            op("dve", lambda h, b=b, ptv=ptv, gbc=gbc: h.tensor_tensor(out=hT[:, :, b * 128:(b + 1) * 128], in0=ptv, in1=gbc, op=ALU.mult),
               reads=[bk(2 * (b % 2)), bk(2 * (b % 2) + 1), "gA", "gF"], writes=[("hT", b)])

    Lall = P.sb("Lall", [128, NBMAX, 8], F32)
    rtb = P.sb("rtb", [128, 6, NBMAX, 8], F32)
    rts = P.sb("rts", [128, 6, NBMAX], F32)

    def router_gates_all(nblk):
        L = Lall[:, 0:nblk, :]
        mk1, L2, mk2, t1 = (rtb[:, i, 0:nblk, :] for i in range(4))
        m1, m2, dd, w1, w2 = (rts[:, i, 0:nblk] for i in range(5))

        def bc(v):
            return v.unsqueeze(2).to_broadcast([128, nblk, 8])
        K = "rt"
        LK = [("Lall", b) for b in range(nblk)]
        op("dve", lambda h: h.tensor_reduce(out=m1, in_=L, axis=AX.X, op=ALU.max), reads=LK, writes=[K])
        op("dve", lambda h: h.tensor_tensor(out=mk1, in0=L, in1=bc(m1), op=ALU.is_equal), reads=LK + [K], writes=[K])
        op("dve", lambda h: h.scalar_tensor_tensor(out=L2, in0=mk1, scalar=-1e30, in1=L, op0=ALU.mult, op1=ALU.add), reads=LK + [K], writes=[K])
        op("dve", lambda h: h.tensor_reduce(out=m2, in_=L2, axis=AX.X, op=ALU.max), reads=[K], writes=[K])
        op("dve", lambda h: h.tensor_tensor(out=mk2, in0=L2, in1=bc(m2), op=ALU.is_equal), reads=[K], writes=[K])
        op("dve", lambda h: h.tensor_tensor(out=dd, in0=m2, in1=m1, op=ALU.subtract), reads=[K], writes=[K])
        op("act", lambda h: h.activation(out=dd, in_=dd, func=AF.Exp), reads=[K], writes=[K])
        op("dve", lambda h: h.tensor_scalar(out=w1, in0=dd, scalar1=1.0, scalar2=None, op0=ALU.add), reads=[K], writes=[K])
        op("dve", lambda h: h.reciprocal(out=w1, in_=w1), reads=[K], writes=[K])
        op("dve", lambda h: h.tensor_tensor(out=w2, in0=dd, in1=w1, op=ALU.mult), reads=[K], writes=[K])
        op("dve", lambda h: h.tensor_tensor(out=mk1, in0=mk1, in1=bc(w1), op=ALU.mult), reads=[K], writes=[K])
        op("dve", lambda h: h.tensor_tensor(out=t1, in0=mk2, in1=bc(w2), op=ALU.mult), reads=[K], writes=[K])
        op("dve", lambda h: h.tensor_tensor(out=gates[:, 0:nblk, :], in0=mk1, in1=t1, op=ALU.add), reads=[K],
           writes=[("gates", b) for b in range(nblk)])

    def rms_moe(nblk):
        op("sp", lambda h: h.dma_start(out=bo_bc[:], in_=ffn_norm[1, :].partition_broadcast(128)), writes=["bo_bc"], lane="bo_bc")
        rms_stage_a(0, bo_bc[:])
        for b in range(nblk):
            if b + 1 < nblk:
                rms_stage_a(b + 1, bo_bc[:])
            s = b % 2
            op("pool", lambda h, b=b, s=s: h.tensor_copy(out=htok(b), in_=hn[s]), reads=[("hn", s)], writes=[("htok", b)])
            ptv = rms_transposes(b)
            op("act", lambda h, ptv=ptv: h.activation(out=hT32[:], in_=ptv, func=AF.Copy),
               reads=[bk(2 * (b % 2)), bk(2 * (b % 2) + 1)], writes=["hT32"])
            rb = 4 + (b % 2)
            for c in range(NCH):
                op("pe", lambda h, c=c, rb=rb: h.matmul(out=bank(rb)[:, 0:8], lhsT=hT32[:, c, :], rhs=wr[:, c, :],
                                                        start=(c == 0), stop=(c == NCH - 1)),
                   reads=["hT32", "wr"], writes=[bk(rb)])
            op("act", lambda h, b=b, rb=rb: h.activation(out=Lall[:, b, :], in_=bank(rb)[:, 0:8], func=AF.Copy),
               reads=[bk(rb)], writes=[("Lall", b)])
        router_gates_all(nblk)

    def moe_positions(nblk):
        n = nblk * 8
        gk = [("gates", b) for b in range(nblk)]
        gflat = gates[:, 0:nblk, :].rearrange("p b e -> p (b e)")
        op("dve", lambda h: h.tensor_scalar(out=rmask[:, 0:n], in0=gflat, scalar1=0.0, scalar2=None, op0=ALU.is_gt),
           reads=gk, writes=["rmask"])
        pb = 6
        op("pe", lambda h: h.matmul(out=bank(pb)[:, 0:n], lhsT=ustrict[:], rhs=rmask[:, 0:n], start=True, stop=True),
           reads=["rmask", "ustrict"], writes=[bk(pb)])
        op("pe", lambda h: h.matmul(out=bank(pb)[:, 64:64 + n], lhsT=ones32[:], rhs=rmask[:, 0:n], start=True, stop=True),
           reads=["rmask", "ones32"], writes=[bk(pb)])
        op("dve", lambda h: h.tensor_copy(out=rtot[:, 0:n], in_=bank(pb)[:, 64:64 + n]), reads=[bk(pb)], writes=["rtot"])
        op("dve", lambda h: h.memset(roff[:, 0:8], 0.0), writes=["roff"])
        for b in range(1, nblk):
            op("dve", lambda h, b=b: h.tensor_tensor(out=roff[:, 8 * b:8 * b + 8], in0=roff[:, 8 * b - 8:8 * b],
                                                     in1=rtot[:, 8 * b - 8:8 * b], op=ALU.add),
               reads=["roff", "rtot"], writes=["roff"])
        op("dve", lambda h: h.tensor_tensor(out=rpos[:, 0:n], in0=bank(pb)[:, 0:n], in1=roff[:, 0:n], op=ALU.add),
           reads=[bk(pb), "roff"], writes=["rpos"])
        op("dve", lambda h: h.scalar_tensor_tensor(out=rpos[:, 0:n], in0=rpos[:, 0:n], scalar=1.0, in1=rmask[:, 0:n],
                                                   op0=ALU.add, op1=ALU.mult), reads=["rpos", "rmask"], writes=["rpos"])
        op("dve", lambda h: h.tensor_scalar(out=rpos[:, 0:n], in0=rpos[:, 0:n], scalar1=-1.0, scalar2=None, op0=ALU.add),
           reads=["rpos"], writes=["rpos"])
        lb_ = 8 * (nblk - 1)
        op("dve", lambda h: h.tensor_tensor(out=ncnt[:, 0:8], in0=roff[:, lb_:lb_ + 8], in1=rtot[:, lb_:lb_ + 8], op=ALU.add),
           reads=["roff", "rtot"], writes=["ncnt"])
        op("dve", lambda h: h.tensor_reduce(out=rt[:, 48:49], in_=ncnt[:, 0:8], axis=AX.X, op=ALU.max), reads=["ncnt"], writes=["rtmax"])
        op("dve", lambda h: h.tensor_copy(out=cnt_i[:], in_=rt[:, 48:49]), reads=["rtmax"], writes=["cnt_i"])
        op("dve", lambda h: h.tensor_copy(out=cnt8_i[:], in_=ncnt[:, 0:8]), reads=["ncnt"], writes=["cnt8"])
        op("dve", lambda h: h.tensor_copy(out=ghi[:, 0:n], in_=gflat), reads=gk, writes=["ghi"])
        op("dve", lambda h: h.tensor_copy(out=g2[:, 0:n, 0], in_=ghi[:, 0:n]), reads=["ghi"], writes=["g2"])
        op("dve", lambda h: h.tensor_tensor(out=g2[:, 0:n, 1], in0=gflat, in1=ghi[:, 0:n], op=ALU.subtract),
           reads=gk + ["ghi", "g2"], writes=["g2"])

    def sparse_sbuild(e, nblk):
        capv = cur_cap[0]
        ntv = (capv + 127) // 128
        tw = [min(128, capv - 128 * i) for i in range(ntv)]
        for b in range(nblk):
            op("dve", lambda h, b=b, r_=cur_r[0]: h.tensor_scalar(out=S_v[:, b, 0:capv], in0=iota_row[:, 0:capv], scalar1=rposr_t[:, r_, 8 * b + e:8 * b + e + 1],
                                                     scalar2=None, op0=ALU.is_equal),
               reads=[("rposr", cur_r[0]), "iota_row"], writes=["S"])

    def ext_begin(e):
        rgn[0] += 1
        P.begin_region(cnt8_i[0:1, e:e + 1], BASE, light=True, cond_id=("n", rgn[0] // 100000, cur_st[0], e), cond_key="cnt8")

    def col_sets(elastic):
        capv = cur_cap[0]
        ntv = (capv + 127) // 128
        tw = [min(128, capv - 128 * i) for i in range(ntv)]
        if elastic and BASE < capv:
            nbt = BASE // 128
            return [(0, BASE, False), (BASE, capv - BASE, True)], [(list(range(nbt)), False), (list(range(nbt, ntv)), True)]
        return [(0, capv, False)], [(list(range(ntv)), False)]

    def sparse_pre(e, nblk, gub, build_s=True, elastic=False):
        capv = cur_cap[0]
        ntv = (capv + 127) // 128
        tw = [min(128, capv - 128 * i) for i in range(ntv)]
        if build_s:
            sparse_sbuild(e, nblk)
        cols, tsets = col_sets(elastic)
        for (c0, w, cond), (tiles, _) in zip(cols, tsets):
            if cond:
                ext_begin(e)
            hk = "hTeX" if cond else "hTe"
            for c in range(NCH):
                bi = gub[0] % 4
                gub[0] += 1
                for b in range(nblk):
                    op("pe", lambda h, bi=bi, b=b, c=c, c0=c0, w=w: h.matmul(out=bank(bi)[:, c0:c0 + w], lhsT=htok(b)[:, c * 128:(c + 1) * 128],
                                                                             rhs=S_v[:, b, c0:c0 + w], start=(b == 0), stop=(b == nblk - 1)),
                       reads=["S", ("htok", b)], writes=[bk(bi)])
                op("act", lambda h, bi=bi, c=c, c0=c0, w=w: h.activation(out=hTe_v[:, c, c0:c0 + w], in_=bank(bi)[:, c0:c0 + w], func=AF.Copy),
                   reads=[bk(bi)], writes=[hk])
            gub[0] += gub[0] % 2
            for i in tiles:
                wi = tw[i]
                for b in range(nblk):
                    op("pe", lambda h, i=i, b=b, wi=wi: h.matmul(out=bank(6)[0:wi, 2 * i:2 * i + 2], lhsT=S_v[:, b, i * 128:i * 128 + wi],
                                                                 rhs=g2[:, 8 * b + e, :], start=(b == 0), stop=(b == nblk - 1)),
                       reads=["S", "g2"], writes=[bk(6)])
            for i in tiles:
                wi = tw[i]
                g6 = bank(6)[0:wi, 2 * i:2 * i + 2]
                op("dve", lambda h, i=i, wi=wi, g6=g6: h.tensor_reduce(out=gpos[0:wi, i:i + 1], in_=g6, axis=AX.X, op=ALU.add),
                   reads=[bk(6)], writes=[("gpos", i)])
            pqb = pq[3].bitcast(BF16)
            for i in tiles:
                wi = tw[i]
                for b in range(nblk):
                    op("pe", lambda h, i=i, b=b, wi=wi: h.transpose(out=pqb[0:wi, 1024 + b * 128:1024 + (b + 1) * 128],
                                                                    in_=S_v[:, b, i * 128:i * 128 + wi], identity=ident_bf[:]),
                       reads=["S", "ident_bf"], writes=[bk(7)])
                op("act", lambda h, i=i, wi=wi: h.activation(out=G_v[0:wi, i, 0:nblk * 128], in_=pqb[0:wi, 1024:1024 + nblk * 128], func=AF.Copy),
                   reads=[bk(7)], writes=[("G", i)])
            if cond:
                P.end_region()

    def sparse_scatter(e, nblk, elastic=False):
        capv = cur_cap[0]
        ntv = (capv + 127) // 128
        tw = [min(128, capv - 128 * i) for i in range(ntv)]
        cols, tsets = col_sets(elastic)
        for tiles, cond in tsets:
            if cond:
                ext_begin(e)
            for b in range(nblk):
                for half in range(2):
                    bi = 4 + ((2 * b + half) % 2)
                    for i in tiles:
                        wi = tw[i]
                        op("pe", lambda h, bi=bi, i=i, b=b, half=half, wi=wi, tiles=tiles: h.matmul(
                            out=bank(bi)[:, :], lhsT=G_v[0:wi, i, b * 128:(b + 1) * 128], rhs=ye_v[0:wi, i, half * 512:(half + 1) * 512],
                            start=(i == tiles[0]), stop=(i == tiles[-1])),
                           reads=[("G", i), ("ye", i, half)], writes=[bk(bi)])
                    xs = x_sb[:, b, half * 512:(half + 1) * 512]
                    op("dve", lambda h, bi=bi, xs=xs: h.tensor_tensor(out=xs, in0=bank(bi)[:, :], in1=xs, op=ALU.add),
                       reads=[bk(bi), ("x", b)], writes=[("x", b)])
            if cond:
                P.end_region()

    def load_mixer_weights(l):
        for hh in range(2):
            op("pool", lambda h, hh=hh: h.dma_start(out=win_v[:, 4 * hh:4 * hh + 4, :],
                                                    in_=w_in[l, 512 * hh:512 * hh + 512, :].rearrange("(c p) n -> p c n", p=128)),
               writes=[("wd", hh)], lane=("wd", hh))
        for hh in range(2):
            op("pool", lambda h, hh=hh: h.dma_start(out=wout_v[:, 4 * hh:4 * hh + 4, :],
                                                    in_=w_out[l, 512 * hh:512 * hh + 512, :].rearrange("(c p) n -> p c n", p=128)),
               writes=[("gu", 2 * hh), ("gu", 2 * hh + 1)], lane=("gu", 2 * hh))
        op("sp", lambda h: h.dma_start(out=bo_bc[:], in_=b_out[l, :].partition_broadcast(128)), writes=["bo_bc"], lane="bo_bc")

    def mixer_sub(l, b0, nsb, full=True):
        W = nsb * 128
        c0 = b0 * 128
        WK = WD_KEYS
        op("pool", lambda h: h.tensor_copy(out=uT[:, :, 0:30], in_=utail[:, l, :, :]), reads=[("utail", l), "uT"], writes=["uT"])
        op("pool", lambda h: h.tensor_copy(out=kT[:, 0:128], in_=kprev[:, l, :]), reads=[("kprev", l), "kT"], writes=["kT"])
        op("pool", lambda h: h.tensor_copy(out=vaug[:, 0, :, :], in_=vprev[:, l, :, :]), reads=[("vprev", l), "vaug"], writes=["vaug"])
        if full:
            for b in range(b0, b0 + nsb):
                op("pool", lambda h, b=b: h.tensor_tensor(out=x_sb[:, b, :], in0=x_sb[:, b, :], in1=bo_bc[:], op=ALU.add),
                   reads=[("x", b), "bo_bc"], writes=[("x", b)])
        hkeys = [("hT", b) for b in range(b0, b0 + nsb)]
        for c in range(4):
            ia, ig = next_bank(), next_bank()
            for (m, bi) in ((c, ia), (4 + c, ig)):
                for k in range(NCH):
                    op("pe", lambda h, m=m, bi=bi, k=k: h.matmul(out=bank(bi)[:, 0:W], lhsT=win_v[:, k, m * 128:(m + 1) * 128],
                                                                 rhs=hT[:, k, c0:c0 + W], start=(k == 0), stop=(k == NCH - 1)),
                       reads=WK + hkeys, writes=[bk(bi)])
            s = c % 2
            op("act", lambda h, c=c, ig=ig, s=s: h.activation(out=sgm[s][:, 0:W], in_=bank(ig)[:, 0:W], func=AF.Sigmoid,
                                                             bias=binT[:, l, 4 + c:5 + c]),
               reads=[bk(ig), "binT"], writes=[("sgm", s)])
            op("dve", lambda h, c=c, ia=ia, s=s: h.scalar_tensor_tensor(out=uT[:, c, 30:30 + W], in0=bank(ia)[:, 0:W],
                                                                       scalar=binT[:, l, c:c + 1], in1=sgm[s][:, 0:W],
                                                                       op0=ALU.add, op1=ALU.mult),
               reads=[bk(ia), ("sgm", s), "binT", "uT"], writes=[("uTc", c)])
        UC = [("uTc", c) for c in range(4)]
        for j in range(4):
            bi = next_bank()
            for k in range(NCH):
                op("pe", lambda h, j=j, bi=bi, k=k: h.matmul(out=bank(bi)[:, 0:W], lhsT=win_v[:, k, (8 + j) * 128:(9 + j) * 128],
                                                             rhs=hT[:, k, c0:c0 + W], start=(k == 0), stop=(k == NCH - 1)),
                   reads=WK + hkeys, writes=[bk(bi)])
            op("act", lambda h, j=j, bi=bi: h.activation(out=qT[:, j, 0:W], in_=bank(bi)[:, 0:W], func=AF.Identity,
                                                         bias=binT[:, l, 8 + j:9 + j]),
               reads=[bk(bi), "binT"], writes=[("qT", j)])
        bi = next_bank()
        for k in range(NCH):
            op("pe", lambda h, bi=bi, k=k: h.matmul(out=bank(bi)[:, 0:W], lhsT=win_v[:, k, 1536:1664],
                                                    rhs=hT[:, k, c0:c0 + W], start=(k == 0), stop=(k == NCH - 1)),
               reads=WK + hkeys, writes=[bk(bi)])
        op("act", lambda h, bi=bi: h.activation(out=kT[:, 128:128 + W], in_=bank(bi)[:, 0:W], func=AF.Identity,
                                                bias=binT[:, l, 12:13]),
           reads=[bk(bi), "binT", "kT"], writes=["kTn"])
        for i in range(nsb):
            bi = next_bank()
            for k in range(NCH):
                op("pe", lambda h, bi=bi, k=k, i=i: h.matmul(out=bank(bi)[:, 0:128], lhsT=hT[:, k, c0 + i * 128:c0 + (i + 1) * 128],
                                                             rhs=win_v[:, k, 1664:1792], start=(k == 0), stop=(k == NCH - 1)),
                   reads=WK + hkeys, writes=[bk(bi)])
            op("dve", lambda h, bi=bi, i=i: h.tensor_tensor(out=vaug[:, 1 + i, :, 0:64],
                                                            in0=bank(bi)[:, 0:128].rearrange("p (a d) -> p a d", d=64),
                                                            in1=bv_bc[:, l, :].rearrange("p (a d) -> p a d", d=64), op=ALU.add),
               reads=[bk(bi), "bv_bc%d" % l, "vaug"], writes=[("vaug", 1 + i)])
        VK = [("vaug", 1 + i) for i in range(nsb)]
        op("pool", lambda h: h.tensor_copy(out=utail[:, l, :, :], in_=uT[:, :, W:W + 30]), reads=UC + ["uT"], writes=[("utail", l)])
        op("pool", lambda h: h.tensor_copy(out=kprev[:, l, :], in_=kT[:, W:W + 128]), reads=["kTn", "kT"], writes=[("kprev", l)])
        op("pool", lambda h: h.tensor_copy(out=vprev[:, l, :, :], in_=vaug[:, nsb, :, :]), reads=VK + ["vaug"], writes=[("vprev", l)])
        if not full:
            return
        for c in range(4):
            bi = next_bank()
            for k in range(CONV_K):
                sl = dgc[0] % NDG
                dgc[0] += 1
                de = ("act", "dve")[dgc[0] % 2]
                if de == "act":
                    op("act", lambda h, c=c, k=k, sl=sl: h.activation(out=dg[:, sl, :], in_=ident_bf[:], func=AF.Copy,
                                                                     scale=cw[:, l, c, k:k + 1]),
                       reads=["ident_bf"] + CW_KEYS, writes=[("dg", sl)])
                else:
                    op(de, lambda h, c=c, k=k, sl=sl: h.tensor_scalar(out=dg[:, sl, :], in0=ident_bf[:], scalar1=cw[:, l, c, k:k + 1],
                                                                      scalar2=None, op0=ALU.mult),
                       reads=["ident_bf"] + CW_KEYS, writes=[("dg", sl)])
                op("pe", lambda h, c=c, k=k, sl=sl, bi=bi: h.matmul(out=bank(bi)[:, 0:W], lhsT=dg[:, sl, :], rhs=uT[:, c, k:k + W],
                                                                    start=(k == 0), stop=(k == CONV_K - 1)),
                   reads=[("dg", sl), ("uTc", c), "uT"], writes=[bk(bi)])
            op("act", lambda h, c=c, bi=bi: h.activation(out=yconv[:, c, 0:W], in_=bank(bi)[:, 0:W], func=AF.Identity,
                                                         bias=cb[:, l, c:c + 1]),
               reads=[bk(bi), "cb"], writes=[("yc", c)])
        YC = [("yc", c) for c in range(4)]
        op("act", lambda h: h.activation(out=ysq[:, :, 0:W], in_=yconv[:, :, 0:W], func=AF.Square), reads=YC, writes=["ysq"])
        b1, b2 = next_bank(), next_bank()
        for c in range(4):
            op("pe", lambda h, c=c: h.matmul(out=bank(b1)[:, 0:W], lhsT=ones32[:], rhs=yconv[:, c, 0:W], start=(c == 0), stop=(c == 3)),
               reads=YC + ["ones32"], writes=[bk(b1)])
        for c in range(4):
            op("pe", lambda h, c=c: h.matmul(out=bank(b2)[:, 0:W], lhsT=ones32[:], rhs=ysq[:, c, 0:W], start=(c == 0), stop=(c == 3)),
               reads=["ysq", "ones32"], writes=[bk(b2)])
        mean, var = lnt[0], lnt[1]
        op("act", lambda h: h.activation(out=mean[:, 0:W], in_=bank(b1)[:, 0:W], func=AF.Copy, scale=1.0 / 512), reads=[bk(b1)], writes=["mean"])
        op("dve", lambda h: h.tensor_tensor(out=var[:, 0:W], in0=mean[:, 0:W], in1=mean[:, 0:W], op=ALU.mult), reads=["mean"], writes=["var"])
        op("dve", lambda h: h.scalar_tensor_tensor(out=var[:, 0:W], in0=bank(b2)[:, 0:W], scalar=1.0 / 512, in1=var[:, 0:W],
                                                   op0=ALU.mult, op1=ALU.subtract), reads=[bk(b2), "var"], writes=["var"])
        op("act", lambda h: h.activation(out=var[:, 0:W], in_=var[:, 0:W], func=AF.Sqrt, bias=EPS), reads=["var"], writes=["var"])
        op("dve", lambda h: h.reciprocal(out=var[:, 0:W], in_=var[:, 0:W]), reads=["var"], writes=["var"])
        op("dve", lambda h: h.tensor_tensor(out=yconv[:, :, 0:W], in0=yconv[:, :, 0:W],
                                            in1=mean[:, 0:W].unsqueeze(1).to_broadcast([128, 4, W]), op=ALU.subtract),
           reads=YC + ["mean"], writes=YC)
        op("dve", lambda h: h.tensor_tensor(out=yconv[:, :, 0:W], in0=yconv[:, :, 0:W],
                                            in1=var[:, 0:W].unsqueeze(1).to_broadcast([128, 4, W]), op=ALU.mult),
           reads=YC + ["var"], writes=YC)
        for c in range(4):
            op("act", lambda h, c=c: h.activation(out=yT[:, c, 0:W], in_=yconv[:, c, 0:W], func=AF.Silu,
                                                  bias=lb[:, l, c:c + 1], scale=lg[:, l, c:c + 1]),
               reads=YC + ["lg", "lb"], writes=[("yT", c)])
        def att_scores(i):
            for kv in range(2):
                r0 = 64 * kv
                for kb in range(2):
                    bi = next_bank()
                    while bi >= 6:
                        bi = next_bank()
                    op("pe", lambda h, bi=bi, kb=kb, r0=r0: h.matmul(
                        out=bank(bi)[:, :], lhsT=kT[r0:r0 + 64, (i + kb) * 128:(i + kb + 1) * 128],
                        rhs=qT[r0:r0 + 64, :, i * 128:(i + 1) * 128], start=True, stop=True),
                       reads=["kT", "kTn"] + [("qT", j) for j in range(4)], writes=[bk(bi)])
                    es = kb
                    ps_ = 4 * (i % 2) + 2 * kv + kb
                    op("act", lambda h, bi=bi, es=es: h.activation(out=eT[es][:], in_=bank(bi)[:, :], func=AF.Exp, scale=0.125),
                       reads=[bk(bi)], writes=[("eT", es)])
                    mk_ = maskp if kb == 0 else maskc
                    op("pool", lambda h, es=es, ps_=ps_, mk_=mk_: h.tensor_tensor(
                        out=pT[ps_][:, :].rearrange("p (j q) -> p j q", q=128), in0=eT[es][:, :].rearrange("p (j q) -> p j q", q=128),
                        in1=mk_[:, :].unsqueeze(1).to_broadcast([128, 4, 128]), op=ALU.mult),
                       reads=[("eT", es), "maskp", "maskc"], writes=[("pT", ps_)])

        def att_pv(i):
            po = pq[3]
            for kv in range(2):
                for j in range(4):
                    hh = kv * 4 + j
                    for kb in range(2):
                        ps_ = 4 * (i % 2) + 2 * kv + kb
                        op("pe", lambda h, hh=hh, kv=kv, j=j, kb=kb, ps_=ps_: h.matmul(
                            out=po[:, hh * 128:hh * 128 + 65], lhsT=pT[ps_][:, j * 128:(j + 1) * 128],
                            rhs=vaug[:, i + kb, kv, :], start=(kb == 0), stop=(kb == 1)),
                           reads=[("pT", ps_), "vaug"] + VK, writes=[bk(6 + hh // 4)])
            pov = po[:, :].rearrange("p (h d) -> p h d", d=128)
            op("dve", lambda h: h.tensor_tensor(out=dent[:, 0:8], in0=pov[:, :, 64], in1=esink[:, l, :], op=ALU.add),
               reads=[bk(6), bk(7), "esink"], writes=["dent"])
            op("dve", lambda h: h.reciprocal(out=dent[:, 8:16], in_=dent[:, 0:8]), reads=["dent"], writes=["dent2"])
            op("dve", lambda h: h.tensor_tensor(out=o_sb[:, :].rearrange("p (h d) -> p h d", d=64), in0=pov[:, :, 0:64],
                                                in1=dent[:, 8:16].unsqueeze(2).to_broadcast([128, 8, 64]), op=ALU.mult),
               reads=[bk(6), bk(7), "dent2"], writes=["o_sb"])
            bi = next_bank()
            while bi >= 6:
                bi = next_bank()
            for j in range(4):
                op("pe", lambda h, bi=bi, j=j: h.transpose(out=bank(bi)[:, j * 128:(j + 1) * 128], in_=o_sb[:, j * 128:(j + 1) * 128],
                                                           identity=ident[:]),
                   reads=["o_sb", "ident"], writes=[bk(bi)])
            op("act", lambda h, bi=bi: h.activation(out=yT[:, 4:8, i * 128:(i + 1) * 128],
                                                    in_=bank(bi)[:, :].rearrange("p (j q) -> p j q", q=128), func=AF.Copy),
               reads=[bk(bi)], writes=[("yTa", i)])

        att_scores(0)
        for i in range(nsb):
            if i + 1 < nsb:
                att_scores(i + 1)
            att_pv(i)
        GK = [("gu", s) for s in range(4)]
        for i in range(nsb):
            b = b0 + i
            for half in range(2):
                bi = next_bank()
                for c in range(NCH):
                    op("pe", lambda h, bi=bi, c=c, i=i, half=half: h.matmul(
                        out=bank(bi)[:, :], lhsT=yT[:, c, i * 128:(i + 1) * 128], rhs=wout_v[:, c, half * 512:(half + 1) * 512],
                        start=(c == 0), stop=(c == NCH - 1)),
                       reads=GK + [("yT", c) for c in range(4)] + [("yTa", i)], writes=[bk(bi)])
                op("dve", lambda h, bi=bi, b=b, half=half: h.tensor_tensor(out=x_sb[:, b, half * 512:(half + 1) * 512],
                                                                           in0=bank(bi)[:, :], in1=x_sb[:, b, half * 512:(half + 1) * 512],
                                                                           op=ALU.add),
                   reads=[bk(bi), ("x", b)], writes=[("x", b)])

    def swiglu_phase(units, nblk, sparse_mode=False, elastic=False, wmode="hbm"):
        capv = cur_cap[0]
        ntv = (capv + 127) // 128
        tw = [min(128, capv - 128 * i) for i in range(ntv)]
        T = capv if sparse_mode else nblk * 128
        ncg = (T + 511) // 512
        groups = []
        for ui, (wg, wu, wd, F, gc) in enumerate(units):
            nchunk = F // 128
            g0 = (nchunk + 1) // 2
            groups.append((ui, 0, g0))
            groups.append((ui, g0, nchunk - g0))
        pairs = []
        for gi, (ui, cs, n) in enumerate(groups):
            j = 0
            while j < n:
                m = min(2, n - j)
                pairs.append((gi, cs + j, m))
                j += m
        NSLOT = 3

        def load_pair(pi):
            gi, cs, m = pairs[pi]
            ui = groups[gi][0]
            wg, wu = units[ui][0], units[ui][1]
            e_ = units[ui][4]
            pidx = cs // 2
            s = pi % NSLOT
            for (w, off) in ((wg, 0), (wu, 1)):
                slot = 2 * s + off
                flat = gubuf[:, slot * 2048:(slot + 1) * 2048]
                if wmode == "scr":
                    op("sp", lambda h, slot=slot, flat=flat, off=off: h.dma_start(out=flat, in_=scr_gu[e_, pidx, off]),
                       reads=[("scrgu", e_, pidx, off)], writes=[("gu", slot)], lane=("gu", slot))
                    continue
                op("pool", lambda h, w=w, slot=slot, cs=cs, m=m: h.dma_start(
                    out=gu_v(slot)[:, :, 0:128 * m], in_=w[:, cs * 128:(cs + m) * 128].rearrange("(c p) n -> p c n", p=128)),
                   writes=[("gu", slot)], lane=("gu", slot))
                if wmode == "store":
                    op("sp", lambda h, flat=flat, off=off: h.dma_start(out=scr_gu[e_, pidx, off], in_=flat),
                       reads=[("gu", slot)], writes=[("scrgu", e_, pidx, off)], lane=("sts", slot))

        def load_wd(gi, half):
            ui, cs, n = groups[gi]
            wd = units[ui][2]
            e_ = units[ui][4]
            g_ = gi % 2
            flat = wdbuf[:, half * 7168:(half + 1) * 7168]
            if wmode == "scr":
                op("sp", lambda h: h.dma_start(out=flat, in_=scr_wd[e_, g_, half]),
                   reads=[("scrwd", e_, g_, half)], writes=[("wd", half)], lane=("wd", half))
                return
            op("pool", lambda h: h.dma_start(out=wd_v(half)[:, 0:n, :],
                                             in_=wd[cs * 128:(cs + n) * 128, half * 512:(half + 1) * 512].rearrange("(j p) n -> p j n", p=128)),
               writes=[("wd", half)], lane=("wd", half))
            if wmode == "store":
                op("sp", lambda h: h.dma_start(out=scr_wd[e_, g_, half], in_=flat),
                   reads=[("wd", half)], writes=[("scrwd", e_, g_, half)], lane=("stw", half))

        LOOK = 2
        for pi in range(min(LOOK, len(pairs))):
            load_pair(pi)
        load_wd(0, 0)
        load_wd(0, 1)
        hkeys = ["hTe"] if sparse_mode else [("hT", b) for b in range(nblk)]
        cols, tsets = col_sets(elastic) if sparse_mode else (None, None)
        akey = "actTe" if sparse_mode else "actT"
        abuf = actTe if sparse_mode else actT
        gub = [0]
        pi = 0
        for gi, (ui, cs, n) in enumerate(groups):
            gc = units[ui][4]
            first_group = (gi % 2 == 0)
            if sparse_mode and first_group:
                sparse_pre(gc, nblk, gub, build_s=(gi == 0), elastic=elastic)
            pig = 0
            while pi < len(pairs) and pairs[pi][0] == gi:
                _, pcs, m = pairs[pi]
                if pi + LOOK < len(pairs):
                    load_pair(pi + LOOK)
                pig += 1
                s = pi % NSLOT
                for jj in range(m):
                    jl = pcs + jj - cs
                    if sparse_mode:
                        bg = gub[0] % 4
                        bu = (gub[0] + 1) % 4
                        gub[0] += 2
                        ss = (gub[0] // 2) % 2
                        for (c0, w, cond) in cols:
                            if cond:
                                ext_begin(gc)
                            for (slot, bi) in ((2 * s, bg), (2 * s + 1, bu)):
                                for k in range(NCH):
                                    op("pe", lambda h, slot=slot, bi=bi, k=k, jj=jj, c0=c0, w=w: h.matmul(
                                        out=bank(bi)[:, c0:c0 + w], lhsT=gu_v(slot)[:, k, jj * 128:(jj + 1) * 128],
                                        rhs=hTe_v[:, k, c0:c0 + w], start=(k == 0), stop=(k == NCH - 1)),
                                       reads=[("gu", slot), "hTeX" if cond else "hTe"], writes=[bk(bi)])
                            sk = ("sgsX" if cond else "sgs", ss)
                            op("act", lambda h, bg=bg, ss=ss, c0=c0, w=w: h.activation(out=sgs[ss][:, c0:c0 + w], in_=bank(bg)[:, c0:c0 + w],
                                                                                      func=AF.Silu),
                               reads=[bk(bg)], writes=[sk])
                            op("dve", lambda h, bu=bu, ss=ss, jl=jl, c0=c0, w=w: h.tensor_tensor(
                                out=actTe[:, jl, c0:c0 + w], in0=bank(bu)[:, c0:c0 + w], in1=sgs[ss][:, c0:c0 + w], op=ALU.mult),
                               reads=[bk(bu), sk], writes=[("actTeX" if cond else "actTe", jl)])
                            if cond:
                                P.end_region()
                        continue
                    for cg in range(ncg):
                        wcols = min(512, T - cg * 512)
                        bg = gub[0] % 4
                        bu = (gub[0] + 1) % 4
                        gub[0] += 2
                        for (slot, bi) in ((2 * s, bg), (2 * s + 1, bu)):
                            for k in range(NCH):
                                op("pe", lambda h, slot=slot, bi=bi, k=k, jj=jj, cg=cg, wcols=wcols: h.matmul(
                                    out=bank(bi)[:, 0:wcols], lhsT=gu_v(slot)[:, k, jj * 128:(jj + 1) * 128],
                                    rhs=hT[:, k, cg * 512:cg * 512 + wcols], start=(k == 0), stop=(k == NCH - 1)),
                                   reads=[("gu", slot)] + hkeys, writes=[bk(bi)])
                        ss = (gub[0] // 2) % 2
                        op("act", lambda h, bg=bg, ss=ss, wcols=wcols: h.activation(out=sgs[ss][:, 0:wcols], in_=bank(bg)[:, 0:wcols],
                                                                                   func=AF.Silu),
                           reads=[bk(bg)], writes=[("sgs", ss)])
                        op("dve", lambda h, bu=bu, ss=ss, jl=jl, cg=cg, wcols=wcols: h.tensor_tensor(
                            out=actT[:, jl, cg * 512:cg * 512 + wcols], in0=bank(bu)[:, 0:wcols], in1=sgs[ss][:, 0:wcols], op=ALU.mult),
                           reads=[bk(bu), ("sgs", ss)], writes=[("actT", jl)])
                pi += 1
            if sparse_mode:
                if (not first_group) and gi + 1 < len(groups):
                    sparse_sbuild(units[groups[gi + 1][0]][4], nblk)
                for half in range(2):
                    for tiles, cond in tsets:
                        if cond:
                            ext_begin(gc)
                        for i in tiles:
                            wi = tw[i]
                            bi = 4 + (i % 2)
                            for jl in range(n):
                                op("pe", lambda h, bi=bi, jl=jl, i=i, half=half, n=n, wi=wi: h.matmul(
                                    out=bank(bi)[0:wi, :], lhsT=actTe[:, jl, i * 128:i * 128 + wi], rhs=wd_v(half)[:, jl, :],
                                    start=(jl == 0), stop=(jl == n - 1)),
                                   reads=[("actTeX" if cond else "actTe", jl), ("wd", half)], writes=[bk(bi)])
                            yv = ye_v[0:wi, i, half * 512:(half + 1) * 512]
                            if first_group:
                                op("act", lambda h, bi=bi, yv=yv, i=i, wi=wi: h.activation(out=yv, in_=bank(bi)[0:wi, :], func=AF.Copy,
                                                                                          scale=gpos[0:wi, i:i + 1]),
                                   reads=[bk(bi), ("gpos", i)], writes=[("ye", i, half)])
                            else:
                                op("dve", lambda h, bi=bi, yv=yv, i=i, wi=wi: h.scalar_tensor_tensor(
                                    out=yv, in0=bank(bi)[0:wi, :], scalar=gpos[0:wi, i:i + 1], in1=yv, op0=ALU.mult, op1=ALU.add),
                                   reads=[bk(bi), ("gpos", i), ("ye", i, half)], writes=[("ye", i, half)])
                        if cond:
                            P.end_region()
                    if gi + 1 < len(groups):
                        load_wd(gi + 1, half)
                if not first_group:
                    sparse_scatter(gc, nblk, elastic=elastic)
                continue
            for half in range(2):
                for b in range(nblk):
                    bi = 4 + (b % 2)
                    for jl in range(n):
                        op("pe", lambda h, bi=bi, jl=jl, b=b, half=half, n=n: h.matmul(
                            out=bank(bi)[:, :], lhsT=actT[:, jl, b * 128:(b + 1) * 128], rhs=wd_v(half)[:, jl, :],
                            start=(jl == 0), stop=(jl == n - 1)),
                           reads=[("actT", jl), ("wd", half)], writes=[bk(bi)])
                    xs = x_sb[:, b, half * 512:(half + 1) * 512]
                    if gc is None:
                        op("dve", lambda h, bi=bi, xs=xs: h.tensor_tensor(out=xs, in0=bank(bi)[:, :], in1=xs, op=ALU.add),
                           reads=[bk(bi), ("x", b)], writes=[("x", b)])
                    else:
                        op("dve", lambda h, bi=bi, xs=xs, b=b, gc=gc: h.scalar_tensor_tensor(
                            out=xs, in0=bank(bi)[:, :], scalar=gates[:, b, gc:gc + 1], in1=xs, op0=ALU.mult, op1=ALU.add),
                           reads=[bk(bi), ("x", b), ("gates", b)], writes=[("x", b)])
                if gi + 1 < len(groups):
                    load_wd(gi + 1, half)

    def final_store(nblk, row0):
        op("sp", lambda h: h.dma_start(out=bo_bc[:], in_=final_norm.partition_broadcast(128)), writes=["bo_bc"], lane="bo_bc")
        for b in range(nblk):
            s = b % 2
            ssc = stat[:, 4 * s:4 * s + 1]
            rsc = stat[:, 4 * s + 1:4 * s + 2]
            op("act", lambda h, b=b, ssc=ssc: h.activation(out=sqj[:], in_=x_sb[:, b, :], func=AF.Square, accum_out=ssc),
               reads=[("x", b)], writes=["sqj", ("stat", s)])
            op("act", lambda h, ssc=ssc, rsc=rsc: h.activation(out=rsc, in_=ssc, func=AF.Sqrt, bias=EPS, scale=1.0 / D),
               reads=[("stat", s)], writes=[("statr", s)])
            op("dve", lambda h, rsc=rsc: h.reciprocal(out=rsc, in_=rsc), reads=[("statr", s)], writes=[("statr", s)])
            op("dve", lambda h, b=b, s=s, rsc=rsc: h.scalar_tensor_tensor(out=ost[s], in0=x_sb[:, b, :], scalar=rsc, in1=bo_bc[:],
                                                                          op0=ALU.mult, op1=ALU.mult),
               reads=[("x", b), ("statr", s), "bo_bc"], writes=[("ost", s)])
            op("sp", lambda h, b=b, s=s: h.dma_start(out=out_d[row0 + b * 128:row0 + (b + 1) * 128, :], in_=ost[s]),
               reads=[("ost", s)], lane=("ost", s))

    ffn_units = [(ffn_wg[0], ffn_wu[0], ffn_wd[0], D_FF, None)]
    moe_units = [(moe_wg[0, e], moe_wu[0, e], moe_wd[0, e], D_FFE, e) for e in range(NEXP)]

    def load_x(tok0, nblk):
        for b in range(nblk):
            op("sp", lambda h, b=b: h.dma_start(out=x_sb[:, b, :], in_=x_d[tok0 + b * 128:tok0 + (b + 1) * 128, :]),
               writes=[("x", b)], lane=("x", b))

    def mixer_layer(l, nblk, full=True):
        load_mixer_weights(l)
        rms_transpose(gA, l, nblk)
        P.retire(ACT_KEYS, MIX_KEYS + [("uTc", c) for c in range(4)] + [("yc", c) for c in range(4)])
        b0 = 0
        while b0 < nblk:
            nsb = min(SW // 128, nblk - b0)
            mixer_sub(l, b0, nsb, full=full)
            b0 += nsb
        P.retire(MIX_KEYS + [("uTc", c) for c in range(4)] + [("yc", c) for c in range(4)], ACT_KEYS)

    load_x(0, HALO_BLKS)
    mixer_layer(0, HALO_BLKS)
    rms_transpose(gF, 0, HALO_BLKS)
    swiglu_phase(ffn_units, HALO_BLKS)
    mixer_layer(1, HALO_BLKS, full=False)
    for l in range(2):
        op("dve", lambda h, l=l: h.tensor_scalar(out=vprev[:, l, :, :], in0=vprev[:, l, :, :], scalar1=flag[:, 0:1], scalar2=None,
                                                 op0=ALU.mult), reads=[("vprev", l), "flag"], writes=[("vprev", l)])
        op("dve", lambda h, l=l: h.tensor_scalar(out=utail[:, l, :, :], in0=utail[:, l, :, :], scalar1=flag[:, 0:1], scalar2=None,
                                                 op0=ALU.mult), reads=[("utail", l), "flag"], writes=[("utail", l)])
    for st in range(n_st):
        tok0 = (HALO_BLKS + st * nb) * 128
        load_x(tok0, nb)
        mixer_layer(0, nb)
        rms_transpose(gF, 0, nb)
        swiglu_phase(ffn_units, nb)
        mixer_layer(1, nb)
        assert sparse
        if True:
            HTK = [("hT", b) for b in range(NBMAX)]
            TKK = [("htok", b) for b in range(NBMAX)]
            P.retire(HTK, TKK)
            rms_moe(nb)
            moe_positions(nb)
            if dbg:
                op("sp", lambda h, st=st: h.dma_start(out=dbg_d[st:st + 1, :], in_=ncnt[0:1, 0:8]), reads=["ncnt"], lane="dbg")
            P.retire(ACT_KEYS, SPARSE_KEYS)
            P.retire([("ost", 0), ("ost", 1)], ["S"])
            P.retire([("hn", 0), ("hn", 1)], [("G", i) for i in range(NT)])
            nr = (nb * 128 + CAP - 1) // CAP
            assert nr <= 4
            cur_st[0] = st
            for r in range(nr):
                op("dve", lambda h, r=r: h.tensor_scalar(out=rposr_t[:, r, 0:8 * nb], in0=rpos[:, 0:8 * nb], scalar1=float(-CAP * r),
                                                         scalar2=None, op0=ALU.add), reads=["rpos"], writes=[("rposr", r)])
            cur_r[0] = 0
            wm = "store" if st == 0 else "scr"
            if st == 0:
                swiglu_phase(moe_units, nb, sparse_mode=True, elastic=False, wmode=wm)
            for e in (range(NEXP) if st > 0 else ()):
                for (capx, thr, cmpx) in ((CAP, CAP_LO, "IS_GT"), (CAP_LO, CAP_LO + 1, "IS_LT")):
                    rgn[0] += 1
                    P.begin_region(cnt8_i[0:1, e:e + 1], thr, light=True, cond_id=("v", rgn[0]), cond_key="cnt8", cmp=cmpx)
                    cur_cap[0] = capx
                    swiglu_phase([moe_units[e]], nb, sparse_mode=True, elastic=False, wmode=wm)
                    P.end_region()
            cur_cap[0] = CAP
            for r in range(1, nr):
                cur_r[0] = r
                for e in range(NEXP):
                    rgn[0] += 1
                    P.begin_region(cnt8_i[0:1, e:e + 1], CAP * r, light=True, cond_id=("ov", st, e), cond_key="cnt8")
                    swiglu_phase([moe_units[e]], nb, sparse_mode=True, elastic=False, wmode=("hbm" if st == 0 else "scr"))
                    P.end_region()
            cur_r[0] = 0
            P.retire(SPARSE_KEYS, ACT_KEYS)
            P.retire(["S"], [("ost", 0), ("ost", 1)])
            P.retire([("G", i) for i in range(NT)], [("hn", 0), ("hn", 1)])
            P.retire(TKK, HTK)
        final_store(nb, st * nb * 128)

    P.emit(final_lanes=[("ost", 0), ("ost", 1)] + (["dbg"] if dbg else []))
    P.stack.close()
    return nc, P


def _q_perm():
    idx = np.zeros(512, dtype=np.int64)
    for j in range(4):
        for kv in range(2):
            for d in range(64):
                idx[j * 128 + kv * 64 + d] = (kv * 4 + j) * 64 + d
    return idx


def prep_weights(inputs):
    f = lambda a: np.ascontiguousarray(np.asarray(a, dtype=np.float32))
    w = {k: f(v) for k, v in inputs.items() if k != "x"}
    perm = np.arange(IN_COLS)
    perm[1024:1536] = 1024 + _q_perm()
    w["w_in"] = np.ascontiguousarray(w["w_in"][:, :, perm])
    w["b_in"] = np.ascontiguousarray(w["b_in"][:, perm])
    return w


_CACHE = {}


def kernel(**inputs):
    x = np.asarray(inputs["x"], dtype=np.float32)
    B, S, _ = x.shape
    w = prep_weights(inputs)
    n_st, nb = 4, 8
    per = n_st * nb * 128
    halves = S // per
    assert B * halves == N_CORES
    if "prog" not in _CACHE:
        nc, P = build_program(n_st, nb)
        _CACHE["prog"] = (nc, P)
    nc, P = _CACHE["prog"]
    in_maps = []
    for c in range(N_CORES):
        b, hf = c // halves, c % halves
        xs = np.zeros((HALO_BLKS * 128 + per, D), dtype=np.float32)
        xs[HALO_BLKS * 128:] = x[b, hf * per:(hf + 1) * per]
        if hf > 0:
            xs[:HALO_BLKS * 128] = x[b, hf * per - HALO_BLKS * 128:hf * per]
        m = dict(w)
        m["x"] = xs
        m["flag"] = np.full((128, 1), 1.0 if hf > 0 else 0.0, dtype=np.float32)
        in_maps.append(m)
    res = run_bass_kernel_spmd(nc, in_maps, core_ids=list(range(N_CORES)))
    out = np.zeros((B, S, D), dtype=np.float32)
    for c in range(N_CORES):
        b, hf = c // halves, c % halves
        out[b, hf * per:(hf + 1) * per] = res.results[c]["out"]
    return out
```
